# Optimizing a Trainium2 kernel written in Bass

```python
import jax
import jax.numpy as jnp
from jax import lax
import numpy as np

D_MODEL = 2048
BATCH = 1
SEQ = 8192
DEPTH = 4

HEAD_DIM = 128
N_HEADS_NSA = 8
N_KV_NSA = 2
N_HEADS_SB = 8
D_MIX = (N_HEADS_NSA + N_HEADS_SB) * HEAD_DIM
ROPE_DIM = HEAD_DIM // 4
ROPE_THETA = 500000.0
CMP_BLOCK = 32
CMP_STRIDE = 16
CMP_HIDDEN = 256
SLC_BLOCK = 64
SLC_TOP_N = 16
WINDOW = 512
Q_BLOCK = 128
N_BRANCH = 3
D_FF = ((8 * D_MODEL + 3 * 256 - 1) // (3 * 256)) * 256
EPS = 1e-6
NEG_INF = -1e30
FORCE_SCORE = 1e4
NEG_SCORE = -1e4
SPLIT_SIZES = [N_HEADS_NSA * HEAD_DIM] + [N_KV_NSA * HEAD_DIM] * 6 + [N_BRANCH * N_HEADS_NSA] + [N_HEADS_SB * HEAD_DIM] * 3
D_IN = sum(SPLIT_SIZES)

kernel_name = 'hybrid_nsa_stickbreaking_trunk'


def rms_norm(x, g):
    xf = x.astype(jnp.float32)
    y = xf * lax.rsqrt(jnp.mean(xf * xf, axis=-1, keepdims=True) + EPS)
    return (y * g.astype(jnp.float32)).astype(x.dtype)


def rope_tables(pos):
    inv = ROPE_THETA ** (-jnp.arange(0, ROPE_DIM, 2, dtype=jnp.float32) / ROPE_DIM)
    ang = pos.astype(jnp.float32)[..., None] * inv
    return jnp.cos(ang)[:, None], jnp.sin(ang)[:, None]


def apply_partial_rope(x, cos, sin):
    half = ROPE_DIM // 2
    x1 = x[..., :half].astype(jnp.float32)
    x2 = x[..., half:ROPE_DIM].astype(jnp.float32)
    r1 = (x1 * cos - x2 * sin).astype(x.dtype)
    r2 = (x2 * cos + x1 * sin).astype(x.dtype)
    return jnp.concatenate([r1, r2, x[..., ROPE_DIM:]], axis=-1)


def masked_softmax(s, mask):
    s = jnp.where(mask, s.astype(jnp.float32), NEG_INF)
    p = jax.nn.softmax(s, axis=-1)
    return jnp.where(mask, p, 0.0)


def unblock(o, axis):
    o = jnp.moveaxis(o, 0, axis)
    shp = o.shape
    return o.reshape(shp[:axis] + (shp[axis] * shp[axis + 1],) + shp[axis + 2:])


def nsa_attention(q, k_c, v_c, k_s, v_s, k_w, v_w, gates, positions, cos, sin,
                  cmp_pos, w_ck1, w_ck2, w_cv1, w_cv2):
    B, H, S, d = q.shape
    G = k_c.shape[1]
    R = H // G
    scale = d ** -0.5
    q = apply_partial_rope(q, cos, sin)
    k_s = apply_partial_rope(k_s, cos, sin)
    k_w = apply_partial_rope(k_w, cos, sin)
    qg = q.reshape(B, G, R, S, d)
    t_idx = jnp.arange(S)

    n_cmp = (S - CMP_BLOCK) // CMP_STRIDE + 1
    blk_idx = jnp.arange(n_cmp)[:, None] * CMP_STRIDE + jnp.arange(CMP_BLOCK)[None, :]

    def compress(t, w1, w2):
        blocks = t[:, :, blk_idx, :] + cmp_pos
        flat = blocks.reshape(B, G, n_cmp, CMP_BLOCK * d)
        return jax.nn.gelu(flat @ w1) @ w2

    kc = compress(k_c, w_ck1, w_ck2)
    vc = compress(v_c, w_cv1, w_cv2)
    cmp_end = jnp.arange(n_cmp) * CMP_STRIDE + CMP_BLOCK - 1
    ccos, csin = rope_tables(positions[:, cmp_end])
    kc = apply_partial_rope(kc, ccos, csin)
    s_cmp = jnp.einsum('bgrsd,bgnd->bgrsn', qg, kc) * scale
    mask_cmp = cmp_end[None, :] <= t_idx[:, None]
    p_cmp = masked_softmax(s_cmp, mask_cmp)
    o_cmp = jnp.einsum('bgrsn,bgnd->bgrsd', p_cmp.astype(vc.dtype), vc)

    n_slc = S // SLC_BLOCK
    nb = CMP_BLOCK // CMP_STRIDE
    ratio = SLC_BLOCK // CMP_STRIDE
    p_imp = p_cmp.sum(axis=2)
    p_ext = jnp.pad(p_imp, ((0, 0), (0, 0), (0, 0), (nb - 1, ratio * n_slc - n_cmp)))
    q_imp = p_ext[..., nb - 1: nb - 1 + ratio * n_slc]
    for n in range(1, nb):
        q_imp = q_imp + p_ext[..., nb - 1 - n: nb - 1 - n + ratio * n_slc]
    p_slc = q_imp.reshape(B, G, S, n_slc, ratio).sum(axis=-1)
    blk_t = t_idx // SLC_BLOCK
    j = jnp.arange(n_slc)
    causal_blk = j[None, :] <= blk_t[:, None]
    forced = (j[None, :] == 0) | (j[None, :] == blk_t[:, None]) | (j[None, :] == blk_t[:, None] - 1)
    imp = jnp.where(causal_blk, jnp.where(forced, FORCE_SCORE, p_slc), NEG_SCORE)
    top_n = min(SLC_TOP_N, n_slc)
    sel_val, sel_idx = lax.top_k(imp, top_n)
    sel_ok = sel_val > 0.5 * NEG_SCORE

    k_blocks = k_s.reshape(B, G, n_slc, SLC_BLOCK, d)
    v_blocks = v_s.reshape(B, G, n_slc, SLC_BLOCK, d)
    k_w_pad = jnp.pad(k_w, ((0, 0), (0, 0), (WINDOW, 0), (0, 0)))
    v_w_pad = jnp.pad(v_w, ((0, 0), (0, 0), (WINDOW, 0), (0, 0)))
    bi = jnp.arange(B)[:, None, None, None]
    gi = jnp.arange(G)[None, :, None, None]
    off = jnp.arange(SLC_BLOCK)
    n_keys = top_n * SLC_BLOCK

    def block(nq):
        q0 = nq * Q_BLOCK
        qb = lax.dynamic_slice_in_dim(qg, q0, Q_BLOCK, axis=3)
        tq = q0 + jnp.arange(Q_BLOCK)
        idx = lax.dynamic_slice_in_dim(sel_idx, q0, Q_BLOCK, axis=2)
        ok = lax.dynamic_slice_in_dim(sel_ok, q0, Q_BLOCK, axis=2)
        ks = k_blocks[bi, gi, idx].reshape(B, G, Q_BLOCK, n_keys, d)
        vs = v_blocks[bi, gi, idx].reshape(B, G, Q_BLOCK, n_keys, d)
        kpos = (idx[..., None] * SLC_BLOCK + off).reshape(B, G, Q_BLOCK, n_keys)
        kok = jnp.broadcast_to(ok[..., None], (B, G, Q_BLOCK, top_n, SLC_BLOCK)).reshape(B, G, Q_BLOCK, n_keys)
        m_s = kok & (kpos <= tq[:, None])
        s_s = jnp.einsum('bgrqd,bgqkd->bgrqk', qb, ks) * scale
        p_s = masked_softmax(s_s, m_s[:, :, None])
        o_s = jnp.einsum('bgrqk,bgqkd->bgrqd', p_s.astype(vs.dtype), vs)
        kw = lax.dynamic_slice_in_dim(k_w_pad, q0, WINDOW + Q_BLOCK, axis=2)
        vw = lax.dynamic_slice_in_dim(v_w_pad, q0, WINDOW + Q_BLOCK, axis=2)
        wpos = q0 - WINDOW + jnp.arange(WINDOW + Q_BLOCK)
        diff = tq[:, None] - wpos[None, :]
        m_w = (wpos[None, :] >= 0) & (diff >= 0) & (diff < WINDOW)
        s_w = jnp.einsum('bgrqd,bgkd->bgrqk', qb, kw) * scale
        p_w = masked_softmax(s_w, m_w)
        o_w = jnp.einsum('bgrqk,bgkd->bgrqd', p_w.astype(vw.dtype), vw)
        return o_s, o_w

    o_slc, o_win = lax.map(block, jnp.arange(S // Q_BLOCK))
    o_slc = unblock(o_slc, 3).reshape(B, H, S, d)
    o_win = unblock(o_win, 3).reshape(B, H, S, d)
    o_cmp = o_cmp.reshape(B, H, S, d)
    return gates[:, 0] * o_cmp + gates[:, 1] * o_slc + gates[:, 2] * o_win


def stick_breaking_attention(q, k, v):
    B, H, S, d = q.shape
    scale = d ** -0.5
    kpos = jnp.arange(S)

    def block(nq):
        q0 = nq * Q_BLOCK
        qb = lax.dynamic_slice_in_dim(q, q0, Q_BLOCK, axis=2)
        tq = q0 + jnp.arange(Q_BLOCK)
        valid = kpos[None, :] < tq[:, None]
        z = jnp.einsum('bhqd,bhkd->bhqk', qb, k).astype(jnp.float32) * scale
        log_1m = jnp.where(valid, jax.nn.log_sigmoid(-z), 0.0)
        after = lax.cumsum(log_1m, axis=3, reverse=True) - log_1m
        a = jnp.where(valid, jnp.exp(jax.nn.log_sigmoid(z) + after), 0.0)
        return jnp.einsum('bhqk,bhkd->bhqd', a.astype(v.dtype), v)

    o = lax.map(block, jnp.arange(S // Q_BLOCK))
    return unblock(o, 2)


def hybrid_mixer(xn, positions, cos, sin, w_in, b_gate, cmp_pos, w_ck1, w_ck2,
                 w_cv1, w_cv2, g_grp, w_out):
    B, S, _ = xn.shape
    proj = xn @ w_in
    offsets = [int(o) for o in np.cumsum(SPLIT_SIZES)[:-1]]
    q_n, kc, vc, ksl, vsl, kw, vw, g_logit, q_sb, k_sb, v_sb = jnp.split(proj, offsets, axis=-1)

    def heads(t, h):
        return t.reshape(B, S, h, HEAD_DIM).transpose(0, 2, 1, 3)

    gates = jax.nn.sigmoid((g_logit + b_gate).astype(jnp.float32))
    gates = gates.reshape(B, S, N_BRANCH, N_HEADS_NSA).transpose(0, 2, 3, 1)[..., None]
    o_nsa = nsa_attention(heads(q_n, N_HEADS_NSA), heads(kc, N_KV_NSA), heads(vc, N_KV_NSA),
                          heads(ksl, N_KV_NSA), heads(vsl, N_KV_NSA), heads(kw, N_KV_NSA),
                          heads(vw, N_KV_NSA), gates, positions, cos, sin, cmp_pos,
                          w_ck1, w_ck2, w_cv1, w_cv2)
    o_sb = stick_breaking_attention(heads(q_sb, N_HEADS_SB), heads(k_sb, N_HEADS_SB), heads(v_sb, N_HEADS_SB))
    o = jnp.concatenate([o_nsa.astype(jnp.float32), o_sb.astype(jnp.float32)], axis=1)
    o = o.transpose(0, 2, 1, 3)
    o = o * lax.rsqrt(jnp.mean(o * o, axis=-1, keepdims=True) + EPS)
    o = o.reshape(B, S, D_MIX) * g_grp.astype(jnp.float32)
    return o.astype(xn.dtype) @ w_out


def swiglu(xn, w_gate, w_up, w_down):
    return (jax.nn.silu(xn @ w_gate) * (xn @ w_up)) @ w_down


def setup_inputs(seed: int = 0) -> dict:
    key = jax.random.key(seed)
    ks = jax.random.split(key, 16)
    f32 = jnp.float32

    def nrm(k, shape, scale):
        return jax.random.normal(k, shape, f32) * scale

    L = DEPTH
    return {
        'x': nrm(ks[0], (BATCH, SEQ, D_MODEL), 1.0),
        'positions': jnp.broadcast_to(jnp.arange(SEQ, dtype=jnp.int32)[None, :], (BATCH, SEQ)),
        'norm_mix': 1.0 + nrm(ks[1], (L, D_MODEL), 0.02),
        'w_in': nrm(ks[2], (L, D_MODEL, D_IN), D_MODEL ** -0.5),
        'b_gate': nrm(ks[3], (L, N_BRANCH * N_HEADS_NSA), 0.01),
        'cmp_pos': nrm(ks[4], (L, CMP_BLOCK, HEAD_DIM), 0.1),
        'w_cmp_k1': nrm(ks[5], (L, CMP_BLOCK * HEAD_DIM, CMP_HIDDEN), (CMP_BLOCK * HEAD_DIM) ** -0.5),
        'w_cmp_k2': nrm(ks[6], (L, CMP_HIDDEN, HEAD_DIM), CMP_HIDDEN ** -0.5),
        'w_cmp_v1': nrm(ks[7], (L, CMP_BLOCK * HEAD_DIM, CMP_HIDDEN), (CMP_BLOCK * HEAD_DIM) ** -0.5),
        'w_cmp_v2': nrm(ks[8], (L, CMP_HIDDEN, HEAD_DIM), CMP_HIDDEN ** -0.5),
        'norm_grp': 1.0 + nrm(ks[9], (L, D_MIX), 0.02),
        'w_out': nrm(ks[10], (L, D_MIX, D_MODEL), D_MIX ** -0.5),
        'norm_ffn': 1.0 + nrm(ks[11], (L, D_MODEL), 0.02),
        'w_gate': nrm(ks[12], (L, D_MODEL, D_FF), D_MODEL ** -0.5),
        'w_up': nrm(ks[13], (L, D_MODEL, D_FF), D_MODEL ** -0.5),
        'w_down': nrm(ks[14], (L, D_FF, D_MODEL), D_FF ** -0.5),
        'norm_final': 1.0 + nrm(ks[15], (D_MODEL,), 0.02),
    }


def reference(x, positions, norm_mix, w_in, b_gate, cmp_pos, w_cmp_k1, w_cmp_k2,
              w_cmp_v1, w_cmp_v2, norm_grp, w_out, norm_ffn, w_gate, w_up, w_down,
              norm_final):
    cos, sin = rope_tables(positions)
    h = x
    for layer in range(DEPTH):
        xn = rms_norm(h, norm_mix[layer])
        h = h + hybrid_mixer(xn, positions, cos, sin, w_in[layer], b_gate[layer], cmp_pos[layer],
                             w_cmp_k1[layer], w_cmp_k2[layer], w_cmp_v1[layer], w_cmp_v2[layer],
                             norm_grp[layer], w_out[layer]).astype(h.dtype)
        xn = rms_norm(h, norm_ffn[layer])
        h = h + swiglu(xn, w_gate[layer], w_up[layer], w_down[layer]).astype(h.dtype)
    return rms_norm(h, norm_final)
```

```python
from contextlib import ExitStack
import numpy as np
import ml_dtypes
import concourse.bass as bass
import concourse.mybir as mybir
from concourse.bass_utils import run_bass_kernel_spmd

F32 = mybir.dt.float32
BF16 = mybir.dt.bfloat16
I32 = mybir.dt.int32
ALU = mybir.AluOpType
AF = mybir.ActivationFunctionType
AX = mybir.AxisListType

NCORES = 8
D = 2048
S = 8192
TL = 1024
NK = 8
D_IN = 5656
D_FF = 5632
HD = 128
SCALE = HD ** -0.5
NEG = -30000.0
DV = 130
EPS = 1e-6
N_DMA_SEMS = 24
PI = float(np.pi)


class Buf:
    __slots__ = ("name", "w", "r", "psum")

    def __init__(self, name, psum=False):
        self.name = name
        self.w = None
        self.r = {}
        self.psum = psum


class Ctx:
    def __init__(self, nc):
        self.nc = nc
        self.es = ExitStack()
        self.eng = {"pe": nc.tensor, "act": nc.scalar, "dve": nc.vector,
                    "pool": nc.gpsimd, "sp": nc.sync}
        self.sems = {}
        self.cnt = {}
        self.seen = {k: {} for k in self.eng}
        for k in self.eng:
            self.sems[k] = self.es.enter_context(nc.semaphore("s_" + k))
            self.cnt[k] = 0
        for i in range(N_DMA_SEMS):
            k = "d%d" % i
            self.sems[k] = self.es.enter_context(nc.semaphore("s_" + k))
            self.cnt[k] = 0
        self.dma_rr = 0
        self.n_inst = 0

    def sb(self, name, shape, dt, es=None):
        t = (es or self.es).enter_context(self.nc.sbuf_tensor(name, list(shape), dt))
        return t, Buf(name)

    def ps(self, name, shape, dt=F32):
        t = self.es.enter_context(self.nc.psum_tensor(name, list(shape), dt))
        return t, Buf(name, psum=True)

    def _need(self, tok, needs):
        if tok is None:
            return
        sk, v = tok
        if needs.get(sk, 0) < v:
            needs[sk] = v

    def _deps(self, ek, reads, writes):
        needs = {}
        for b in reads:
            self._need(b.w, needs)
            if b.psum:
                for sk, v in b.r.items():
                    self._need((sk, v), needs)
        for b in writes:
            self._need(b.w, needs)
            for sk, v in b.r.items():
                self._need((sk, v), needs)
        e = self.eng[ek]
        seen = self.seen[ek]
        for sk, v in needs.items():
            if sk == ek and ek == "pe":
                continue
            if seen.get(sk, 0) < v:
                e.wait_ge(self.sems[sk], v)
                seen[sk] = v

    def _mark(self, tok, reads, writes):
        sk, v = tok
        for b in reads:
            if b.psum:
                b.w = tok
                b.r = {}
            elif b.r.get(sk, 0) < v:
                b.r[sk] = v
        for b in writes:
            b.w = tok
            b.r = {}

    def op(self, ek, fn, reads=(), writes=()):
        self._deps(ek, reads, writes)
        ins = fn(self.eng[ek])
        self.cnt[ek] += 1
        ins.then_inc(self.sems[ek], 1)
        tok = (ek, self.cnt[ek])
        self._mark(tok, reads, writes)
        self.n_inst += 1
        return tok

    def dma(self, out, in_, reads=(), writes=(), q="sp"):
        dk = "d%d" % self.dma_rr
        self.dma_rr = (self.dma_rr + 1) % N_DMA_SEMS
        prev = self.cnt[dk]
        e = self.eng[q]
        if prev > 0 and self.seen[q].get(dk, 0) < prev:
            e.wait_ge(self.sems[dk], prev)
            self.seen[q][dk] = prev
        self._deps(q, reads, writes)
        ins = e.dma_start(out=out, in_=in_)
        self.cnt[dk] += 16
        ins.then_inc(self.sems[dk], 16)
        tok = (dk, self.cnt[dk])
        self._mark(tok, reads, writes)
        self.n_inst += 1
        return tok

    def wait_tok(self, ek, tok):
        sk, v = tok
        if self.seen[ek].get(sk, 0) < v:
            self.eng[ek].wait_ge(self.sems[sk], v)
            self.seen[ek][sk] = v

    def barrier(self):
        for ek in self.eng:
            for sk, v in self.cnt.items():
                if v > 0 and sk != ek:
                    self.wait_tok(ek, (sk, v))

    def finish(self):
        for sk, v in self.cnt.items():
            if v > 0 and sk != "sp":
                self.wait_tok("sp", (sk, v))
        self.es.close()


def bc(ap, shape):
    return ap.to_broadcast(list(shape))


class K:
    def __init__(self, nc):
        self.nc = nc
        self.c = Ctx(nc)
        c = self.c
        self.bank = []
        self.bankb = []
        for i in range(8):
            t, b = c.ps("bank%d" % i, [128, 512], F32)
            self.bank.append(t)
            self.bankb.append(b)
        self.ident, self.ident_b = c.sb("ident_sb", [128, 128], BF16)

    def bank_bf16(self, i):
        return self.bank[i][:].bitcast(BF16)

    def load_ident(self, ident_dram):
        self.c.dma(self.ident[:], ident_dram, writes=[self.ident_b], q="pool")


def emit_rmsnorm_T(kk, h_ap, h_b, gvec, gvec_b, xT, xT_b, kblk, tmp_pool, tbank):
    c = kk.c
    junk, junk_b, ss, ss_b, xn, xn_b = tmp_pool
    c.op("act", lambda e: e.activation(out=junk[:], in_=h_ap, func=AF.Square, accum_out=ss[:]),
         reads=[h_b], writes=[junk_b, ss_b])
    c.op("act", lambda e: e.activation(out=ss[:], in_=ss[:], func=AF.Ln, scale=1.0 / D, bias=EPS),
         reads=[ss_b], writes=[ss_b])
    c.op("act", lambda e: e.activation(out=ss[:], in_=ss[:], func=AF.Exp, scale=-0.5),
         reads=[ss_b], writes=[ss_b])
    c.op("dve", lambda e: e.scalar_tensor_tensor(out=xn[:], in0=h_ap, scalar=ss[:, 0:1], in1=gvec[:],
                                                 op0=ALU.mult, op1=ALU.mult),
         reads=[h_b, ss_b, gvec_b], writes=[xn_b])
    for grp in range(4):
        bi = tbank[grp % 2]
        pt = kk.bank_bf16(bi)
        for j in range(4):
            dc = grp * 4 + j
            c.op("pe", lambda e: e.transpose(out=pt[:, j * 128:(j + 1) * 128],
                                             in_=xn[:, dc * 128:(dc + 1) * 128], identity=kk.ident[:]),
                 reads=[xn_b, kk.ident_b], writes=[kk.bankb[bi]])
        eng = "dve" if grp % 2 == 0 else "act"
        src = pt[:, 0:512].rearrange("p (j t) -> p j t", j=4)
        dst = xT[:, grp * 4:(grp + 1) * 4, kblk * 128:(kblk + 1) * 128]
        if eng == "dve":
            c.op("dve", lambda e: e.tensor_copy(out=dst, in_=src), reads=[kk.bankb[bi]], writes=[xT_b])
        else:
            c.op("act", lambda e: e.copy(out=dst, in_=src), reads=[kk.bankb[bi]], writes=[xT_b])


def emit_sin(c, dst, dst_b, ang, ang_b, shift, ri, ri_b, rf, rf_b, rx, rx_b):
    c.op("dve", lambda e: e.tensor_scalar_add(out=rx[:], in0=ang[:], scalar1=shift), reads=[ang_b], writes=[rx_b])
    c.op("dve", lambda e: e.tensor_scalar_mul(out=rf[:], in0=rx[:], scalar1=1.0 / (2 * PI)), reads=[rx_b], writes=[rf_b])
    c.op("dve", lambda e: e.tensor_copy(out=ri[:], in_=rf[:]), reads=[rf_b], writes=[ri_b])
    c.op("dve", lambda e: e.tensor_copy(out=rf[:], in_=ri[:]), reads=[ri_b], writes=[rf_b])
    c.op("dve", lambda e: e.scalar_tensor_tensor(out=rx[:], in0=rf[:], scalar=-2 * PI, in1=rx[:],
                                                 op0=ALU.mult, op1=ALU.add), reads=[rf_b, rx_b], writes=[rx_b])
    c.op("dve", lambda e: e.tensor_single_scalar(out=rf[:], in_=rx[:], scalar=PI, op=ALU.is_gt), reads=[rx_b], writes=[rf_b])
    c.op("dve", lambda e: e.scalar_tensor_tensor(out=rx[:], in0=rf[:], scalar=-2 * PI, in1=rx[:],
                                                 op0=ALU.mult, op1=ALU.add), reads=[rf_b, rx_b], writes=[rx_b])
    c.op("dve", lambda e: e.tensor_scalar(out=rx[:], in0=rx[:], scalar1=-3.141592, scalar2=3.141592,
                                          op0=ALU.max, op1=ALU.min), reads=[rx_b], writes=[rx_b])
    c.op("act", lambda e: e.activation(out=dst[:], in_=rx[:], func=AF.Sin), reads=[rx_b], writes=[dst_b])

C_QN, C_KC, C_VC, C_KS, C_VS, C_KW, C_VW, C_G, C_SQ, C_SK, C_SV = (
    0, 1024, 1280, 1536, 1792, 2048, 2304, 2560, 2584, 3608, 4632)
PA_SLABS = [
    ("qn", 0, 512), ("qn", 512, 512), ("kcvc", 1024, 512), ("ksvs", 1536, 512),
    ("kwvw", 2048, 512), ("g", 2560, 24), ("sq", 2584, 512), ("sq", 3096, 512),
    ("sk", 3608, 512), ("sk", 4120, 512), ("sv", 4632, 512), ("sv", 5144, 512),
]


def emit_phase_a(kk, h_src, w_in, nmix, bg, pos, invf, QT, KT, V, gates_out, h_resident=None):
    c = kk.c
    nc = kk.nc
    es = ExitStack()
    xT, xT_b = c.sb("a_xT", [128, 16, TL], BF16, es)
    gvec, gvec_b = c.sb("a_gvec", [128, D], F32, es)
    junk, junk_b = c.sb("a_junk", [128, D], F32, es)
    ss, ss_b = c.sb("a_ss", [128, 1], F32, es)
    xn, xn_b = c.sb("a_xn", [128, D], BF16, es)
    hblk = [c.sb("a_h%d" % i, [128, D], F32, es) for i in range(2)]
    wsl = [c.sb("a_w%d" % i, [128, 16, 512], BF16, es) for i in range(2)]
    ev = [c.sb("a_ev%d" % i, [128, 4, 128], BF16, es) for i in range(2)]
    tst = [c.sb("a_ts%d" % i, [128, 4, TL], BF16, es) for i in range(2)]
    vst = [c.sb("a_vs%d" % i, [128, 4, NK, DV], BF16, es) for i in range(2)]
    gsb, gsb_b = c.sb("a_gates", [128, NK, 24], F32, es)
    bgb, bgb_b = c.sb("a_bg", [128, 24], F32, es)
    posi, posi_b = c.sb("a_posi", [128, NK], I32, es)
    posf, posf_b = c.sb("a_posf", [128, NK], F32, es)
    invb, invb_b = c.sb("a_invf", [128, 16], F32, es)
    ang, ang_b = c.sb("a_ang", [128, NK, 16], F32, es)
    cs, cs_b = c.sb("a_cs", [128, NK, 16], F32, es)
    sn, sn_b = c.sb("a_sn", [128, NK, 16], F32, es)
    csq, csq_b = c.sb("a_csq", [128, NK, 16], F32, es)
    snq, snq_b = c.sb("a_snq", [128, NK, 16], F32, es)
    rt = [c.sb("a_rt%d" % i, [128, 4, 16], F32, es) for i in range(4)]
    gtmp, gtmp_b = c.sb("a_gtmp", [128, 24], F32, es)

    c.dma(gvec[:], nmix.broadcast_to([128, D]), writes=[gvec_b])
    c.dma(bgb[:], bg.broadcast_to([128, 24]), writes=[bgb_b])
    c.dma(invb[:], invf.broadcast_to([128, 16]), writes=[invb_b])
    c.dma(posi[:], pos, writes=[posi_b])
    for i in range(2):
        c.op("pool", lambda e: e.memset(vst[i][0][:, :, :, 128:129], 1.0), writes=[vst[i][1]])
        c.op("pool", lambda e: e.memset(vst[i][0][:, :, :, 129:130], 0.0), writes=[vst[i][1]])

    c.op("dve", lambda e: e.tensor_copy(out=posf[:], in_=posi[:]), reads=[posi_b], writes=[posf_b])
    c.op("dve", lambda e: e.tensor_tensor(out=ang[:], in0=bc(posf[:].unsqueeze(2), [128, NK, 16]),
                                          in1=bc(invb[:].unsqueeze(1), [128, NK, 16]), op=ALU.mult),
         reads=[posf_b, invb_b], writes=[ang_b])
    rr_i, rr_ib = c.sb("a_rri", [128, NK, 16], I32, es)
    rr_f, rr_fb = c.sb("a_rrf", [128, NK, 16], F32, es)
    rr_x, rr_xb = c.sb("a_rrx", [128, NK, 16], F32, es)
    for (dst, dst_b, shift) in ((sn, sn_b, 0.0), (cs, cs_b, 0.5 * PI)):
        emit_sin(c, dst, dst_b, ang, ang_b, shift, rr_i, rr_ib, rr_f, rr_fb, rr_x, rr_xb)
    c.op("dve", lambda e: e.tensor_scalar_mul(out=csq[:], in0=cs[:], scalar1=SCALE), reads=[cs_b], writes=[csq_b])
    c.op("dve", lambda e: e.tensor_scalar_mul(out=snq[:], in0=sn[:], scalar1=SCALE), reads=[sn_b], writes=[snq_b])

    tmp_pool = (junk, junk_b, ss, ss_b, xn, xn_b)
    for k in range(NK):
        if h_resident is None:
            ht, ht_b = hblk[k % 2]
            c.dma(ht[:], h_src[k * 128:(k + 1) * 128, :], writes=[ht_b])
            h_ap = ht[:]
        else:
            ht_b = h_resident[1]
            h_ap = h_resident[0][:, k, :]
        emit_rmsnorm_T(kk, h_ap, ht_b, gvec, gvec_b, xT, xT_b, k, tmp_pool, (6, 7))

    def rope(pt, dst, dst_b, h0, nh, cosb, sinb, tabs_b, k, pb):
        co = bc(cosb[:, k, :].unsqueeze(1), [128, nh, 16])
        si = bc(sinb[:, k, :].unsqueeze(1), [128, nh, 16])
        x1 = pt[:, h0:h0 + nh, 0:16]
        x2 = pt[:, h0:h0 + nh, 16:32]
        t = [r[0][:, 0:nh, :] for r in rt]
        tb = [r[1] for r in rt]
        c.op("dve", lambda e: e.tensor_tensor(out=t[0], in0=x1, in1=co, op=ALU.mult), reads=[pb] + tabs_b, writes=[tb[0]])
        c.op("dve", lambda e: e.tensor_tensor(out=t[1], in0=x2, in1=si, op=ALU.mult), reads=[pb] + tabs_b, writes=[tb[1]])
        c.op("dve", lambda e: e.tensor_tensor(out=t[2], in0=x2, in1=co, op=ALU.mult), reads=[pb] + tabs_b, writes=[tb[2]])
        c.op("dve", lambda e: e.tensor_tensor(out=t[3], in0=x1, in1=si, op=ALU.mult), reads=[pb] + tabs_b, writes=[tb[3]])
        c.op("dve", lambda e: e.tensor_tensor(out=dst[:, h0:h0 + nh, 0:16], in0=t[0], in1=t[1], op=ALU.subtract),
             reads=[tb[0], tb[1]], writes=[dst_b])
        c.op("dve", lambda e: e.tensor_tensor(out=dst[:, h0:h0 + nh, 16:32], in0=t[2], in1=t[3], op=ALU.add),
             reads=[tb[2], tb[3]], writes=[dst_b])

    n_t = 0
    n_v = 0
    for si, (typ, c0, ncol) in enumerate(PA_SLABS):
        wt, wt_b = wsl[si % 2]
        c.dma(wt[:, :, 0:ncol], w_in[:, c0:c0 + ncol].rearrange("(dc p) n -> p dc n", p=128),
              writes=[wt_b], q="pool")
        uses_t = typ in ("qn", "kcvc", "ksvs", "kwvw", "sq", "sk")
        uses_v = typ in ("ksvs", "kwvw", "sv")
        if uses_t:
            ts, ts_b = tst[n_t % 2]
            n_t += 1
        if uses_v:
            vs, vs_b = vst[n_v % 2]
            n_v += 1
        for k in range(NK):
            bi = k % 2
            pb = kk.bankb[bi]
            pfull = kk.bank[bi]
            for dc in range(16):
                c.op("pe", lambda e: e.matmul(pfull[:, 0:ncol], lhsT=xT[:, dc, k * 128:(k + 1) * 128],
                                              rhs=wt[:, dc, 0:ncol], start=(dc == 0), stop=(dc == 15)),
                     reads=[xT_b, wt_b], writes=[pb])
            pt = pfull[:, :].rearrange("p (h e) -> p h e", h=4)
            evt, evt_b = ev[k % 2]
            nT = 0
            if typ == "qn":
                c.op("act", lambda e: e.activation(out=evt[:, :, 32:128], in_=pt[:, :, 32:128], func=AF.Copy, scale=SCALE),
                     reads=[pb], writes=[evt_b])
                rope(pt, evt, evt_b, 0, 4, csq, snq, [csq_b, snq_b], k, pb)
                nT = 4
            elif typ == "sq":
                c.op("act", lambda e: e.activation(out=evt[:], in_=pt, func=AF.Copy, scale=SCALE),
                     reads=[pb], writes=[evt_b])
                nT = 4
            elif typ in ("kcvc", "sk"):
                c.op("act", lambda e: e.copy(out=evt[:], in_=pt), reads=[pb], writes=[evt_b])
                nT = 4
            elif typ in ("ksvs", "kwvw"):
                c.op("act", lambda e: e.copy(out=evt[:, 0:2, 32:128], in_=pt[:, 0:2, 32:128]), reads=[pb], writes=[evt_b])
                rope(pt, evt, evt_b, 0, 2, cs, sn, [cs_b, sn_b], k, pb)
                c.op("act", lambda e: e.copy(out=vs[:, 0:2, k, 0:128], in_=pt[:, 2:4, :]), reads=[pb], writes=[vs_b])
                nT = 2
            elif typ == "sv":
                c.op("act", lambda e: e.copy(out=vs[:, 0:4, k, 0:128], in_=pt), reads=[pb], writes=[vs_b])
            elif typ == "g":
                c.op("dve", lambda e: e.tensor_tensor(out=gtmp[:], in0=pfull[:, 0:24], in1=bgb[:], op=ALU.add),
                     reads=[pb, bgb_b], writes=[gtmp_b])
                c.op("act", lambda e: e.activation(out=gsb[:, k, :], in_=gtmp[:], func=AF.Sigmoid),
                     reads=[gtmp_b], writes=[gsb_b])
            if nT:
                tbi = 6 + (k % 2)
                ptb = kk.bank_bf16(tbi)
                for j in range(nT):
                    c.op("pe", lambda e: e.transpose(out=ptb[:, j * 128:(j + 1) * 128], in_=evt[:, j, :],
                                                     identity=kk.ident[:]),
                         reads=[evt_b, kk.ident_b], writes=[kk.bankb[tbi]])
                c.op("dve", lambda e: e.tensor_copy(
                    out=ts[:, 0:nT, k * 128:(k + 1) * 128],
                    in_=ptb[:, 0:nT * 128].rearrange("p (j t) -> p j t", j=nT)),
                    reads=[kk.bankb[tbi]], writes=[ts_b])
        if typ == "qn":
            h0 = c0 // 128
            c.dma(QT[h0:h0 + 4].rearrange("h p t -> p h t"), ts[:], reads=[ts_b])
        elif typ == "sq":
            h0 = 8 + (c0 - C_SQ) // 128
            c.dma(QT[h0:h0 + 4].rearrange("h p t -> p h t"), ts[:], reads=[ts_b])
        elif typ == "kcvc":
            c.dma(KT[0:4].rearrange("h p t -> p h t"), ts[:], reads=[ts_b])
        elif typ == "ksvs":
            c.dma(KT[4:6].rearrange("h p t -> p h t"), ts[:, 0:2, :], reads=[ts_b])
            c.dma(V[0:2].rearrange("h p k e -> p h k e"), vs[:, 0:2], reads=[vs_b])
        elif typ == "kwvw":
            c.dma(KT[6:8].rearrange("h p t -> p h t"), ts[:, 0:2, :], reads=[ts_b])
            c.dma(V[2:4].rearrange("h p k e -> p h k e"), vs[:, 0:2], reads=[vs_b])
        elif typ == "sk":
            h0 = 8 + (c0 - C_SK) // 128
            c.dma(KT[h0:h0 + 4].rearrange("h p t -> p h t"), ts[:], reads=[ts_b])
        elif typ == "sv":
            h0 = 4 + (c0 - C_SV) // 128
            c.dma(V[h0:h0 + 4].rearrange("h p k e -> p h k e"), vs[:], reads=[vs_b])
        elif typ == "g":
            c.dma(gates_out, gsb[:], reads=[gsb_b])
    c.barrier()
    es.close()


def build_pa():
    nc = bass.Bass("TRN2", target_bir_lowering=False)

    def din(name, shape, dt=F32):
        return nc.dram_tensor(name, list(shape), dt, kind="ExternalInput").ap()

    def dout(name, shape, dt=F32):
        return nc.dram_tensor(name, list(shape), dt, kind="ExternalOutput").ap()

    h = din("h", [TL, D])
    w_in = din("w_in", [D, D_IN])
    nmix = din("nmix", [1, D])
    bg = din("bg", [1, 24])
    pos = din("pos", [128, NK], I32)
    invf = din("invf", [1, 16])
    ident = din("ident", [128, 128])
    QT = dout("QT", [16, 128, TL], BF16)
    KT = dout("KT", [16, 128, TL], BF16)
    V = dout("V", [12, 128, NK, DV], BF16)
    gates = dout("gates", [128, NK, 24])
    kk = K(nc)
    kk.load_ident(ident)
    emit_phase_a(kk, h, w_in, nmix, bg, pos, invf, QT, KT, V, gates)
    kk.c.finish()
    return nc


def _inv_freq():
    return (500000.0 ** (-np.arange(0, 32, 2, dtype=np.float32) / 32)).astype(np.float32)[None, :]


def shard_tokens(a):
    blk = a.reshape(64, 128, *a.shape[1:])
    return [np.ascontiguousarray(blk[c::8].reshape(TL, *a.shape[1:])) for c in range(NCORES)]


def unshard_tokens(parts):
    out = np.empty((64, 128) + parts[0].shape[1:], parts[0].dtype)
    for c in range(NCORES):
        out[c::8] = parts[c].reshape(8, 128, *parts[c].shape[1:])
    return out.reshape(S, *parts[0].shape[1:])


_PROG = {}


def run_pa(h_parts, pos_parts, w_in, nmix, bg):
    if "pa" not in _PROG:
        _PROG["pa"] = build_pa()
    nc = _PROG["pa"]
    ident = np.eye(128, dtype=np.float32)
    invf = _inv_freq()
    in_maps = []
    for c in range(NCORES):
        in_maps.append({
            "h": h_parts[c], "w_in": w_in, "nmix": nmix[None, :], "bg": bg[None, :],
            "pos": np.ascontiguousarray(pos_parts[c].reshape(NK, 128).T), "invf": invf, "ident": ident,
        })
    res = run_bass_kernel_spmd(nc, in_maps, core_ids=list(range(NCORES)))
    return res.results


DEBUG = False
GELU_C0 = 0.7978845608028654
GELU_C1 = 0.044715


def emit_phase_b(kk, P):
    c = kk.c
    nc = kk.nc
    bank, bankb = kk.bank, kk.bankb
    ident, ident_b = kk.ident, kk.ident_b
    es_all = ExitStack()
    es_att = ExitStack()

    def mm(out, lhsT, rhs, start, stop, reads, wb):
        c.op("pe", lambda e: e.matmul(out, lhsT=lhsT, rhs=rhs, start=start, stop=stop, skip_group_check=True),
             reads=reads, writes=[wb])

    onT, onT_b = c.sb("b_onT", [128, 16, TL], BF16, es_all)
    KTr = [c.sb("b_KT%d" % i, [128, 8, TL], BF16, es_att) for i in range(3)]
    Vr = [c.sb("b_V%d" % i, [128, 8, NK, DV], BF16, es_att) for i in range(3)]
    Gm, Gm_b = c.sb("b_G", [128, 64 * 128], BF16, es_att)
    mctn, mctn_b = c.sb("b_mctn", [128, 960], BF16, es_att)
    mcnt, mcnt_b = c.sb("b_mcnt", [128, 3, 128], BF16, es_att)
    Arel, Arel_b = c.sb("b_Arel", [128, 240], F32, es_att)
    Brel, Brel_b = c.sb("b_Brel", [128, 240], F32, es_att)
    dz, dz_b = c.sb("b_dz", [128, 8, 128], BF16, es_att)
    wz, wz_b = c.sb("b_wz", [128, 12, 128], BF16, es_att)
    vzr, vzr_b = c.sb("b_vzr", [128, 8, 128], BF16, es_att)
    ntri, ntri_b = c.sb("b_ntri", [128, 128], BF16, es_att)
    nones, nones_b = c.sb("b_nones", [128, 128], BF16, es_att)
    gates, gates_b = c.sb("b_gates", [128, NK, 24], F32, es_att)
    ggT, ggT_b = c.sb("b_ggT", [128, 16], F32, es_att)
    kcT = [c.sb("b_kcT%d" % g, [128, 512], BF16, es_att) for g in range(2)]
    vcs = [c.sb("b_vc%d" % g, [128, 4, DV], BF16, es_att) for g in range(2)]
    qsb = [c.sb("b_q%d" % i, [128, 4, TL], BF16, es_att) for i in range(1)]
    ef = [c.sb("b_ef%d" % i, [128, 512], F32, es_att) for i in range(2)]
    pb16 = [c.sb("b_p%d" % i, [128, 4, 128], BF16, es_att) for i in range(2)]
    lb16 = [c.sb("b_l%d" % i, [128, 4, 128], BF16, es_att) for i in range(2)]
    padbuf, padbuf_b = c.sb("b_pad", [128, 516], F32, es_att)
    qi, qi_b = c.sb("b_qi", [128, 512], F32, es_att)
    pslc, pslc_b = c.sb("b_pslc", [128, 128], F32, es_att)
    imp, imp_b = c.sb("b_imp", [128, 128], F32, es_att)
    imp2, imp2_b = c.sb("b_imp2", [128, 128], F32, es_att)
    sel2, sel2_b = c.sb("b_sel2", [128, 128], F32, es_att)
    selb, selb_b = c.sb("b_selb", [128, 128], BF16, es_att)
    selT, selT_b = c.sb("b_selT", [128, 128], BF16, es_att)
    m8a, m8a_b = c.sb("b_m8a", [128, 8], F32, es_att)
    m8b, m8b_b = c.sb("b_m8b", [128, 8], F32, es_att)
    sm, sm_b = c.sb("b_sm", [128, 16], F32, es_att)
    oacc, oacc_b = c.sb("b_oacc", [128, 4, 128], F32, es_att)
    onb, onb_b = c.sb("b_onb", [128, 4, 128], BF16, es_att)
    sqj, sqj_b = c.sb("b_sqj", [128, 128], F32, es_att)
    r32, r32_b = c.sb("b_r32", [128, 128], F32, es_att)
    rsum, rsum_b = c.sb("b_rsum", [128, 128], F32, es_att)
    rb, rb_b = c.sb("b_rb", [128, 128], BF16, es_att)
    qs1 = [c.sb("b_qs%d" % i, [128, TL], BF16, es_att) for i in range(2)]

    c.dma(Gm[:], P["Gm"], writes=[Gm_b], q="pool")
    c.dma(mctn[:], P["mctn"], writes=[mctn_b], q="pool")
    c.dma(mcnt[:], P["mcnt"], writes=[mcnt_b], q="pool")
    c.dma(Arel[:], P["Arel"], writes=[Arel_b])
    c.dma(Brel[:], P["Brel"], writes=[Brel_b])
    c.dma(dz[:], P["dz"], writes=[dz_b], q="pool")
    c.dma(wz[:], P["wz"], writes=[wz_b], q="pool")
    c.dma(vzr[:], P["vzr"], writes=[vzr_b], q="pool")
    c.dma(ntri[:], P["ntri"], writes=[ntri_b], q="pool")
    c.dma(nones[:], P["nones"], writes=[nones_b], q="pool")
    c.dma(gates[:], P["gates"], writes=[gates_b])
    with nc.allow_non_contiguous_dma(reason="tiny one-off per-head scale table"):
        c.dma(ggT[:], P["ngrp"].rearrange("o (h d) -> d (o h)", d=128), writes=[ggT_b])
    c.op("dve", lambda e: e.memset(padbuf[:], 0.0), writes=[padbuf_b])

    es_c = ExitStack()
    w1 = [(KTr[1 + i][0][:].rearrange("p c t -> p (c t)").rearrange("p (l h) -> p l h", h=256), KTr[1 + i][1]) for i in range(2)]
    w2 = [c.sb("c_w2%d" % i, [128, 2, 128], BF16, es_c) for i in range(2)]
    cpos, cpos_b = c.sb("c_cpos", [32, 128], BF16, es_c)
    cposT, cposT_b = c.sb("c_cposT", [128, 32], BF16, es_c)
    b1, b1_b = c.sb("c_b1", [128, 4], F32, es_c)
    xs, xs_b = c.sb("c_xs", [128, 512], F32, es_c)
    uu, uu_b = c.sb("c_uu", [128, 512], F32, es_c)
    gT = [c.sb("c_gT%d" % i, [128, 512], BF16, es_c) for i in range(2)]
    ktok, ktok_b = c.sb("c_ktok", [128, 128], BF16, es_c)
    pci, pci_b = c.sb("c_pci", [128, 4], I32, es_c)
    pcf, pcf_b = c.sb("c_pcf", [128, 4], F32, es_c)
    invb, invb_b = c.sb("c_invf", [128, 16], F32, es_c)
    cang, cang_b = c.sb("c_ang", [128, 4, 16], F32, es_c)
    ccs, ccs_b = c.sb("c_cs", [128, 4, 16], F32, es_c)
    csn, csn_b = c.sb("c_sn", [128, 4, 16], F32, es_c)
    rri, rri_b = c.sb("c_rri", [128, 4, 16], I32, es_c)
    rrf, rrf_b = c.sb("c_rrf", [128, 4, 16], F32, es_c)
    rrx, rrx_b = c.sb("c_rrx", [128, 4, 16], F32, es_c)
    crt = [c.sb("c_rt%d" % i, [128, 16], F32, es_c) for i in range(4)]

    c.dma(w1[0][0], P["wck1"].rearrange("(l d) h -> d l h", d=128), writes=[w1[0][1]], q="pool")
    c.dma(w1[1][0], P["wcv1"].rearrange("(l d) h -> d l h", d=128), writes=[w1[1][1]], q="pool")
    c.dma(w2[0][0][:], P["wck2"].rearrange("(c p) d -> p c d", p=128), writes=[w2[0][1]], q="pool")
    c.dma(w2[1][0][:], P["wcv2"].rearrange("(c p) d -> p c d", p=128), writes=[w2[1][1]], q="pool")
    c.dma(cpos[:], P["cmp_pos"], writes=[cpos_b], q="pool")
    c.dma(pci[:], P["poscmp"], writes=[pci_b])
    c.dma(invb[:], P["invf"].broadcast_to([128, 16]), writes=[invb_b])
    c.op("dve", lambda e: e.tensor_copy(out=pcf[:], in_=pci[:]), reads=[pci_b], writes=[pcf_b])
    c.op("dve", lambda e: e.tensor_tensor(out=cang[:], in0=bc(pcf[:].unsqueeze(2), [128, 4, 16]),
                                          in1=bc(invb[:].unsqueeze(1), [128, 4, 16]), op=ALU.mult),
         reads=[pcf_b, invb_b], writes=[cang_b])
    emit_sin(c, csn, csn_b, cang, cang_b, 0.0, rri, rri_b, rrf, rrf_b, rrx, rrx_b)
    emit_sin(c, ccs, ccs_b, cang, cang_b, 0.5 * PI, rri, rri_b, rrf, rrf_b, rrx, rrx_b)
    ptb = kk.bank_bf16(7)
    c.op("pe", lambda e: e.transpose(out=ptb[:, 0:32], in_=cpos[:], identity=ident[0:32, 0:32]),
         reads=[cpos_b, ident_b], writes=[bankb[7]])
    c.op("dve", lambda e: e.tensor_copy(out=cposT[:], in_=ptb[:, 0:32]), reads=[bankb[7]], writes=[cposT_b])
    for X in range(2):
        for hc in range(2):
            for l in range(32):
                mm(bank[6][:, X * 2 + hc:X * 2 + hc + 1], w1[X][0][:, l, hc * 128:(hc + 1) * 128], cposT[:, l:l + 1],
                   (l == 0 and X == 0 and hc == 0), (l == 31), [w1[X][1], cposT_b], bankb[6])
    c.op("dve", lambda e: e.tensor_copy(out=b1[:], in_=bank[6][:, 0:4]), reads=[bankb[6]], writes=[b1_b])
    c.op("dve", lambda e: e.memset(gT[0][0][:], 0.0), writes=[gT[0][1]])
    c.op("dve", lambda e: e.memset(gT[1][0][:], 0.0), writes=[gT[1][1]])
    for g in range(2):
        c.op("pool", lambda e: e.memset(vcs[g][0][:, :, 128:129], 1.0), writes=[vcs[g][1]])
        c.op("pool", lambda e: e.memset(vcs[g][0][:, :, 129:130], 0.0), writes=[vcs[g][1]])

    kcg, kcg_b = KTr[0]
    kcg_flat = kcg[:].rearrange("p c t -> p (c t)")
    kcg_v = kcg[:].rearrange("p k (c t) -> p k c t", c=8)
    for X in range(2):
        for g in range(2):
            hh = 2 * X + g
            for cc in range(8):
                c.dma(kcg_v[:, :, cc, :], P["KTg"][cc, hh].rearrange("d (k t) -> d k t", k=8), writes=[kcg_b])
            for hc in range(2):
                bi = hc
                for l in range(32):
                    mm(bank[bi][:, 0:511], w1[X][0][:, l, hc * 128:(hc + 1) * 128],
                       kcg_flat[:, l:l + 16 * 510 + 1:16], (l == 0), (l == 31), [w1[X][1], kcg_b], bankb[bi])
                c.op("act", lambda e: e.activation(out=xs[:, 0:511], in_=bank[bi][:, 0:511], func=AF.Identity,
                                                   bias=b1[:, X * 2 + hc:X * 2 + hc + 1]),
                     reads=[bankb[bi], b1_b], writes=[xs_b])
                c.op("dve", lambda e: e.tensor_tensor(out=uu[:, 0:511], in0=xs[:, 0:511], in1=xs[:, 0:511], op=ALU.mult),
                     reads=[xs_b], writes=[uu_b])
                c.op("dve", lambda e: e.tensor_scalar(out=uu[:, 0:511], in0=uu[:, 0:511], scalar1=GELU_C1, scalar2=1.0,
                                                      op0=ALU.mult, op1=ALU.add), reads=[uu_b], writes=[uu_b])
                c.op("dve", lambda e: e.tensor_tensor(out=uu[:, 0:511], in0=uu[:, 0:511], in1=xs[:, 0:511], op=ALU.mult),
                     reads=[uu_b, xs_b], writes=[uu_b])
                c.op("act", lambda e: e.activation(out=uu[:, 0:511], in_=uu[:, 0:511], func=AF.Sigmoid, scale=2 * GELU_C0),
                     reads=[uu_b], writes=[uu_b])
                c.op("dve", lambda e: e.tensor_tensor(out=gT[hc][0][:, 0:511], in0=uu[:, 0:511], in1=xs[:, 0:511], op=ALU.mult),
                     reads=[uu_b, xs_b], writes=[gT[hc][1]])
            for nch in range(4):
                bi = 2 + nch % 2
                for hc in range(2):
                    mm(bank[bi][:, 0:128], gT[hc][0][:, nch * 128:(nch + 1) * 128], w2[X][0][:, hc, :],
                       (hc == 0), (hc == 1), [gT[hc][1], w2[X][1]], bankb[bi])
                if X == 1:
                    c.op("act", lambda e: e.copy(out=vcs[g][0][:, nch, 0:128], in_=bank[bi][:, 0:128]),
                         reads=[bankb[bi]], writes=[vcs[g][1]])
                else:
                    pt = bank[bi]
                    co = ccs[:, nch, :]
                    si = csn[:, nch, :]
                    t = [r[0][:] for r in crt]
                    tb = [r[1] for r in crt]
                    c.op("act", lambda e: e.copy(out=ktok[:, 32:128], in_=pt[:, 32:128]), reads=[bankb[bi]], writes=[ktok_b])
                    c.op("dve", lambda e: e.tensor_tensor(out=t[0], in0=pt[:, 0:16], in1=co, op=ALU.mult), reads=[bankb[bi], ccs_b], writes=[tb[0]])
                    c.op("dve", lambda e: e.tensor_tensor(out=t[1], in0=pt[:, 16:32], in1=si, op=ALU.mult), reads=[bankb[bi], csn_b], writes=[tb[1]])
                    c.op("dve", lambda e: e.tensor_tensor(out=t[2], in0=pt[:, 16:32], in1=co, op=ALU.mult), reads=[bankb[bi], ccs_b], writes=[tb[2]])
                    c.op("dve", lambda e: e.tensor_tensor(out=t[3], in0=pt[:, 0:16], in1=si, op=ALU.mult), reads=[bankb[bi], csn_b], writes=[tb[3]])
                    c.op("dve", lambda e: e.tensor_tensor(out=ktok[:, 0:16], in0=t[0], in1=t[1], op=ALU.subtract), reads=[tb[0], tb[1]], writes=[ktok_b])
                    c.op("dve", lambda e: e.tensor_tensor(out=ktok[:, 16:32], in0=t[2], in1=t[3], op=ALU.add), reads=[tb[2], tb[3]], writes=[ktok_b])
                    tbi = 6 + nch % 2
                    ptt = kk.bank_bf16(tbi)
                    c.op("pe", lambda e: e.transpose(out=ptt[:, 0:128], in_=ktok[:], identity=ident[:]),
                         reads=[ktok_b, ident_b], writes=[bankb[tbi]])
                    c.op("dve", lambda e: e.tensor_copy(out=kcT[g][0][:, nch * 128:(nch + 1) * 128], in_=ptt[:, 0:128]),
                         reads=[bankb[tbi]], writes=[kcT[g][1]])
    c.barrier()
    es_c.close()

    state = {"s": 0, "o": 0, "e": 0, "p": 0}

    def next_s():
        state["s"] ^= 1
        return state["s"]

    def next_o():
        state["o"] ^= 1
        return (2, 3) if state["o"] else (4, 5)

    def gate_ap(k, branch, h8):
        return gates[:, k, branch * 8 + h8:branch * 8 + h8 + 1]

    def evac_nsa(obanks, k, g, branch, first):
        for h in range(4):
            ob = obanks[h // 2]
            ov = bank[ob][:, 0:2 * DV].rearrange("p (h e) -> p h e", h=2)
            den = sm[:, h:h + 1]
            c.op("dve", lambda e: e.tensor_scalar_max(out=den, in0=ov[:, h % 2, 128:129], scalar1=1e-30),
                 reads=[bankb[ob]], writes=[sm_b])
            c.op("dve", lambda e: e.reciprocal(out=den, in_=den), reads=[sm_b], writes=[sm_b])
            c.op("dve", lambda e: e.tensor_tensor(out=den, in0=den, in1=gate_ap(k, branch, 4 * g + h), op=ALU.mult),
                 reads=[sm_b, gates_b], writes=[sm_b])
            if first:
                c.op("dve", lambda e: e.tensor_scalar_mul(out=oacc[:, h, :], in0=ov[:, h % 2, 0:128], scalar1=den),
                     reads=[bankb[ob], sm_b], writes=[oacc_b])
            else:
                c.op("dve", lambda e: e.scalar_tensor_tensor(out=oacc[:, h, :], in0=ov[:, h % 2, 0:128], scalar=den,
                                                             in1=oacc[:, h, :], op0=ALU.mult, op1=ALU.add),
                     reads=[bankb[ob], sm_b, oacc_b], writes=[oacc_b])

    def head_norm_T(src_ap_fn, src_b, nh, hh0, k):
        for h in range(nh):
            c.op("act", lambda e: e.activation(out=sqj[:], in_=src_ap_fn(h), func=AF.Square, accum_out=sm[:, 8 + h:9 + h]),
                 reads=[src_b], writes=[sqj_b, sm_b])
        c.op("act", lambda e: e.activation(out=sm[:, 8:8 + nh], in_=sm[:, 8:8 + nh], func=AF.Ln, scale=1.0 / HD, bias=EPS),
             reads=[sm_b], writes=[sm_b])
        c.op("act", lambda e: e.activation(out=sm[:, 8:8 + nh], in_=sm[:, 8:8 + nh], func=AF.Exp, scale=-0.5),
             reads=[sm_b], writes=[sm_b])
        for h in range(nh):
            c.op("dve", lambda e: e.tensor_scalar_mul(out=onb[:, h, :], in0=src_ap_fn(h), scalar1=sm[:, 8 + h:9 + h]),
                 reads=[src_b, sm_b], writes=[onb_b])
        tbi = 6 + (k % 2)
        ptt = kk.bank_bf16(tbi)
        for h in range(nh):
            c.op("pe", lambda e: e.transpose(out=ptt[:, h * 128:(h + 1) * 128], in_=onb[:, h, :], identity=ident[:]),
                 reads=[onb_b, ident_b], writes=[bankb[tbi]])
        c.op("dve", lambda e: e.tensor_tensor(out=onT[:, hh0:hh0 + nh, k * 128:(k + 1) * 128],
                                              in0=ptt[:, 0:nh * 128].rearrange("p (j t) -> p j t", j=nh),
                                              in1=bc(ggT[:, hh0:hh0 + nh].unsqueeze(2), [128, nh, 128]), op=ALU.mult),
             reads=[bankb[tbi], ggT_b], writes=[onT_b])

    def attn_block(KT_ap, bias_list, V_ap, Qk, obanks, first, reads_kv):
        sb_i = next_s()
        n = len(bias_list)
        mm(bank[sb_i][:], KT_ap, Qk, True, n == 0, reads_kv + [qcur["b"]], bankb[sb_i])
        for bi_, (lhsT, rhs, rb_) in enumerate(bias_list):
            mm(bank[sb_i][:], lhsT, rhs, False, bi_ == n - 1, rb_, bankb[sb_i])
        state["p"] ^= 1
        pt_, pt_b = pb16[state["p"]]
        c.op("act", lambda e: e.activation(out=pt_[:].rearrange("p h t -> p (h t)"), in_=bank[sb_i][:], func=AF.Exp),
             reads=[bankb[sb_i]], writes=[pt_b])
        for h in range(4):
            ob = obanks[h // 2]
            mm(bank[ob][:, (h % 2) * DV:(h % 2 + 1) * DV], pt_[:, h, :], V_ap,
               (first and h % 2 == 0), False, [pt_b] + reads_kv, bankb[ob])

    qcur = {"b": None}

    ring = {"kt": 0, "v": 0}

    def next_kt():
        i = ring["kt"]
        ring["kt"] = (i + 1) % 3
        return KTr[i]

    def next_v():
        i = ring["v"]
        ring["v"] = (i + 1) % 3
        return Vr[i]

    for g in range(2):
        KTs, KTs_b = next_kt()
        KTw, KTw_b = next_kt()
        Vs, Vs_b = next_v()
        Vw, Vw_b = next_v()
        c.dma(KTs[:], P["KTg"][:, 4 + g].rearrange("c d t -> d c t"), writes=[KTs_b])
        c.dma(Vs[:], P["Vg"][:, g].rearrange("c p k e -> p c k e"), writes=[Vs_b])
        c.dma(KTw[:], P["KTg"][:, 6 + g].rearrange("c d t -> d c t"), writes=[KTw_b])
        c.dma(Vw[:], P["Vg"][:, 2 + g].rearrange("c p k e -> p c k e"), writes=[Vw_b])
        qt, qt_b = qsb[0]
        c.dma(qt[:], P["QT"][4 * g:4 * g + 4].rearrange("h d t -> d h t"), writes=[qt_b])
        qcur["b"] = qt_b
        kct, kct_b = kcT[g]
        vct, vct_b = vcs[g]
        for k in range(NK):
            Qk = qt[:, :, k * 128:(k + 1) * 128]
            offc = 448 - 64 * k
            for h in range(4):
                xb = 6 + (h % 2)
                mm(bank[xb][:], qt[:, h, k * 128:(k + 1) * 128], kct[:], True, False, [qt_b, kct_b], bankb[xb])
                mm(bank[xb][:], ident[:], mctn[:, offc:offc + 512], False, True, [ident_b, mctn_b], bankb[xb])
                et, et_b = ef[h % 2]
                c.op("act", lambda e: e.activation(out=et[:], in_=bank[xb][:], func=AF.Exp, accum_out=sm[:, 4 + h:5 + h]),
                     reads=[bankb[xb]], writes=[et_b, sm_b])
                c.op("dve", lambda e: e.tensor_scalar_max(out=sm[:, 4 + h:5 + h], in0=sm[:, 4 + h:5 + h], scalar1=1e-30),
                     reads=[sm_b], writes=[sm_b])
                c.op("dve", lambda e: e.reciprocal(out=sm[:, 4 + h:5 + h], in_=sm[:, 4 + h:5 + h]), reads=[sm_b], writes=[sm_b])
                if h == 0:
                    c.op("dve", lambda e: e.tensor_scalar_mul(out=padbuf[:, 1:513], in0=et[:], scalar1=sm[:, 4:5]),
                         reads=[et_b, sm_b], writes=[padbuf_b])
                else:
                    c.op("dve", lambda e: e.scalar_tensor_tensor(out=padbuf[:, 1:513], in0=et[:], scalar=sm[:, 4 + h:5 + h],
                                                                 in1=padbuf[:, 1:513], op0=ALU.mult, op1=ALU.add),
                         reads=[et_b, sm_b, padbuf_b], writes=[padbuf_b])
            c.op("dve", lambda e: e.tensor_tensor(out=qi[:], in0=padbuf[:, 1:513], in1=padbuf[:, 0:512], op=ALU.add),
                 reads=[padbuf_b], writes=[qi_b])
            c.op("dve", lambda e: e.tensor_reduce(out=pslc[:], in_=qi[:].rearrange("p (j r) -> p j r", r=4),
                                                  axis=AX.X, op=ALU.add), reads=[qi_b], writes=[pslc_b])
            offa = 112 - 16 * k
            c.op("dve", lambda e: e.tensor_tensor(out=imp[:], in0=pslc[:], in1=Arel[:, offa:offa + 128], op=ALU.mult),
                 reads=[pslc_b, Arel_b], writes=[imp_b])
            c.op("dve", lambda e: e.tensor_tensor(out=imp[:], in0=imp[:], in1=Brel[:, offa:offa + 128], op=ALU.add),
                 reads=[imp_b, Brel_b], writes=[imp_b])
            c.op("dve", lambda e: e.memset(imp[:, 0:1], 1e4), writes=[imp_b])
            c.op("dve", lambda e: e.max(out=m8a[:], in_=imp[:]), reads=[imp_b], writes=[m8a_b])
            c.op("dve", lambda e: e.match_replace(out=imp2[:], in_to_replace=m8a[:], in_values=imp[:], imm_value=-1e9),
                 reads=[imp_b, m8a_b], writes=[imp2_b])
            c.op("dve", lambda e: e.max(out=m8b[:], in_=imp2[:]), reads=[imp2_b], writes=[m8b_b])
            c.op("dve", lambda e: e.tensor_single_scalar(out=sel2[:], in_=imp[:], scalar=-5000.0, op=ALU.is_gt),
                 reads=[imp_b], writes=[sel2_b])
            c.op("dve", lambda e: e.scalar_tensor_tensor(out=sel2[:], in0=imp[:], scalar=m8b[:, 7:8], in1=sel2[:],
                                                         op0=ALU.is_ge, op1=ALU.mult),
                 reads=[imp_b, m8b_b, sel2_b], writes=[sel2_b])
            c.op("dve", lambda e: e.tensor_scalar(out=selb[:], in0=sel2[:], scalar1=-1.0, scalar2=-NEG,
                                                  op0=ALU.add, op1=ALU.mult), reads=[sel2_b], writes=[selb_b])
            tbi = 6 + (k % 2)
            ptt = kk.bank_bf16(tbi)
            c.op("pe", lambda e: e.transpose(out=ptt[:, 0:128], in_=selb[:], identity=ident[:]),
                 reads=[selb_b, ident_b], writes=[bankb[tbi]])
            c.op("dve", lambda e: e.tensor_copy(out=selT[:], in_=ptt[:, 0:128]), reads=[bankb[tbi]], writes=[selT_b])
            selT4 = bc(selT[:].unsqueeze(1), [128, 4, 128])

            ob = next_o()
            for nch in range(k // 2 + 1):
                dprime = 16 * nch - 8 * k
                bl = []
                if dprime in (-16, -8, 0):
                    idx = dprime // 8 + 2
                    bl.append((ident[:], bc(mcnt[:, idx, :].unsqueeze(1), [128, 4, 128]), [ident_b, mcnt_b]))
                attn_block(kct[:, nch * 128:(nch + 1) * 128], bl, vct[:, nch, :], Qk, ob, nch == 0, [kct_b, vct_b])
            evac_nsa(ob, k, g, 0, True)

            ob = next_o()
            for kb in range(8 * k + 8):
                ck, kq = kb % 8, kb // 8
                bl = [(Gm[:, kb * 128:(kb + 1) * 128], selT4, [Gm_b, selT_b])]
                if kb >= 8 * k:
                    bl.append((ident[:], bc(dz[:, kb - 8 * k, :].unsqueeze(1), [128, 4, 128]), [ident_b, dz_b]))
                attn_block(KTs[:, ck, kq * 128:(kq + 1) * 128], bl, Vs[:, ck, kq, :], Qk, ob, kb == 0, [KTs_b, Vs_b])
            evac_nsa(ob, k, g, 1, False)

            ob = next_o()
            first = True
            for r in range(12):
                kb = 8 * k - 4 + r
                if kb < 0:
                    continue
                ck, kq = kb % 8, kb // 8
                bl = [(ident[:], bc(wz[:, r, :].unsqueeze(1), [128, 4, 128]), [ident_b, wz_b])]
                attn_block(KTw[:, ck, kq * 128:(kq + 1) * 128], bl, Vw[:, ck, kq, :], Qk, ob, first, [KTw_b, Vw_b])
                first = False
            evac_nsa(ob, k, g, 2, False)

            head_norm_T(lambda h: oacc[:, h, :], oacc_b, 4, 4 * g, k)

    for h in range(8):
        KTh, KTh_b = next_kt()
        Vh, Vh_b = next_v()
        c.dma(KTh[:], P["KTg"][:, 8 + h].rearrange("c d t -> d c t"), writes=[KTh_b])
        c.dma(Vh[:], P["Vg"][:, 4 + h].rearrange("c p k e -> p c k e"), writes=[Vh_b])
        qh, qh_b = qs1[h % 2]
        c.dma(qh[:], P["QT"][8 + h], writes=[qh_b])
        for k in range(NK):
            Qk = qh[:, k * 128:(k + 1) * 128]
            obank = 2 + 2 * (k % 2)
            ntile = 2 * k + 2
            for ti in range(ntile):
                kb_hi = 8 * k + 7 - 4 * ti
                zb = next_s()
                Z = bank[zb]
                Zv = Z[:, :].rearrange("p (u t) -> p u t", u=4)
                for u in range(4):
                    kb = kb_hi - u
                    ck, kq = kb % 8, kb // 8
                    mm(Z[:, u * 128:(u + 1) * 128], KTh[:, ck, kq * 128:(kq + 1) * 128], Qk, (u == 0), False,
                       [KTh_b, qh_b], bankb[zb])
                state["e"] ^= 1
                et, et_b = ef[state["e"]]
                lt, lt_b = lb16[state["e"]]
                at, at_b = pb16[state["e"]]
                c.op("act", lambda e: e.activation(out=et[:], in_=Z[:], func=AF.Exp), reads=[bankb[zb]], writes=[et_b])
                c.op("act", lambda e: e.activation(out=lt[:].rearrange("p u t -> p (u t)"), in_=et[:], func=AF.Ln, bias=1.0),
                     reads=[et_b], writes=[lt_b])
                if ti < 2:
                    c.op("pool", lambda e: e.tensor_tensor(out=lt[:], in0=lt[:], in1=vzr[:, 4 * ti:4 * ti + 4, :], op=ALU.mult),
                         reads=[lt_b, vzr_b], writes=[lt_b])
                mm(Z[:], ntri[:], lt[:].rearrange("p u t -> p (u t)"), False, False, [ntri_b, lt_b], bankb[zb])
                for u2 in range(3):
                    mm(Zv[:, u2 + 1:4, :], nones[:], bc(lt[:, u2, :].unsqueeze(1), [128, 3 - u2, 128]), False, False,
                       [nones_b, lt_b], bankb[zb])
                if ti > 0:
                    mm(Zv, nones[:], bc(rb[:].unsqueeze(1), [128, 4, 128]), False, True, [nones_b, rb_b], bankb[zb])
                c.op("act", lambda e: e.activation(out=at[:].rearrange("p u t -> p (u t)"), in_=Z[:], func=AF.Exp),
                     reads=[bankb[zb]], writes=[at_b])
                if ti < 2:
                    c.op("pool", lambda e: e.tensor_tensor(out=at[:], in0=at[:], in1=vzr[:, 4 * ti:4 * ti + 4, :], op=ALU.mult),
                         reads=[at_b, vzr_b], writes=[at_b])
                if ti < ntile - 1:
                    c.op("dve", lambda e: e.tensor_reduce(out=rsum[:], in_=lt[:].rearrange("p u t -> p t u"),
                                                          axis=AX.X, op=ALU.add), reads=[lt_b], writes=[rsum_b])
                    if ti == 0:
                        c.op("dve", lambda e: e.tensor_copy(out=r32[:], in_=rsum[:]), reads=[rsum_b], writes=[r32_b])
                    else:
                        c.op("dve", lambda e: e.tensor_tensor(out=r32[:], in0=r32[:], in1=rsum[:], op=ALU.add),
                             reads=[r32_b, rsum_b], writes=[r32_b])
                    c.op("dve", lambda e: e.tensor_copy(out=rb[:], in_=r32[:]), reads=[r32_b], writes=[rb_b])
                for u in range(4):
                    kb = kb_hi - u
                    ck, kq = kb % 8, kb // 8
                    mm(bank[obank][:, 0:128], at[:, u, :], Vh[:, ck, kq, 0:128], (ti == 0 and u == 0),
                       (ti == ntile - 1 and u == 3), [at_b, Vh_b], bankb[obank])
            head_norm_T(lambda hh_: bank[obank][:, 0:128], bankb[obank], 1, 8 + h, k)

    if "dbg_onT" in P:
        c.dma(P["dbg_onT"], onT[:], reads=[onT_b])
    c.barrier()
    es_att.close()

    es_f = ExitStack()
    hres, hres_b = c.sb("f_h", [128, NK, D], F32, es_f)
    gv, gv_b = c.sb("f_gv", [128, D], F32, es_f)
    junk, junk_b = c.sb("f_junk", [128, D], F32, es_f)
    ss, ss_b = c.sb("f_ss", [128, 1], F32, es_f)
    xn, xn_b = c.sb("f_xn", [128, D], BF16, es_f)
    es_o = ExitStack()
    wo = [c.sb("f_wo%d" % i, [128, 16, 512], BF16, es_o) for i in range(2)]

    for k in range(NK):
        c.dma(hres[:, k, :], P["h"][k * 128:(k + 1) * 128, :], writes=[hres_b])
    for sl in range(4):
        wt, wt_b = wo[sl % 2]
        c.dma(wt[:], P["w_out"][:, sl * 512:(sl + 1) * 512].rearrange("(dc p) n -> p dc n", p=128), writes=[wt_b], q="pool")
        for k in range(NK):
            bi = k % 2
            for dc in range(16):
                mm(bank[bi][:], onT[:, dc, k * 128:(k + 1) * 128], wt[:, dc, :], dc == 0, dc == 15, [onT_b, wt_b], bankb[bi])
            c.op("dve", lambda e: e.tensor_tensor(out=hres[:, k, sl * 512:(sl + 1) * 512], in0=bank[bi][:],
                                                  in1=hres[:, k, sl * 512:(sl + 1) * 512], op=ALU.add),
                 reads=[bankb[bi], hres_b], writes=[hres_b])
    c.barrier()
    es_o.close()
    wg = [c.sb("f_wg%d" % i, [128, 16, 256], BF16, es_f) for i in range(2)]
    wu = [c.sb("f_wu%d" % i, [128, 16, 256], BF16, es_f) for i in range(2)]
    wd = [c.sb("f_wd%d" % i, [128, 2, D], BF16, es_f) for i in range(2)]
    mid = [c.sb("f_mid%d" % i, [128, 2, TL], BF16, es_f) for i in range(2)]
    sg = [c.sb("f_sg%d" % i, [128, 512], F32, es_f) for i in range(2)]
    c.dma(gv[:], P["nffn"].broadcast_to([128, D]), writes=[gv_b])
    tmp_pool = (junk, junk_b, ss, ss_b, xn, xn_b)
    xT, xT_b = onT, onT_b
    for k in range(NK):
        emit_rmsnorm_T(kk, hres[:, k, :], hres_b, gv, gv_b, xT, xT_b, k, tmp_pool, (6, 7))
    NSL = D_FF // 256
    for sl in range(NSL):
        wgt, wgt_b = wg[sl % 2]
        wut, wut_b = wu[sl % 2]
        wdt, wdt_b = wd[sl % 2]
        mt, mt_b = mid[sl % 2]
        f0 = sl * 256
        c.dma(wgt[:], P["w_gate"][:, f0:f0 + 256].rearrange("(dc p) n -> p dc n", p=128), writes=[wgt_b], q="pool")
        c.dma(wut[:], P["w_up"][:, f0:f0 + 256].rearrange("(dc p) n -> p dc n", p=128), writes=[wut_b], q="pool")
        c.dma(wdt[:], P["w_down"][f0:f0 + 256, :].rearrange("(fc p) n -> p fc n", p=128), writes=[wdt_b], q="pool")
        for fc in range(2):
            for th in range(2):
                ba, bb = 2 * th, 2 * th + 1
                for dc in range(16):
                    mm(bank[ba][:], wgt[:, dc, fc * 128:(fc + 1) * 128], xT[:, dc, th * 512:(th + 1) * 512],
                       dc == 0, dc == 15, [wgt_b, xT_b], bankb[ba])
                for dc in range(16):
                    mm(bank[bb][:], wut[:, dc, fc * 128:(fc + 1) * 128], xT[:, dc, th * 512:(th + 1) * 512],
                       dc == 0, dc == 15, [wut_b, xT_b], bankb[bb])
                sgt, sgt_b = sg[th]
                c.op("act", lambda e: e.activation(out=sgt[:], in_=bank[ba][:], func=AF.Silu), reads=[bankb[ba]], writes=[sgt_b])
                c.op("dve", lambda e: e.tensor_tensor(out=mt[:, fc, th * 512:(th + 1) * 512], in0=bank[bb][:], in1=sgt[:],
                                                      op=ALU.mult), reads=[bankb[bb], sgt_b], writes=[mt_b])
        for k in range(NK):
            for ct in range(4):
                bi = 4 + (ct % 2) + 2 * (k % 2)
                for fc in range(2):
                    mm(bank[bi][:], mt[:, fc, k * 128:(k + 1) * 128], wdt[:, fc, ct * 512:(ct + 1) * 512],
                       fc == 0, fc == 1, [mt_b, wdt_b], bankb[bi])
                c.op("dve", lambda e: e.tensor_tensor(out=hres[:, k, ct * 512:(ct + 1) * 512], in0=bank[bi][:],
                                                      in1=hres[:, k, ct * 512:(ct + 1) * 512], op=ALU.add),
                     reads=[bankb[bi], hres_b], writes=[hres_b])
    c.dma(gv[:], P["nfinal"].broadcast_to([128, D]), reads=[], writes=[gv_b])
    for k in range(NK):
        c.dma(P["h_out"][k * 128:(k + 1) * 128, :], hres[:, k, :], reads=[hres_b])
        c.op("act", lambda e: e.activation(out=junk[:], in_=hres[:, k, :], func=AF.Square, accum_out=ss[:]),
             reads=[hres_b], writes=[junk_b, ss_b])
        c.op("act", lambda e: e.activation(out=ss[:], in_=ss[:], func=AF.Ln, scale=1.0 / D, bias=EPS), reads=[ss_b], writes=[ss_b])
        c.op("act", lambda e: e.activation(out=ss[:], in_=ss[:], func=AF.Exp, scale=-0.5), reads=[ss_b], writes=[ss_b])
        c.op("dve", lambda e: e.scalar_tensor_tensor(out=junk[:], in0=hres[:, k, :], scalar=ss[:, 0:1], in1=gv[:],
                                                     op0=ALU.mult, op1=ALU.mult),
             reads=[hres_b, ss_b, gv_b], writes=[junk_b])
        c.dma(P["hn_out"][k * 128:(k + 1) * 128, :], junk[:], reads=[junk_b])
    c.barrier()
    es_f.close()
    es_all.close()


def build_pb():
    nc = bass.Bass("TRN2", target_bir_lowering=False)

    def din(name, shape, dt=F32):
        return nc.dram_tensor(name, list(shape), dt, kind="ExternalInput").ap()

    def dout(name, shape, dt=F32):
        return nc.dram_tensor(name, list(shape), dt, kind="ExternalOutput").ap()

    P = {
        "h": din("h", [TL, D]), "QT": din("QT", [16, 128, TL], BF16), "gates": din("gates", [128, NK, 24]),
        "KTg": din("KTg", [8, 16, 128, TL], BF16), "Vg": din("Vg", [8, 12, 128, NK, DV], BF16),
        "wck1": din("wck1", [4096, 256]), "wck2": din("wck2", [256, 128]),
        "wcv1": din("wcv1", [4096, 256]), "wcv2": din("wcv2", [256, 128]),
        "cmp_pos": din("cmp_pos", [32, 128]), "poscmp": din("poscmp", [128, 4], I32), "invf": din("invf", [1, 16]),
        "ngrp": din("ngrp", [1, D]), "w_out": din("w_out", [D, D]), "nffn": din("nffn", [1, D]),
        "w_gate": din("w_gate", [D, D_FF]), "w_up": din("w_up", [D, D_FF]), "w_down": din("w_down", [D_FF, D]),
        "nfinal": din("nfinal", [1, D]), "ident": din("ident", [128, 128]),
        "Gm": din("Gm", [128, 64 * 128]), "mctn": din("mctn", [128, 960]), "mcnt": din("mcnt", [128, 3, 128]),
        "Arel": din("Arel", [128, 240]), "Brel": din("Brel", [128, 240]),
        "dz": din("dz", [128, 8, 128]), "wz": din("wz", [128, 12, 128]), "vzr": din("vzr", [128, 8, 128]),
        "ntri": din("ntri", [128, 128]), "nones": din("nones", [128, 128]),
        "h_out": dout("h_out", [TL, D]), "hn_out": dout("hn_out", [TL, D]),
    }
    if DEBUG:
        P["dbg_onT"] = dout("dbg_onT", [128, 16, TL], BF16)
    kk = K(nc)
    kk.load_ident(P["ident"])
    emit_phase_b(kk, P)
    kk.c.finish()
    return nc


def make_consts(cidx):
    p = np.arange(128)
    out = {}
    u = np.arange(64 * 128)
    out["Gm"] = (np.arange(128)[:, None] == (u[None, :] // 64)).astype(np.float32)
    idx = np.arange(960)
    m = idx - 448 - 8 * cidx
    out["mctn"] = np.where(16 * m[None, :] + 31 <= p[:, None], 0.0, NEG).astype(np.float32)
    mcnt = np.zeros((128, 3, 128), np.float32)
    for i, dp in enumerate((-16, -8, 0)):
        dd = dp - cidx
        ok = (16 * p[:, None] + 31 + 128 * dd) <= p[None, :]
        mcnt[:, i, :] = np.where(ok, 0.0, NEG)
    out["mcnt"] = mcnt
    jr = np.arange(240) - 112
    bt = 2 * cidx + (p >= 64).astype(np.int64)
    causal = jr[None, :] <= bt[:, None]
    forced = (jr[None, :] == bt[:, None]) | (jr[None, :] == bt[:, None] - 1)
    out["Arel"] = (causal & ~forced).astype(np.float32)
    out["Brel"] = np.where(~causal, -1e4, np.where(forced, 1e4, 0.0)).astype(np.float32)
    dzt = np.zeros((128, 8, 128), np.float32)
    dzt[:, cidx, :] = np.where(p[:, None] <= p[None, :], 0.0, NEG)
    out["dz"] = dzt
    wzt = np.zeros((128, 12, 128), np.float32)
    for r in range(12):
        diff = 128 * (cidx + 4 - r) + p[None, :] - p[:, None]
        wzt[:, r, :] = np.where((diff >= 0) & (diff < 512), 0.0, NEG)
    out["wz"] = wzt
    vz = np.zeros((128, 8, 128), np.float32)
    for r in range(8):
        if r < cidx:
            vz[:, 7 - r, :] = 1.0
        elif r == cidx:
            vz[:, 7 - r, :] = (p[:, None] < p[None, :]).astype(np.float32)
    out["vzr"] = vz
    out["ntri"] = -(p[:, None] >= p[None, :]).astype(np.float32)
    out["nones"] = -np.ones((128, 128), np.float32)
    out["ident"] = np.eye(128, dtype=np.float32)
    out["invf"] = _inv_freq()
    return out


_CONSTS = {}


def run_pb(h_parts, pa_res, positions, W, l):
    if "pb" not in _PROG:
        _PROG["pb"] = build_pb()
    nc = _PROG["pb"]
    KTg = np.stack([np.asarray(pa_res[c]["KT"]) for c in range(NCORES)])
    Vg = np.stack([np.asarray(pa_res[c]["V"]) for c in range(NCORES)])
    cmp_end = np.arange(511) * 16 + 31
    pc = np.zeros(512, np.int32)
    pc[:511] = positions[cmp_end]
    poscmp = np.ascontiguousarray(pc.reshape(4, 128).T)
    in_maps = []
    for c in range(NCORES):
        if c not in _CONSTS:
            _CONSTS[c] = make_consts(c)
        m = dict(_CONSTS[c])
        m.update({
            "h": h_parts[c], "QT": np.asarray(pa_res[c]["QT"]), "gates": np.asarray(pa_res[c]["gates"]),
            "KTg": KTg, "Vg": Vg,
            "wck1": W["w_cmp_k1"][l], "wck2": W["w_cmp_k2"][l], "wcv1": W["w_cmp_v1"][l], "wcv2": W["w_cmp_v2"][l],
            "cmp_pos": W["cmp_pos"][l], "poscmp": poscmp,
            "ngrp": W["norm_grp"][l][None, :], "w_out": W["w_out"][l], "nffn": W["norm_ffn"][l][None, :],
            "w_gate": W["w_gate"][l], "w_up": W["w_up"][l], "w_down": W["w_down"][l],
            "nfinal": W["norm_final"][None, :],
        })
        in_maps.append(m)
    res = run_bass_kernel_spmd(nc, in_maps, core_ids=list(range(NCORES)))
    return res.results


def kernel(**inputs):
    W = {k: np.asarray(v) for k, v in inputs.items()}
    x = W["x"][0]
    positions = W["positions"][0]
    h_parts = shard_tokens(np.ascontiguousarray(x, dtype=np.float32))
    pos_parts = shard_tokens(positions.astype(np.int32))
    hn = None
    for l in range(4):
        pa = run_pa(h_parts, pos_parts, W["w_in"][l], W["norm_mix"][l], W["b_gate"][l])
        pb = run_pb(h_parts, pa, positions.astype(np.int32), W, l)
        h_parts = [np.asarray(pb[c]["h_out"]) for c in range(NCORES)]
        hn = [np.asarray(pb[c]["hn_out"]) for c in range(NCORES)]
    out = unshard_tokens(hn)
    return out[None].astype(np.float32)
```

```python
from contextlib import ExitStack
import numpy as np
import ml_dtypes
import concourse.bass as bass
import concourse.mybir as mybir
from concourse.bass_utils import run_bass_kernel_spmd

F32 = mybir.dt.float32
BF16 = mybir.dt.bfloat16
I32 = mybir.dt.int32
ALU = mybir.AluOpType
AF = mybir.ActivationFunctionType
AX = mybir.AxisListType

NCORES = 8
D = 2048
S = 8192
TL = 1024
NK = 8
D_IN = 5656
D_FF = 5632
HD = 128
SCALE = HD ** -0.5
NEG = -30000.0
DV = 130
EPS = 1e-6
N_DMA_SEMS = 24
PI = float(np.pi)


class Buf:
    __slots__ = ("name", "w", "r", "psum")

    def __init__(self, name, psum=False):
        self.name = name
        self.w = None
        self.r = {}
        self.psum = psum


class Ctx:
    def __init__(self, nc):
        self.nc = nc
        self.es = ExitStack()
        self.eng = {"pe": nc.tensor, "act": nc.scalar, "dve": nc.vector,
                    "pool": nc.gpsimd, "sp": nc.sync}
        self.sems = {}
        self.cnt = {}
        self.seen = {k: {} for k in self.eng}
        for k in self.eng:
            self.sems[k] = self.es.enter_context(nc.semaphore("s_" + k))
            self.cnt[k] = 0
        for i in range(N_DMA_SEMS):
            k = "d%d" % i
            self.sems[k] = self.es.enter_context(nc.semaphore("s_" + k))
            self.cnt[k] = 0
        self.dma_rr = 0
        self.n_inst = 0

    def sb(self, name, shape, dt, es=None):
        t = (es or self.es).enter_context(self.nc.sbuf_tensor(name, list(shape), dt))
        return t, Buf(name)

    def ps(self, name, shape, dt=F32):
        t = self.es.enter_context(self.nc.psum_tensor(name, list(shape), dt))
        return t, Buf(name, psum=True)

    def _need(self, tok, needs):
        if tok is None:
            return
        sk, v = tok
        if needs.get(sk, 0) < v:
            needs[sk] = v

    def _deps(self, ek, reads, writes):
        needs = {}
        for b in reads:
            self._need(b.w, needs)
            if b.psum:
                for sk, v in b.r.items():
                    self._need((sk, v), needs)
        for b in writes:
            self._need(b.w, needs)
            for sk, v in b.r.items():
                self._need((sk, v), needs)
        e = self.eng[ek]
        seen = self.seen[ek]
        for sk, v in needs.items():
            if sk == ek and ek == "pe":
                continue
            if seen.get(sk, 0) < v:
                e.wait_ge(self.sems[sk], v)
                seen[sk] = v

    def _mark(self, tok, reads, writes):
        sk, v = tok
        for b in reads:
            if b.psum:
                b.w = tok
                b.r = {}
            elif b.r.get(sk, 0) < v:
                b.r[sk] = v
        for b in writes:
            b.w = tok
            b.r = {}

    def op(self, ek, fn, reads=(), writes=()):
        self._deps(ek, reads, writes)
        ins = fn(self.eng[ek])
        self.cnt[ek] += 1
        ins.then_inc(self.sems[ek], 1)
        tok = (ek, self.cnt[ek])
        self._mark(tok, reads, writes)
        self.n_inst += 1
        return tok

    def dma(self, out, in_, reads=(), writes=(), q="sp"):
        dk = "d%d" % self.dma_rr
        self.dma_rr = (self.dma_rr + 1) % N_DMA_SEMS
        prev = self.cnt[dk]
        e = self.eng[q]
        if prev > 0 and self.seen[q].get(dk, 0) < prev:
            e.wait_ge(self.sems[dk], prev)
            self.seen[q][dk] = prev
        self._deps(q, reads, writes)
        ins = e.dma_start(out=out, in_=in_)
        self.cnt[dk] += 16
        ins.then_inc(self.sems[dk], 16)
        tok = (dk, self.cnt[dk])
        self._mark(tok, reads, writes)
        self.n_inst += 1
        return tok

    def coll_allgather(self, in_ap, out_ap, reads=(), writes=()):
        dk = "d%d" % self.dma_rr
        self.dma_rr = (self.dma_rr + 1) % N_DMA_SEMS
        prev = self.cnt[dk]
        e = self.eng["pool"]
        if prev > 0 and self.seen["pool"].get(dk, 0) < prev:
            e.wait_ge(self.sems[dk], prev)
            self.seen["pool"][dk] = prev
        self._deps("pool", reads, writes)
        ins = e.collective_compute("AllGather", ALU.bypass, replica_groups=[list(range(NCORES))],
                                   ins=[in_ap], outs=[out_ap])
        self.cnt[dk] += 16
        ins.then_inc(self.sems[dk], 16)
        tok = (dk, self.cnt[dk])
        self._mark(tok, reads, writes)
        self.n_inst += 1
        return tok

    def wait_tok(self, ek, tok):
        sk, v = tok
        if self.seen[ek].get(sk, 0) < v:
            self.eng[ek].wait_ge(self.sems[sk], v)
            self.seen[ek][sk] = v

    def barrier(self):
        for ek in self.eng:
            for sk, v in self.cnt.items():
                if v > 0 and sk != ek:
                    self.wait_tok(ek, (sk, v))

    def finish(self):
        for sk, v in self.cnt.items():
            if v > 0 and sk != "sp":
                self.wait_tok("sp", (sk, v))
        self.es.close()


def bc(ap, shape):
    return ap.to_broadcast(list(shape))


class K:
    def __init__(self, nc):
        self.nc = nc
        self.c = Ctx(nc)
        c = self.c
        self.bank = []
        self.bankb = []
        for i in range(8):
            t, b = c.ps("bank%d" % i, [128, 512], F32)
            self.bank.append(t)
            self.bankb.append(b)
        self.ident, self.ident_b = c.sb("ident_sb", [128, 128], BF16)

    def bank_bf16(self, i):
        return self.bank[i][:].bitcast(BF16)

    def load_ident(self, ident_dram):
        self.c.dma(self.ident[:], ident_dram, writes=[self.ident_b], q="pool")


def emit_rmsnorm_T(kk, h_ap, h_b, gvec, gvec_b, xT, xT_b, kblk, tmp_pool, tbank):
    c = kk.c
    junk, junk_b, ss, ss_b, xn, xn_b = tmp_pool
    c.op("act", lambda e: e.activation(out=junk[:], in_=h_ap, func=AF.Square, accum_out=ss[:]),
         reads=[h_b], writes=[junk_b, ss_b])
    c.op("act", lambda e: e.activation(out=ss[:], in_=ss[:], func=AF.Ln, scale=1.0 / D, bias=EPS),
         reads=[ss_b], writes=[ss_b])
    c.op("act", lambda e: e.activation(out=ss[:], in_=ss[:], func=AF.Exp, scale=-0.5),
         reads=[ss_b], writes=[ss_b])
    c.op("dve", lambda e: e.scalar_tensor_tensor(out=xn[:], in0=h_ap, scalar=ss[:, 0:1], in1=gvec[:],
                                                 op0=ALU.mult, op1=ALU.mult),
         reads=[h_b, ss_b, gvec_b], writes=[xn_b])
    for grp in range(4):
        bi = tbank[grp % 2]
        pt = kk.bank_bf16(bi)
        for j in range(4):
            dc = grp * 4 + j
            c.op("pe", lambda e: e.transpose(out=pt[:, j * 128:(j + 1) * 128],
                                             in_=xn[:, dc * 128:(dc + 1) * 128], identity=kk.ident[:]),
                 reads=[xn_b, kk.ident_b], writes=[kk.bankb[bi]])
        eng = "dve" if grp % 2 == 0 else "act"
        src = pt[:, 0:512].rearrange("p (j t) -> p j t", j=4)
        dst = xT[:, grp * 4:(grp + 1) * 4, kblk * 128:(kblk + 1) * 128]
        if eng == "dve":
            c.op("dve", lambda e: e.tensor_copy(out=dst, in_=src), reads=[kk.bankb[bi]], writes=[xT_b])
        else:
            c.op("act", lambda e: e.copy(out=dst, in_=src), reads=[kk.bankb[bi]], writes=[xT_b])


def emit_sin(c, dst, dst_b, ang, ang_b, shift, ri, ri_b, rf, rf_b, rx, rx_b):
    c.op("dve", lambda e: e.tensor_scalar_add(out=rx[:], in0=ang[:], scalar1=shift), reads=[ang_b], writes=[rx_b])
    c.op("dve", lambda e: e.tensor_scalar_mul(out=rf[:], in0=rx[:], scalar1=1.0 / (2 * PI)), reads=[rx_b], writes=[rf_b])
    c.op("dve", lambda e: e.tensor_copy(out=ri[:], in_=rf[:]), reads=[rf_b], writes=[ri_b])
    c.op("dve", lambda e: e.tensor_copy(out=rf[:], in_=ri[:]), reads=[ri_b], writes=[rf_b])
    c.op("dve", lambda e: e.scalar_tensor_tensor(out=rx[:], in0=rf[:], scalar=-2 * PI, in1=rx[:],
                                                 op0=ALU.mult, op1=ALU.add), reads=[rf_b, rx_b], writes=[rx_b])
    c.op("dve", lambda e: e.tensor_single_scalar(out=rf[:], in_=rx[:], scalar=PI, op=ALU.is_gt), reads=[rx_b], writes=[rf_b])
    c.op("dve", lambda e: e.scalar_tensor_tensor(out=rx[:], in0=rf[:], scalar=-2 * PI, in1=rx[:],
                                                 op0=ALU.mult, op1=ALU.add), reads=[rf_b, rx_b], writes=[rx_b])
    c.op("dve", lambda e: e.tensor_scalar(out=rx[:], in0=rx[:], scalar1=-3.141592, scalar2=3.141592,
                                          op0=ALU.max, op1=ALU.min), reads=[rx_b], writes=[rx_b])
    c.op("act", lambda e: e.activation(out=dst[:], in_=rx[:], func=AF.Sin), reads=[rx_b], writes=[dst_b])

C_QN, C_KC, C_VC, C_KS, C_VS, C_KW, C_VW, C_G, C_SQ, C_SK, C_SV = (
    0, 1024, 1280, 1536, 1792, 2048, 2304, 2560, 2584, 3608, 4632)
PA_SLABS = [
    ("qn", 0, 512), ("qn", 512, 512), ("kcvc", 1024, 512), ("ksvs", 1536, 512),
    ("kwvw", 2048, 512), ("g", 2560, 24), ("sq", 2584, 512), ("sq", 3096, 512),
    ("sk", 3608, 512), ("sk", 4120, 512), ("sv", 4632, 512), ("sv", 5144, 512),
]


def emit_phase_a(kk, h_src, w_in, nmix, bg, pos, invf, QT, KT, V, gates_out, h_resident=None):
    c = kk.c
    nc = kk.nc
    es = ExitStack()
    xT, xT_b = c.sb("a_xT", [128, 16, TL], BF16, es)
    gvec, gvec_b = c.sb("a_gvec", [128, D], F32, es)
    junk, junk_b = c.sb("a_junk", [128, D], F32, es)
    ss, ss_b = c.sb("a_ss", [128, 1], F32, es)
    xn, xn_b = c.sb("a_xn", [128, D], BF16, es)
    hblk = [c.sb("a_h%d" % i, [128, D], F32, es) for i in range(2)]
    wsl = [c.sb("a_w%d" % i, [128, 16, 512], BF16, es) for i in range(2)]
    ev = [c.sb("a_ev%d" % i, [128, 4, 128], BF16, es) for i in range(2)]
    tst = [c.sb("a_ts%d" % i, [128, 4, TL], BF16, es) for i in range(2)]
    vst = [c.sb("a_vs%d" % i, [128, 4, NK, DV], BF16, es) for i in range(2)]
    gsb, gsb_b = c.sb("a_gates", [128, NK, 24], F32, es)
    bgb, bgb_b = c.sb("a_bg", [128, 24], F32, es)
    posi, posi_b = c.sb("a_posi", [128, NK], I32, es)
    posf, posf_b = c.sb("a_posf", [128, NK], F32, es)
    invb, invb_b = c.sb("a_invf", [128, 16], F32, es)
    ang, ang_b = c.sb("a_ang", [128, NK, 16], F32, es)
    cs, cs_b = c.sb("a_cs", [128, NK, 16], F32, es)
    sn, sn_b = c.sb("a_sn", [128, NK, 16], F32, es)
    csq, csq_b = c.sb("a_csq", [128, NK, 16], F32, es)
    snq, snq_b = c.sb("a_snq", [128, NK, 16], F32, es)
    rt = [c.sb("a_rt%d" % i, [128, 4, 16], F32, es) for i in range(4)]
    gtmp, gtmp_b = c.sb("a_gtmp", [128, 24], F32, es)

    c.dma(gvec[:], nmix.broadcast_to([128, D]), writes=[gvec_b])
    c.dma(bgb[:], bg.broadcast_to([128, 24]), writes=[bgb_b])
    c.dma(invb[:], invf.broadcast_to([128, 16]), writes=[invb_b])
    c.dma(posi[:], pos, writes=[posi_b])
    for i in range(2):
        c.op("pool", lambda e: e.memset(vst[i][0][:, :, :, 128:129], 1.0), writes=[vst[i][1]])
        c.op("pool", lambda e: e.memset(vst[i][0][:, :, :, 129:130], 0.0), writes=[vst[i][1]])

    c.op("dve", lambda e: e.tensor_copy(out=posf[:], in_=posi[:]), reads=[posi_b], writes=[posf_b])
    c.op("dve", lambda e: e.tensor_tensor(out=ang[:], in0=bc(posf[:].unsqueeze(2), [128, NK, 16]),
                                          in1=bc(invb[:].unsqueeze(1), [128, NK, 16]), op=ALU.mult),
         reads=[posf_b, invb_b], writes=[ang_b])
    rr_i, rr_ib = c.sb("a_rri", [128, NK, 16], I32, es)
    rr_f, rr_fb = c.sb("a_rrf", [128, NK, 16], F32, es)
    rr_x, rr_xb = c.sb("a_rrx", [128, NK, 16], F32, es)
    for (dst, dst_b, shift) in ((sn, sn_b, 0.0), (cs, cs_b, 0.5 * PI)):
        emit_sin(c, dst, dst_b, ang, ang_b, shift, rr_i, rr_ib, rr_f, rr_fb, rr_x, rr_xb)
    c.op("dve", lambda e: e.tensor_scalar_mul(out=csq[:], in0=cs[:], scalar1=SCALE), reads=[cs_b], writes=[csq_b])
    c.op("dve", lambda e: e.tensor_scalar_mul(out=snq[:], in0=sn[:], scalar1=SCALE), reads=[sn_b], writes=[snq_b])

    tmp_pool = (junk, junk_b, ss, ss_b, xn, xn_b)
    for k in range(NK):
        if h_resident is None:
            ht, ht_b = hblk[k % 2]
            c.dma(ht[:], h_src[k * 128:(k + 1) * 128, :], writes=[ht_b])
            h_ap = ht[:]
        else:
            ht_b = h_resident[1]
            h_ap = h_resident[0][:, k, :]
        emit_rmsnorm_T(kk, h_ap, ht_b, gvec, gvec_b, xT, xT_b, k, tmp_pool, (6, 7))

    def rope(pt, dst, dst_b, h0, nh, cosb, sinb, tabs_b, k, pb):
        co = bc(cosb[:, k, :].unsqueeze(1), [128, nh, 16])
        si = bc(sinb[:, k, :].unsqueeze(1), [128, nh, 16])
        x1 = pt[:, h0:h0 + nh, 0:16]
        x2 = pt[:, h0:h0 + nh, 16:32]
        t = [r[0][:, 0:nh, :] for r in rt]
        tb = [r[1] for r in rt]
        c.op("dve", lambda e: e.tensor_tensor(out=t[0], in0=x1, in1=co, op=ALU.mult), reads=[pb] + tabs_b, writes=[tb[0]])
        c.op("dve", lambda e: e.tensor_tensor(out=t[1], in0=x2, in1=si, op=ALU.mult), reads=[pb] + tabs_b, writes=[tb[1]])
        c.op("dve", lambda e: e.tensor_tensor(out=t[2], in0=x2, in1=co, op=ALU.mult), reads=[pb] + tabs_b, writes=[tb[2]])
        c.op("dve", lambda e: e.tensor_tensor(out=t[3], in0=x1, in1=si, op=ALU.mult), reads=[pb] + tabs_b, writes=[tb[3]])
        c.op("dve", lambda e: e.tensor_tensor(out=dst[:, h0:h0 + nh, 0:16], in0=t[0], in1=t[1], op=ALU.subtract),
             reads=[tb[0], tb[1]], writes=[dst_b])
        c.op("dve", lambda e: e.tensor_tensor(out=dst[:, h0:h0 + nh, 16:32], in0=t[2], in1=t[3], op=ALU.add),
             reads=[tb[2], tb[3]], writes=[dst_b])

    n_t = 0
    n_v = 0
    for si, (typ, c0, ncol) in enumerate(PA_SLABS):
        wt, wt_b = wsl[si % 2]
        c.dma(wt[:, :, 0:ncol], w_in[:, c0:c0 + ncol].rearrange("(dc p) n -> p dc n", p=128),
              writes=[wt_b], q="pool")
        uses_t = typ in ("qn", "kcvc", "ksvs", "kwvw", "sq", "sk")
        uses_v = typ in ("ksvs", "kwvw", "sv")
        if uses_t:
            ts, ts_b = tst[n_t % 2]
            n_t += 1
        if uses_v:
            vs, vs_b = vst[n_v % 2]
            n_v += 1
        for k in range(NK):
            bi = k % 2
            pb = kk.bankb[bi]
            pfull = kk.bank[bi]
            for dc in range(16):
                c.op("pe", lambda e: e.matmul(pfull[:, 0:ncol], lhsT=xT[:, dc, k * 128:(k + 1) * 128],
                                              rhs=wt[:, dc, 0:ncol], start=(dc == 0), stop=(dc == 15)),
                     reads=[xT_b, wt_b], writes=[pb])
            pt = pfull[:, :].rearrange("p (h e) -> p h e", h=4)
            evt, evt_b = ev[k % 2]
            nT = 0
            if typ == "qn":
                c.op("act", lambda e: e.activation(out=evt[:, :, 32:128], in_=pt[:, :, 32:128], func=AF.Copy, scale=SCALE),
                     reads=[pb], writes=[evt_b])
                rope(pt, evt, evt_b, 0, 4, csq, snq, [csq_b, snq_b], k, pb)
                nT = 4
            elif typ == "sq":
                c.op("act", lambda e: e.activation(out=evt[:], in_=pt, func=AF.Copy, scale=SCALE),
                     reads=[pb], writes=[evt_b])
                nT = 4
            elif typ in ("kcvc", "sk"):
                c.op("act", lambda e: e.copy(out=evt[:], in_=pt), reads=[pb], writes=[evt_b])
                nT = 4
            elif typ in ("ksvs", "kwvw"):
                c.op("act", lambda e: e.copy(out=evt[:, 0:2, 32:128], in_=pt[:, 0:2, 32:128]), reads=[pb], writes=[evt_b])
                rope(pt, evt, evt_b, 0, 2, cs, sn, [cs_b, sn_b], k, pb)
                c.op("act", lambda e: e.copy(out=vs[:, 0:2, k, 0:128], in_=pt[:, 2:4, :]), reads=[pb], writes=[vs_b])
                nT = 2
            elif typ == "sv":
                c.op("act", lambda e: e.copy(out=vs[:, 0:4, k, 0:128], in_=pt), reads=[pb], writes=[vs_b])
            elif typ == "g":
                c.op("dve", lambda e: e.tensor_tensor(out=gtmp[:], in0=pfull[:, 0:24], in1=bgb[:], op=ALU.add),
                     reads=[pb, bgb_b], writes=[gtmp_b])
                c.op("act", lambda e: e.activation(out=gsb[:, k, :], in_=gtmp[:], func=AF.Sigmoid),
                     reads=[gtmp_b], writes=[gsb_b])
            if nT:
                tbi = 6 + (k % 2)
                ptb = kk.bank_bf16(tbi)
                for j in range(nT):
                    c.op("pe", lambda e: e.transpose(out=ptb[:, j * 128:(j + 1) * 128], in_=evt[:, j, :],
                                                     identity=kk.ident[:]),
                         reads=[evt_b, kk.ident_b], writes=[kk.bankb[tbi]])
                c.op("dve", lambda e: e.tensor_copy(
                    out=ts[:, 0:nT, k * 128:(k + 1) * 128],
                    in_=ptb[:, 0:nT * 128].rearrange("p (j t) -> p j t", j=nT)),
                    reads=[kk.bankb[tbi]], writes=[ts_b])
        if typ == "qn":
            h0 = c0 // 128
            c.dma(QT[h0:h0 + 4].rearrange("h p t -> p h t"), ts[:], reads=[ts_b])
        elif typ == "sq":
            h0 = 8 + (c0 - C_SQ) // 128
            c.dma(QT[h0:h0 + 4].rearrange("h p t -> p h t"), ts[:], reads=[ts_b])
        elif typ == "kcvc":
            c.dma(KT[0:4].rearrange("h p t -> p h t"), ts[:], reads=[ts_b])
        elif typ == "ksvs":
            c.dma(KT[4:6].rearrange("h p t -> p h t"), ts[:, 0:2, :], reads=[ts_b])
            c.dma(V[0:2].rearrange("h p k e -> p h k e"), vs[:, 0:2], reads=[vs_b])
        elif typ == "kwvw":
            c.dma(KT[6:8].rearrange("h p t -> p h t"), ts[:, 0:2, :], reads=[ts_b])
            c.dma(V[2:4].rearrange("h p k e -> p h k e"), vs[:, 0:2], reads=[vs_b])
        elif typ == "sk":
            h0 = 8 + (c0 - C_SK) // 128
            c.dma(KT[h0:h0 + 4].rearrange("h p t -> p h t"), ts[:], reads=[ts_b])
        elif typ == "sv":
            h0 = 4 + (c0 - C_SV) // 128
            c.dma(V[h0:h0 + 4].rearrange("h p k e -> p h k e"), vs[:], reads=[vs_b])
        elif typ == "g":
            c.dma(gates_out, gsb[:], reads=[gsb_b])
    c.barrier()
    es.close()


def build_pa():
    nc = bass.Bass("TRN2", target_bir_lowering=False)

    def din(name, shape, dt=F32):
        return nc.dram_tensor(name, list(shape), dt, kind="ExternalInput").ap()

    def dout(name, shape, dt=F32):
        return nc.dram_tensor(name, list(shape), dt, kind="ExternalOutput").ap()

    h = din("h", [TL, D])
    w_in = din("w_in", [D, D_IN])
    nmix = din("nmix", [1, D])
    bg = din("bg", [1, 24])
    pos = din("pos", [128, NK], I32)
    invf = din("invf", [1, 16])
    ident = din("ident", [128, 128])
    QT = dout("QT", [16, 128, TL], BF16)
    KT = dout("KT", [16, 128, TL], BF16)
    V = dout("V", [12, 128, NK, DV], BF16)
    gates = dout("gates", [128, NK, 24])
    kk = K(nc)
    kk.load_ident(ident)
    emit_phase_a(kk, h, w_in, nmix, bg, pos, invf, QT, KT, V, gates)
    kk.c.finish()
    return nc


def _inv_freq():
    return (500000.0 ** (-np.arange(0, 32, 2, dtype=np.float32) / 32)).astype(np.float32)[None, :]


def shard_tokens(a):
    blk = a.reshape(64, 128, *a.shape[1:])
    return [np.ascontiguousarray(blk[c::8].reshape(TL, *a.shape[1:])) for c in range(NCORES)]


def unshard_tokens(parts):
    out = np.empty((64, 128) + parts[0].shape[1:], parts[0].dtype)
    for c in range(NCORES):
        out[c::8] = parts[c].reshape(8, 128, *parts[c].shape[1:])
    return out.reshape(S, *parts[0].shape[1:])


_PROG = {}


def run_pa(h_parts, pos_parts, w_in, nmix, bg):
    if "pa" not in _PROG:
        _PROG["pa"] = build_pa()
    nc = _PROG["pa"]
    ident = np.eye(128, dtype=np.float32)
    invf = _inv_freq()
    in_maps = []
    for c in range(NCORES):
        in_maps.append({
            "h": h_parts[c], "w_in": w_in, "nmix": nmix[None, :], "bg": bg[None, :],
            "pos": np.ascontiguousarray(pos_parts[c].reshape(NK, 128).T), "invf": invf, "ident": ident,
        })
    res = run_bass_kernel_spmd(nc, in_maps, core_ids=list(range(NCORES)))
    return res.results


DEBUG = False
GELU_C0 = 0.7978845608028654
GELU_C1 = 0.044715


def emit_phase_b(kk, P):
    c = kk.c
    nc = kk.nc
    bank, bankb = kk.bank, kk.bankb
    ident, ident_b = kk.ident, kk.ident_b
    es_all = ExitStack()
    es_att = ExitStack()

    def mm(out, lhsT, rhs, start, stop, reads, wb):
        c.op("pe", lambda e: e.matmul(out, lhsT=lhsT, rhs=rhs, start=start, stop=stop, skip_group_check=True),
             reads=reads, writes=[wb])

    onT, onT_b = c.sb("b_onT", [128, 16, TL], BF16, es_all)
    KTr = [c.sb("b_KT%d" % i, [128, 8, TL], BF16, es_att) for i in range(3)]
    Vr = [c.sb("b_V%d" % i, [128, 8, NK, DV], BF16, es_att) for i in range(3)]
    Gm, Gm_b = c.sb("b_G", [128, 64 * 128], BF16, es_att)
    mctn, mctn_b = c.sb("b_mctn", [128, 960], BF16, es_att)
    mcnt, mcnt_b = c.sb("b_mcnt", [128, 3, 128], BF16, es_att)
    Arel, Arel_b = c.sb("b_Arel", [128, 240], F32, es_att)
    Brel, Brel_b = c.sb("b_Brel", [128, 240], F32, es_att)
    dz, dz_b = c.sb("b_dz", [128, 8, 128], BF16, es_att)
    wz, wz_b = c.sb("b_wz", [128, 12, 128], BF16, es_att)
    vzr, vzr_b = c.sb("b_vzr", [128, 8, 128], BF16, es_att)
    ntri, ntri_b = c.sb("b_ntri", [128, 128], BF16, es_att)
    nones, nones_b = c.sb("b_nones", [128, 128], BF16, es_att)
    gates, gates_b = c.sb("b_gates", [128, NK, 24], F32, es_att)
    ggT, ggT_b = c.sb("b_ggT", [128, 16], F32, es_att)
    kcT = [c.sb("b_kcT%d" % g, [128, 512], BF16, es_att) for g in range(2)]
    vcs = [c.sb("b_vc%d" % g, [128, 4, DV], BF16, es_att) for g in range(2)]
    qsb = [c.sb("b_q%d" % i, [128, 4, TL], BF16, es_att) for i in range(1)]
    ef = [c.sb("b_ef%d" % i, [128, 512], F32, es_att) for i in range(2)]
    pb16 = [c.sb("b_p%d" % i, [128, 4, 128], BF16, es_att) for i in range(2)]
    lb16 = [c.sb("b_l%d" % i, [128, 4, 128], BF16, es_att) for i in range(2)]
    padbuf, padbuf_b = c.sb("b_pad", [128, 516], F32, es_att)
    qi, qi_b = c.sb("b_qi", [128, 512], F32, es_att)
    pslc, pslc_b = c.sb("b_pslc", [128, 128], F32, es_att)
    imp, imp_b = c.sb("b_imp", [128, 128], F32, es_att)
    imp2, imp2_b = c.sb("b_imp2", [128, 128], F32, es_att)
    sel2, sel2_b = c.sb("b_sel2", [128, 128], F32, es_att)
    selb, selb_b = c.sb("b_selb", [128, 128], BF16, es_att)
    selT, selT_b = c.sb("b_selT", [128, 128], BF16, es_att)
    m8a, m8a_b = c.sb("b_m8a", [128, 8], F32, es_att)
    m8b, m8b_b = c.sb("b_m8b", [128, 8], F32, es_att)
    sm, sm_b = c.sb("b_sm", [128, 16], F32, es_att)
    oacc, oacc_b = c.sb("b_oacc", [128, 4, 128], F32, es_att)
    onb, onb_b = c.sb("b_onb", [128, 4, 128], BF16, es_att)
    sqj, sqj_b = c.sb("b_sqj", [128, 128], F32, es_att)
    r32, r32_b = c.sb("b_r32", [128, 128], F32, es_att)
    rsum, rsum_b = c.sb("b_rsum", [128, 128], F32, es_att)
    rb, rb_b = c.sb("b_rb", [128, 128], BF16, es_att)
    qs1 = [c.sb("b_qs%d" % i, [128, TL], BF16, es_att) for i in range(2)]

    c.dma(Gm[:], P["Gm"], writes=[Gm_b], q="pool")
    c.dma(mctn[:], P["mctn"], writes=[mctn_b], q="pool")
    c.dma(mcnt[:], P["mcnt"], writes=[mcnt_b], q="pool")
    c.dma(Arel[:], P["Arel"], writes=[Arel_b])
    c.dma(Brel[:], P["Brel"], writes=[Brel_b])
    c.dma(dz[:], P["dz"], writes=[dz_b], q="pool")
    c.dma(wz[:], P["wz"], writes=[wz_b], q="pool")
    c.dma(vzr[:], P["vzr"], writes=[vzr_b], q="pool")
    c.dma(ntri[:], P["ntri"], writes=[ntri_b], q="pool")
    c.dma(nones[:], P["nones"], writes=[nones_b], q="pool")
    c.dma(gates[:], P["gates"], writes=[gates_b])
    with nc.allow_non_contiguous_dma(reason="tiny one-off per-head scale table"):
        c.dma(ggT[:], P["ngrp"].rearrange("o (h d) -> d (o h)", d=128), writes=[ggT_b])
    c.op("dve", lambda e: e.memset(padbuf[:], 0.0), writes=[padbuf_b])

    es_c = ExitStack()
    w1 = [(KTr[1 + i][0][:].rearrange("p c t -> p (c t)").rearrange("p (l h) -> p l h", h=256), KTr[1 + i][1]) for i in range(2)]
    w2 = [c.sb("c_w2%d" % i, [128, 2, 128], BF16, es_c) for i in range(2)]
    cpos, cpos_b = c.sb("c_cpos", [32, 128], BF16, es_c)
    cposT, cposT_b = c.sb("c_cposT", [128, 32], BF16, es_c)
    b1, b1_b = c.sb("c_b1", [128, 4], F32, es_c)
    xs, xs_b = c.sb("c_xs", [128, 512], F32, es_c)
    uu, uu_b = c.sb("c_uu", [128, 512], F32, es_c)
    gT = [c.sb("c_gT%d" % i, [128, 512], BF16, es_c) for i in range(2)]
    ktok, ktok_b = c.sb("c_ktok", [128, 128], BF16, es_c)
    pci, pci_b = c.sb("c_pci", [128, 4], I32, es_c)
    pcf, pcf_b = c.sb("c_pcf", [128, 4], F32, es_c)
    invb, invb_b = c.sb("c_invf", [128, 16], F32, es_c)
    cang, cang_b = c.sb("c_ang", [128, 4, 16], F32, es_c)
    ccs, ccs_b = c.sb("c_cs", [128, 4, 16], F32, es_c)
    csn, csn_b = c.sb("c_sn", [128, 4, 16], F32, es_c)
    rri, rri_b = c.sb("c_rri", [128, 4, 16], I32, es_c)
    rrf, rrf_b = c.sb("c_rrf", [128, 4, 16], F32, es_c)
    rrx, rrx_b = c.sb("c_rrx", [128, 4, 16], F32, es_c)
    crt = [c.sb("c_rt%d" % i, [128, 16], F32, es_c) for i in range(4)]

    c.dma(w1[0][0], P["wck1"].rearrange("(l d) h -> d l h", d=128), writes=[w1[0][1]], q="pool")
    c.dma(w1[1][0], P["wcv1"].rearrange("(l d) h -> d l h", d=128), writes=[w1[1][1]], q="pool")
    c.dma(w2[0][0][:], P["wck2"].rearrange("(c p) d -> p c d", p=128), writes=[w2[0][1]], q="pool")
    c.dma(w2[1][0][:], P["wcv2"].rearrange("(c p) d -> p c d", p=128), writes=[w2[1][1]], q="pool")
    c.dma(cpos[:], P["cmp_pos"], writes=[cpos_b], q="pool")
    c.dma(pci[:], P["poscmp"], writes=[pci_b])
    c.dma(invb[:], P["invf"].broadcast_to([128, 16]), writes=[invb_b])
    c.op("dve", lambda e: e.tensor_copy(out=pcf[:], in_=pci[:]), reads=[pci_b], writes=[pcf_b])
    c.op("dve", lambda e: e.tensor_tensor(out=cang[:], in0=bc(pcf[:].unsqueeze(2), [128, 4, 16]),
                                          in1=bc(invb[:].unsqueeze(1), [128, 4, 16]), op=ALU.mult),
         reads=[pcf_b, invb_b], writes=[cang_b])
    emit_sin(c, csn, csn_b, cang, cang_b, 0.0, rri, rri_b, rrf, rrf_b, rrx, rrx_b)
    emit_sin(c, ccs, ccs_b, cang, cang_b, 0.5 * PI, rri, rri_b, rrf, rrf_b, rrx, rrx_b)
    ptb = kk.bank_bf16(7)
    c.op("pe", lambda e: e.transpose(out=ptb[:, 0:32], in_=cpos[:], identity=ident[0:32, 0:32]),
         reads=[cpos_b, ident_b], writes=[bankb[7]])
    c.op("dve", lambda e: e.tensor_copy(out=cposT[:], in_=ptb[:, 0:32]), reads=[bankb[7]], writes=[cposT_b])
    for X in range(2):
        for hc in range(2):
            for l in range(32):
                mm(bank[6][:, X * 2 + hc:X * 2 + hc + 1], w1[X][0][:, l, hc * 128:(hc + 1) * 128], cposT[:, l:l + 1],
                   (l == 0 and X == 0 and hc == 0), (l == 31), [w1[X][1], cposT_b], bankb[6])
    c.op("dve", lambda e: e.tensor_copy(out=b1[:], in_=bank[6][:, 0:4]), reads=[bankb[6]], writes=[b1_b])
    c.op("dve", lambda e: e.memset(gT[0][0][:], 0.0), writes=[gT[0][1]])
    c.op("dve", lambda e: e.memset(gT[1][0][:], 0.0), writes=[gT[1][1]])
    for g in range(2):
        c.op("pool", lambda e: e.memset(vcs[g][0][:, :, 128:129], 1.0), writes=[vcs[g][1]])
        c.op("pool", lambda e: e.memset(vcs[g][0][:, :, 129:130], 0.0), writes=[vcs[g][1]])

    kcg, kcg_b = KTr[0]
    kcg_flat = kcg[:].rearrange("p c t -> p (c t)")
    kcg_v = kcg[:].rearrange("p k (c t) -> p k c t", c=8)
    for X in range(2):
        for g in range(2):
            hh = 2 * X + g
            for cc in range(8):
                c.dma(kcg_v[:, :, cc, :], P["KTg"][cc, hh].rearrange("d (k t) -> d k t", k=8), writes=[kcg_b])
            for hc in range(2):
                bi = hc
                for l in range(32):
                    mm(bank[bi][:, 0:511], w1[X][0][:, l, hc * 128:(hc + 1) * 128],
                       kcg_flat[:, l:l + 16 * 510 + 1:16], (l == 0), (l == 31), [w1[X][1], kcg_b], bankb[bi])
                c.op("act", lambda e: e.activation(out=xs[:, 0:511], in_=bank[bi][:, 0:511], func=AF.Identity,
                                                   bias=b1[:, X * 2 + hc:X * 2 + hc + 1]),
                     reads=[bankb[bi], b1_b], writes=[xs_b])
                c.op("dve", lambda e: e.tensor_tensor(out=uu[:, 0:511], in0=xs[:, 0:511], in1=xs[:, 0:511], op=ALU.mult),
                     reads=[xs_b], writes=[uu_b])
                c.op("dve", lambda e: e.tensor_scalar(out=uu[:, 0:511], in0=uu[:, 0:511], scalar1=GELU_C1, scalar2=1.0,
                                                      op0=ALU.mult, op1=ALU.add), reads=[uu_b], writes=[uu_b])
                c.op("dve", lambda e: e.tensor_tensor(out=uu[:, 0:511], in0=uu[:, 0:511], in1=xs[:, 0:511], op=ALU.mult),
                     reads=[uu_b, xs_b], writes=[uu_b])
                c.op("act", lambda e: e.activation(out=uu[:, 0:511], in_=uu[:, 0:511], func=AF.Sigmoid, scale=2 * GELU_C0),
                     reads=[uu_b], writes=[uu_b])
                c.op("dve", lambda e: e.tensor_tensor(out=gT[hc][0][:, 0:511], in0=uu[:, 0:511], in1=xs[:, 0:511], op=ALU.mult),
                     reads=[uu_b, xs_b], writes=[gT[hc][1]])
            for nch in range(4):
                bi = 2 + nch % 2
                for hc in range(2):
                    mm(bank[bi][:, 0:128], gT[hc][0][:, nch * 128:(nch + 1) * 128], w2[X][0][:, hc, :],
                       (hc == 0), (hc == 1), [gT[hc][1], w2[X][1]], bankb[bi])
                if X == 1:
                    c.op("act", lambda e: e.copy(out=vcs[g][0][:, nch, 0:128], in_=bank[bi][:, 0:128]),
                         reads=[bankb[bi]], writes=[vcs[g][1]])
                else:
                    pt = bank[bi]
                    co = ccs[:, nch, :]
                    si = csn[:, nch, :]
                    t = [r[0][:] for r in crt]
                    tb = [r[1] for r in crt]
                    c.op("act", lambda e: e.copy(out=ktok[:, 32:128], in_=pt[:, 32:128]), reads=[bankb[bi]], writes=[ktok_b])
                    c.op("dve", lambda e: e.tensor_tensor(out=t[0], in0=pt[:, 0:16], in1=co, op=ALU.mult), reads=[bankb[bi], ccs_b], writes=[tb[0]])
                    c.op("dve", lambda e: e.tensor_tensor(out=t[1], in0=pt[:, 16:32], in1=si, op=ALU.mult), reads=[bankb[bi], csn_b], writes=[tb[1]])
                    c.op("dve", lambda e: e.tensor_tensor(out=t[2], in0=pt[:, 16:32], in1=co, op=ALU.mult), reads=[bankb[bi], ccs_b], writes=[tb[2]])
                    c.op("dve", lambda e: e.tensor_tensor(out=t[3], in0=pt[:, 0:16], in1=si, op=ALU.mult), reads=[bankb[bi], csn_b], writes=[tb[3]])
                    c.op("dve", lambda e: e.tensor_tensor(out=ktok[:, 0:16], in0=t[0], in1=t[1], op=ALU.subtract), reads=[tb[0], tb[1]], writes=[ktok_b])
                    c.op("dve", lambda e: e.tensor_tensor(out=ktok[:, 16:32], in0=t[2], in1=t[3], op=ALU.add), reads=[tb[2], tb[3]], writes=[ktok_b])
                    tbi = 6 + nch % 2
                    ptt = kk.bank_bf16(tbi)
                    c.op("pe", lambda e: e.transpose(out=ptt[:, 0:128], in_=ktok[:], identity=ident[:]),
                         reads=[ktok_b, ident_b], writes=[bankb[tbi]])
                    c.op("dve", lambda e: e.tensor_copy(out=kcT[g][0][:, nch * 128:(nch + 1) * 128], in_=ptt[:, 0:128]),
                         reads=[bankb[tbi]], writes=[kcT[g][1]])
    c.barrier()
    es_c.close()

    class Pipe:
        def __init__(self, ngroups):
            self.ng = ngroups
            self.items = []

        def add(self, groups):
            assert len(groups) == self.ng
            self.items.append(groups)

        def run(self):
            n = len(self.items)
            for step in range(n + self.ng - 1):
                for j in range(self.ng):
                    i = step - j
                    if 0 <= i < n and self.items[i][j] is not None:
                        self.items[i][j]()
            self.items = []

    rings = {}

    def ring(name, lst):
        i = rings.get(name, 0)
        rings[name] = i + 1
        return lst[i % len(lst)]

    sm_e, sm_e_b = c.sb("b_sme", [128, 4], F32, es_att)
    sm_n, sm_n_b = c.sb("b_smn", [128, 4], F32, es_att)
    pb3 = pb16 + [c.sb("b_p2", [128, 4, 128], BF16, es_att)]
    lb3 = lb16 + [c.sb("b_l2", [128, 4, 128], BF16, es_att)]
    rb3 = [(rb, rb_b)] + [c.sb("b_rb%d" % i, [128, 128], BF16, es_att) for i in range(1, 3)]
    selT2 = [(selT, selT_b), c.sb("b_selT1", [128, 128], BF16, es_att)]
    qs3 = qs1 + [c.sb("b_qs2", [128, TL], BF16, es_att)]

    def gate_ap(k, branch, h8):
        return gates[:, k, branch * 8 + h8:branch * 8 + h8 + 1]

    def evac_nsa(obanks, k, g, branch, first):
        for h in range(4):
            ob = obanks[h // 2]
            ov = bank[ob][:, 0:2 * DV].rearrange("p (h e) -> p h e", h=2)
            den = sm_e[:, h:h + 1]
            c.op("dve", lambda e: e.tensor_scalar_max(out=den, in0=ov[:, h % 2, 128:129], scalar1=1e-30),
                 reads=[bankb[ob]], writes=[sm_e_b])
            c.op("dve", lambda e: e.reciprocal(out=den, in_=den), reads=[sm_e_b], writes=[sm_e_b])
            c.op("dve", lambda e: e.tensor_tensor(out=den, in0=den, in1=gate_ap(k, branch, 4 * g + h), op=ALU.mult),
                 reads=[sm_e_b, gates_b], writes=[sm_e_b])
            if first:
                c.op("dve", lambda e: e.tensor_scalar_mul(out=oacc[:, h, :], in0=ov[:, h % 2, 0:128], scalar1=den),
                     reads=[bankb[ob], sm_e_b], writes=[oacc_b])
            else:
                c.op("dve", lambda e: e.scalar_tensor_tensor(out=oacc[:, h, :], in0=ov[:, h % 2, 0:128], scalar=den,
                                                             in1=oacc[:, h, :], op0=ALU.mult, op1=ALU.add),
                     reads=[bankb[ob], sm_e_b, oacc_b], writes=[oacc_b])

    def head_norm_T(src_ap_fn, src_b, nh, hh0, k):
        for h in range(nh):
            c.op("act", lambda e: e.activation(out=sqj[:], in_=src_ap_fn(h), func=AF.Square, accum_out=sm_n[:, h:h + 1]),
                 reads=[src_b], writes=[sqj_b, sm_n_b])
        c.op("act", lambda e: e.activation(out=sm_n[:, 0:nh], in_=sm_n[:, 0:nh], func=AF.Ln, scale=1.0 / HD, bias=EPS),
             reads=[sm_n_b], writes=[sm_n_b])
        c.op("act", lambda e: e.activation(out=sm_n[:, 0:nh], in_=sm_n[:, 0:nh], func=AF.Exp, scale=-0.5),
             reads=[sm_n_b], writes=[sm_n_b])
        for h in range(nh):
            c.op("dve", lambda e: e.tensor_scalar_mul(out=onb[:, h, :], in0=src_ap_fn(h), scalar1=sm_n[:, h:h + 1]),
                 reads=[src_b, sm_n_b], writes=[onb_b])
        tbi = 6 + (k % 2)
        ptt = kk.bank_bf16(tbi)
        for h in range(nh):
            c.op("pe", lambda e: e.transpose(out=ptt[:, h * 128:(h + 1) * 128], in_=onb[:, h, :], identity=ident[:]),
                 reads=[onb_b, ident_b], writes=[bankb[tbi]])
        c.op("dve", lambda e: e.tensor_tensor(out=onT[:, hh0:hh0 + nh, k * 128:(k + 1) * 128],
                                              in0=ptt[:, 0:nh * 128].rearrange("p (j t) -> p j t", j=nh),
                                              in1=bc(ggT[:, hh0:hh0 + nh].unsqueeze(2), [128, nh, 128]), op=ALU.mult),
             reads=[bankb[tbi], ggT_b], writes=[onT_b])

    ringkv = {"kt": 0, "v": 0}

    def next_kt():
        i = ringkv["kt"]
        ringkv["kt"] = (i + 1) % 3
        return KTr[i]

    def next_v():
        i = ringkv["v"]
        ringkv["v"] = (i + 1) % 3
        return Vr[i]

    def nsa_block_item(KT_ap, bias_list, V_ap, Qk, q_b, obanks, first, reads_kv, pre=None, post=None):
        sb_i = ring("S", [0, 1])
        pt_, pt_b = ring("P", pb3)
        n = len(bias_list)

        def g0():
            if pre is not None:
                pre()
            mm(bank[sb_i][:], KT_ap, Qk, True, n == 0, reads_kv + [q_b], bankb[sb_i])
            for bi_, (lhsT, rhs, rb_) in enumerate(bias_list):
                mm(bank[sb_i][:], lhsT, rhs, False, bi_ == n - 1, rb_, bankb[sb_i])
            c.op("act", lambda e: e.activation(out=pt_[:].rearrange("p h t -> p (h t)"), in_=bank[sb_i][:], func=AF.Exp),
                 reads=[bankb[sb_i]], writes=[pt_b])

        def g1():
            for h in range(4):
                ob = obanks[h // 2]
                mm(bank[ob][:, (h % 2) * DV:(h % 2 + 1) * DV], pt_[:, h, :], V_ap,
                   (first and h % 2 == 0), False, [pt_b] + reads_kv, bankb[ob])
            if post is not None:
                post()

        return [g0, g1]

    for g in range(2):
        KTs, KTs_b = next_kt()
        KTw, KTw_b = next_kt()
        Vs, Vs_b = next_v()
        Vw, Vw_b = next_v()
        c.dma(KTs[:], P["KTg"][:, 4 + g].rearrange("c d t -> d c t"), writes=[KTs_b])
        c.dma(Vs[:], P["Vg"][:, g].rearrange("c p k e -> p c k e"), writes=[Vs_b])
        c.dma(KTw[:], P["KTg"][:, 6 + g].rearrange("c d t -> d c t"), writes=[KTw_b])
        c.dma(Vw[:], P["Vg"][:, 2 + g].rearrange("c p k e -> p c k e"), writes=[Vw_b])
        qt, qt_b = qsb[0]
        c.dma(qt[:], P["QT"][4 * g:4 * g + 4].rearrange("h d t -> d h t"), writes=[qt_b])
        kct, kct_b = kcT[g]
        vct, vct_b = vcs[g]
        selinfo = {}

        def sel_a1(k):
            offc = 448 - 64 * k
            for h in range(4):
                xb = 6 + (h % 2)
                mm(bank[xb][:], qt[:, h, k * 128:(k + 1) * 128], kct[:], True, False, [qt_b, kct_b], bankb[xb])
                mm(bank[xb][:], ident[:], mctn[:, offc:offc + 512], False, True, [ident_b, mctn_b], bankb[xb])
                et, et_b = ef[h % 2]
                c.op("act", lambda e: e.activation(out=et[:], in_=bank[xb][:], func=AF.Exp, accum_out=sm[:, 4 + h:5 + h]),
                     reads=[bankb[xb]], writes=[et_b, sm_b])
                c.op("dve", lambda e: e.tensor_scalar_max(out=sm[:, 4 + h:5 + h], in0=sm[:, 4 + h:5 + h], scalar1=1e-30),
                     reads=[sm_b], writes=[sm_b])
                c.op("dve", lambda e: e.reciprocal(out=sm[:, 4 + h:5 + h], in_=sm[:, 4 + h:5 + h]), reads=[sm_b], writes=[sm_b])
                if h == 0:
                    c.op("dve", lambda e: e.tensor_scalar_mul(out=padbuf[:, 1:513], in0=et[:], scalar1=sm[:, 4:5]),
                         reads=[et_b, sm_b], writes=[padbuf_b])
                else:
                    c.op("dve", lambda e: e.scalar_tensor_tensor(out=padbuf[:, 1:513], in0=et[:], scalar=sm[:, 4 + h:5 + h],
                                                                 in1=padbuf[:, 1:513], op0=ALU.mult, op1=ALU.add),
                         reads=[et_b, sm_b, padbuf_b], writes=[padbuf_b])
            c.op("dve", lambda e: e.tensor_tensor(out=qi[:], in0=padbuf[:, 1:513], in1=padbuf[:, 0:512], op=ALU.add),
                 reads=[padbuf_b], writes=[qi_b])
            c.op("dve", lambda e: e.tensor_reduce(out=pslc[:], in_=qi[:].rearrange("p (j r) -> p j r", r=4),
                                                  axis=AX.X, op=ALU.add), reads=[qi_b], writes=[pslc_b])
            offa = 112 - 16 * k
            c.op("dve", lambda e: e.tensor_tensor(out=imp[:], in0=pslc[:], in1=Arel[:, offa:offa + 128], op=ALU.mult),
                 reads=[pslc_b, Arel_b], writes=[imp_b])
            c.op("dve", lambda e: e.tensor_tensor(out=imp[:], in0=imp[:], in1=Brel[:, offa:offa + 128], op=ALU.add),
                 reads=[imp_b, Brel_b], writes=[imp_b])
            c.op("dve", lambda e: e.memset(imp[:, 0:1], 1e4), writes=[imp_b])
            c.op("dve", lambda e: e.max(out=m8a[:], in_=imp[:]), reads=[imp_b], writes=[m8a_b])
            c.op("dve", lambda e: e.match_replace(out=imp2[:], in_to_replace=m8a[:], in_values=imp[:], imm_value=-1e9),
                 reads=[imp_b, m8a_b], writes=[imp2_b])
            c.op("dve", lambda e: e.max(out=m8b[:], in_=imp2[:]), reads=[imp2_b], writes=[m8b_b])
            c.op("dve", lambda e: e.tensor_single_scalar(out=sel2[:], in_=imp[:], scalar=-5000.0, op=ALU.is_gt),
                 reads=[imp_b], writes=[sel2_b])
            c.op("dve", lambda e: e.scalar_tensor_tensor(out=sel2[:], in0=imp[:], scalar=m8b[:, 7:8], in1=sel2[:],
                                                         op0=ALU.is_ge, op1=ALU.mult),
                 reads=[imp_b, m8b_b, sel2_b], writes=[sel2_b])
            c.op("dve", lambda e: e.tensor_scalar(out=selb[:], in0=sel2[:], scalar1=-1.0, scalar2=-NEG,
                                                  op0=ALU.add, op1=ALU.mult), reads=[sel2_b], writes=[selb_b])

        def sel_a2(k):
            st, st_b = selT2[k % 2]
            tbi = 6 + (k % 2)
            ptt = kk.bank_bf16(tbi)
            c.op("pe", lambda e: e.transpose(out=ptt[:, 0:128], in_=selb[:], identity=ident[:]),
                 reads=[selb_b, ident_b], writes=[bankb[tbi]])
            c.op("dve", lambda e: e.tensor_copy(out=st[:], in_=ptt[:, 0:128]), reads=[bankb[tbi]], writes=[st_b])

        pipe = Pipe(2)
        sel_a1(0)
        sel_a2(0)
        for k in range(NK):
            Qk = qt[:, :, k * 128:(k + 1) * 128]
            st, st_b = selT2[k % 2]
            selT4 = bc(st[:].unsqueeze(1), [128, 4, 128])
            ob = ring("O", [(2, 3), (4, 5)])
            nchs = k // 2 + 1
            for nch in range(nchs):
                dprime = 16 * nch - 8 * k
                bl = []
                if dprime in (-16, -8, 0):
                    idx = dprime // 8 + 2
                    bl.append((ident[:], bc(mcnt[:, idx, :].unsqueeze(1), [128, 4, 128]), [ident_b, mcnt_b]))
                post = (lambda ob=ob, k=k: evac_nsa(ob, k, g, 0, True)) if nch == nchs - 1 else None
                pipe.add(nsa_block_item(kct[:, nch * 128:(nch + 1) * 128], bl, vct[:, nch, :], Qk, qt_b, ob, nch == 0,
                                        [kct_b, vct_b], post=post))
            ob = ring("O", [(2, 3), (4, 5)])
            nkb = 8 * k + 8
            for kb in range(nkb):
                ck, kq = kb % 8, kb // 8
                bl = [(Gm[:, kb * 128:(kb + 1) * 128], selT4, [Gm_b, st_b])]
                if kb >= 8 * k:
                    bl.append((ident[:], bc(dz[:, kb - 8 * k, :].unsqueeze(1), [128, 4, 128]), [ident_b, dz_b]))
                pre = (lambda k=k: sel_a1(k + 1)) if (kb == min(3, nkb - 1) and k + 1 < NK) else None
                post = (lambda ob=ob, k=k: evac_nsa(ob, k, g, 1, False)) if kb == nkb - 1 else None
                pipe.add(nsa_block_item(KTs[:, ck, kq * 128:(kq + 1) * 128], bl, Vs[:, ck, kq, :], Qk, qt_b, ob, kb == 0,
                                        [KTs_b, Vs_b], pre=pre, post=post))
            ob = ring("O", [(2, 3), (4, 5)])
            rs = [r for r in range(12) if 8 * k - 4 + r >= 0]
            for r in rs:
                kb = 8 * k - 4 + r
                ck, kq = kb % 8, kb // 8
                bl = [(ident[:], bc(wz[:, r, :].unsqueeze(1), [128, 4, 128]), [ident_b, wz_b])]
                pre = (lambda k=k: sel_a2(k + 1)) if (r == rs[2] and k + 1 < NK) else None

                def post_w(ob=ob, k=k):
                    evac_nsa(ob, k, g, 2, False)
                    head_norm_T(lambda h: oacc[:, h, :], oacc_b, 4, 4 * g, k)

                pipe.add(nsa_block_item(KTw[:, ck, kq * 128:(kq + 1) * 128], bl, Vw[:, ck, kq, :], Qk, qt_b, ob, r == rs[0],
                                        [KTw_b, Vw_b], pre=pre, post=post_w if r == rs[-1] else None))
        pipe.run()

    sbkv = {}

    def sb_load(h):
        KTh, KTh_b = next_kt()
        Vh, Vh_b = next_v()
        qh, qh_b = qs3[h % 3]
        c.dma(KTh[:], P["KTg"][:, 8 + h].rearrange("c d t -> d c t"), writes=[KTh_b])
        c.dma(Vh[:], P["Vg"][:, 4 + h].rearrange("c p k e -> p c k e"), writes=[Vh_b])
        c.dma(qh[:], P["QT"][8 + h], writes=[qh_b])
        sbkv[h] = (KTh, KTh_b, Vh, Vh_b, qh, qh_b)

    def sb_item(h, k, ti, ntile, pre):
        KTh, KTh_b, Vh, Vh_b, qh, qh_b = sbkv[h]
        Qk = qh[:, k * 128:(k + 1) * 128]
        obank = 4 + (k % 2)
        kb_hi = 8 * k + 7 - 4 * ti
        zb = ring("Z", [0, 1, 2, 3])
        Z = bank[zb]
        Zv = Z[:, :].rearrange("p (u t) -> p u t", u=4)
        et, et_b = ring("E", ef)
        lt, lt_b = ring("L", lb3)
        at, at_b = ring("A", pb3)
        rbi = rings.get("RB", 0)
        rings["RB"] = rbi + 1
        rb_w, rb_w_b = rb3[rbi % 3]
        rb_r, rb_r_b = rb3[(rbi - 1) % 3]

        def g0():
            if pre is not None:
                pre()
            for u in range(4):
                kb = kb_hi - u
                ck, kq = kb % 8, kb // 8
                mm(Z[:, u * 128:(u + 1) * 128], KTh[:, ck, kq * 128:(kq + 1) * 128], Qk, (u == 0), False,
                   [KTh_b, qh_b], bankb[zb])
            c.op("act", lambda e: e.activation(out=et[:], in_=Z[:], func=AF.Exp), reads=[bankb[zb]], writes=[et_b])
            c.op("act", lambda e: e.activation(out=lt[:].rearrange("p u t -> p (u t)"), in_=et[:], func=AF.Ln, bias=1.0),
                 reads=[et_b], writes=[lt_b])
            if ti < 2:
                c.op("pool", lambda e: e.tensor_tensor(out=lt[:], in0=lt[:], in1=vzr[:, 4 * ti:4 * ti + 4, :], op=ALU.mult),
                     reads=[lt_b, vzr_b], writes=[lt_b])
            if ti < ntile - 1:
                c.op("dve", lambda e: e.tensor_reduce(out=rsum[:], in_=lt[:].rearrange("p u t -> p t u"),
                                                      axis=AX.X, op=ALU.add), reads=[lt_b], writes=[rsum_b])
                if ti == 0:
                    c.op("dve", lambda e: e.tensor_copy(out=r32[:], in_=rsum[:]), reads=[rsum_b], writes=[r32_b])
                else:
                    c.op("dve", lambda e: e.tensor_tensor(out=r32[:], in0=r32[:], in1=rsum[:], op=ALU.add),
                         reads=[r32_b, rsum_b], writes=[r32_b])
                c.op("dve", lambda e: e.tensor_copy(out=rb_w[:], in_=r32[:]), reads=[r32_b], writes=[rb_w_b])

        def g1():
            mm(Z[:], ntri[:], lt[:].rearrange("p u t -> p (u t)"), False, False, [ntri_b, lt_b], bankb[zb])
            for u2 in range(3):
                mm(Zv[:, u2 + 1:4, :], nones[:], bc(lt[:, u2, :].unsqueeze(1), [128, 3 - u2, 128]), False, False,
                   [nones_b, lt_b], bankb[zb])
            if ti > 0:
                mm(Zv, nones[:], bc(rb_r[:].unsqueeze(1), [128, 4, 128]), False, True, [nones_b, rb_r_b], bankb[zb])
            c.op("act", lambda e: e.activation(out=at[:].rearrange("p u t -> p (u t)"), in_=Z[:], func=AF.Exp),
                 reads=[bankb[zb]], writes=[at_b])
            if ti < 2:
                c.op("pool", lambda e: e.tensor_tensor(out=at[:], in0=at[:], in1=vzr[:, 4 * ti:4 * ti + 4, :], op=ALU.mult),
                     reads=[at_b, vzr_b], writes=[at_b])

        def g2():
            for u in range(4):
                kb = kb_hi - u
                ck, kq = kb % 8, kb // 8
                mm(bank[obank][:, 0:128], at[:, u, :], Vh[:, ck, kq, 0:128], (ti == 0 and u == 0),
                   (ti == ntile - 1 and u == 3), [at_b, Vh_b], bankb[obank])
            if ti == ntile - 1:
                head_norm_T(lambda hh_: bank[obank][:, 0:128], bankb[obank], 1, 8 + h, k)

        return [g0, g1, g2]

    sb_load(0)
    sb_load(1)
    items = []
    for h in range(8):
        for k in range(NK):
            ntile = 2 * k + 2
            for ti in range(ntile):
                pre = None
                if k == 0 and ti == 0 and h >= 1 and h + 1 < 8:
                    pre = (lambda h=h: sb_load(h + 1))
                items.append((h, k, ti, ntile, pre))
    n_it = len(items)
    built = {}

    def get(i):
        if i not in built:
            h, k, ti, ntile, pre = items[i]
            if h not in sbkv:
                sb_load(h)
            built[i] = sb_item(h, k, ti, ntile, pre)
        return built[i]

    for step in range(n_it + 2):
        for j in range(3):
            i = step - j
            if 0 <= i < n_it:
                get(i)[j]()
                if j == 2:
                    del built[i]

    if "dbg_onT" in P:
        c.dma(P["dbg_onT"], onT[:], reads=[onT_b])
    c.barrier()
    es_att.close()

    es_f = ExitStack()
    hres, hres_b = c.sb("f_h", [128, NK, D], F32, es_f)
    gv, gv_b = c.sb("f_gv", [128, D], F32, es_f)
    junk, junk_b = c.sb("f_junk", [128, D], F32, es_f)
    ss, ss_b = c.sb("f_ss", [128, 1], F32, es_f)
    xn, xn_b = c.sb("f_xn", [128, D], BF16, es_f)
    es_o = ExitStack()
    wo = [c.sb("f_wo%d" % i, [128, 16, 512], BF16, es_o) for i in range(2)]

    for k in range(NK):
        c.dma(hres[:, k, :], P["h"][k * 128:(k + 1) * 128, :], writes=[hres_b])
    for sl in range(4):
        wt, wt_b = wo[sl % 2]
        c.dma(wt[:], P["w_out"][:, sl * 512:(sl + 1) * 512].rearrange("(dc p) n -> p dc n", p=128), writes=[wt_b], q="pool")
        for k in range(NK):
            bi = k % 2
            for dc in range(16):
                mm(bank[bi][:], onT[:, dc, k * 128:(k + 1) * 128], wt[:, dc, :], dc == 0, dc == 15, [onT_b, wt_b], bankb[bi])
            c.op("dve", lambda e: e.tensor_tensor(out=hres[:, k, sl * 512:(sl + 1) * 512], in0=bank[bi][:],
                                                  in1=hres[:, k, sl * 512:(sl + 1) * 512], op=ALU.add),
                 reads=[bankb[bi], hres_b], writes=[hres_b])
    c.barrier()
    es_o.close()
    wg = [c.sb("f_wg%d" % i, [128, 16, 256], BF16, es_f) for i in range(2)]
    wu = [c.sb("f_wu%d" % i, [128, 16, 256], BF16, es_f) for i in range(2)]
    wd = [c.sb("f_wd%d" % i, [128, 2, D], BF16, es_f) for i in range(2)]
    mid = [c.sb("f_mid%d" % i, [128, 2, TL], BF16, es_f) for i in range(2)]
    sg = [c.sb("f_sg%d" % i, [128, 512], F32, es_f) for i in range(2)]
    c.dma(gv[:], P["nffn"].broadcast_to([128, D]), writes=[gv_b])
    tmp_pool = (junk, junk_b, ss, ss_b, xn, xn_b)
    xT, xT_b = onT, onT_b
    for k in range(NK):
        emit_rmsnorm_T(kk, hres[:, k, :], hres_b, gv, gv_b, xT, xT_b, k, tmp_pool, (6, 7))
    NSL = D_FF // 256
    for sl in range(NSL):
        wgt, wgt_b = wg[sl % 2]
        wut, wut_b = wu[sl % 2]
        wdt, wdt_b = wd[sl % 2]
        mt, mt_b = mid[sl % 2]
        f0 = sl * 256
        c.dma(wgt[:], P["w_gate"][:, f0:f0 + 256].rearrange("(dc p) n -> p dc n", p=128), writes=[wgt_b], q="pool")
        c.dma(wut[:], P["w_up"][:, f0:f0 + 256].rearrange("(dc p) n -> p dc n", p=128), writes=[wut_b], q="pool")
        c.dma(wdt[:], P["w_down"][f0:f0 + 256, :].rearrange("(fc p) n -> p fc n", p=128), writes=[wdt_b], q="pool")
        for fc in range(2):
            for th in range(2):
                ba, bb = 2 * th, 2 * th + 1
                for dc in range(16):
                    mm(bank[ba][:], wgt[:, dc, fc * 128:(fc + 1) * 128], xT[:, dc, th * 512:(th + 1) * 512],
                       dc == 0, dc == 15, [wgt_b, xT_b], bankb[ba])
                for dc in range(16):
                    mm(bank[bb][:], wut[:, dc, fc * 128:(fc + 1) * 128], xT[:, dc, th * 512:(th + 1) * 512],
                       dc == 0, dc == 15, [wut_b, xT_b], bankb[bb])
                sgt, sgt_b = sg[th]
                c.op("act", lambda e: e.activation(out=sgt[:], in_=bank[ba][:], func=AF.Silu), reads=[bankb[ba]], writes=[sgt_b])
                c.op("dve", lambda e: e.tensor_tensor(out=mt[:, fc, th * 512:(th + 1) * 512], in0=bank[bb][:], in1=sgt[:],
                                                      op=ALU.mult), reads=[bankb[bb], sgt_b], writes=[mt_b])
        for k in range(NK):
            for ct in range(4):
                bi = 4 + (ct % 2) + 2 * (k % 2)
                for fc in range(2):
                    mm(bank[bi][:], mt[:, fc, k * 128:(k + 1) * 128], wdt[:, fc, ct * 512:(ct + 1) * 512],
                       fc == 0, fc == 1, [mt_b, wdt_b], bankb[bi])
                c.op("dve", lambda e: e.tensor_tensor(out=hres[:, k, ct * 512:(ct + 1) * 512], in0=bank[bi][:],
                                                      in1=hres[:, k, ct * 512:(ct + 1) * 512], op=ALU.add),
                     reads=[bankb[bi], hres_b], writes=[hres_b])
    c.dma(gv[:], P["nfinal"].broadcast_to([128, D]), reads=[], writes=[gv_b])
    for k in range(NK):
        c.dma(P["h_out"][k * 128:(k + 1) * 128, :], hres[:, k, :], reads=[hres_b])
        c.op("act", lambda e: e.activation(out=junk[:], in_=hres[:, k, :], func=AF.Square, accum_out=ss[:]),
             reads=[hres_b], writes=[junk_b, ss_b])
        c.op("act", lambda e: e.activation(out=ss[:], in_=ss[:], func=AF.Ln, scale=1.0 / D, bias=EPS), reads=[ss_b], writes=[ss_b])
        c.op("act", lambda e: e.activation(out=ss[:], in_=ss[:], func=AF.Exp, scale=-0.5), reads=[ss_b], writes=[ss_b])
        c.op("dve", lambda e: e.scalar_tensor_tensor(out=junk[:], in0=hres[:, k, :], scalar=ss[:, 0:1], in1=gv[:],
                                                     op0=ALU.mult, op1=ALU.mult),
             reads=[hres_b, ss_b, gv_b], writes=[junk_b])
        c.dma(P["hn_out"][k * 128:(k + 1) * 128, :], junk[:], reads=[junk_b])
    c.barrier()
    es_f.close()
    es_all.close()


def build_pb(with_pa=False):
    nc = bass.Bass("TRN2", target_bir_lowering=False)

    def din(name, shape, dt=F32):
        return nc.dram_tensor(name, list(shape), dt, kind="ExternalInput").ap()

    def dout(name, shape, dt=F32):
        return nc.dram_tensor(name, list(shape), dt, kind="ExternalOutput").ap()

    P = {
        "h": din("h", [TL, D]), "QT": din("QT", [16, 128, TL], BF16), "gates": din("gates", [128, NK, 24]),
        "KTg": din("KTg", [8, 16, 128, TL], BF16), "Vg": din("Vg", [8, 12, 128, NK, DV], BF16),
        "wck1": din("wck1", [4096, 256]), "wck2": din("wck2", [256, 128]),
        "wcv1": din("wcv1", [4096, 256]), "wcv2": din("wcv2", [256, 128]),
        "cmp_pos": din("cmp_pos", [32, 128]), "poscmp": din("poscmp", [128, 4], I32), "invf": din("invf", [1, 16]),
        "ngrp": din("ngrp", [1, D]), "w_out": din("w_out", [D, D]), "nffn": din("nffn", [1, D]),
        "w_gate": din("w_gate", [D, D_FF]), "w_up": din("w_up", [D, D_FF]), "w_down": din("w_down", [D_FF, D]),
        "nfinal": din("nfinal", [1, D]), "ident": din("ident", [128, 128]),
        "Gm": din("Gm", [128, 64 * 128]), "mctn": din("mctn", [128, 960]), "mcnt": din("mcnt", [128, 3, 128]),
        "Arel": din("Arel", [128, 240]), "Brel": din("Brel", [128, 240]),
        "dz": din("dz", [128, 8, 128]), "wz": din("wz", [128, 12, 128]), "vzr": din("vzr", [128, 8, 128]),
        "ntri": din("ntri", [128, 128]), "nones": din("nones", [128, 128]),
        "h_out": dout("h_out", [TL, D]), "hn_out": dout("hn_out", [TL, D]),
    }
    if DEBUG:
        P["dbg_onT"] = dout("dbg_onT", [128, 16, TL], BF16)
    if with_pa:
        A = {
            "w_in": din("w_in", [D, D_IN]), "nmix": din("nmix", [1, D]), "bg": din("bg", [1, 24]),
            "pos": din("pos", [128, NK], I32),
            "QT_o": dout("QT_o", [16, 128, TL], BF16), "KT_o": dout("KT_o", [16, 128, TL], BF16),
            "V_o": dout("V_o", [12, 128, NK, DV], BF16), "gates_o": dout("gates_o", [128, NK, 24]),
        }
    kk = K(nc)
    kk.load_ident(P["ident"])
    emit_phase_b(kk, P)
    if with_pa:
        emit_phase_a(kk, P["h_out"], A["w_in"], A["nmix"], A["bg"], A["pos"], P["invf"],
                     A["QT_o"], A["KT_o"], A["V_o"], A["gates_o"])
    kk.c.finish()
    return nc


def make_consts(cidx):
    p = np.arange(128)
    out = {}
    u = np.arange(64 * 128)
    out["Gm"] = (np.arange(128)[:, None] == (u[None, :] // 64)).astype(np.float32)
    idx = np.arange(960)
    m = idx - 448 - 8 * cidx
    out["mctn"] = np.where(16 * m[None, :] + 31 <= p[:, None], 0.0, NEG).astype(np.float32)
    mcnt = np.zeros((128, 3, 128), np.float32)
    for i, dp in enumerate((-16, -8, 0)):
        dd = dp - cidx
        ok = (16 * p[:, None] + 31 + 128 * dd) <= p[None, :]
        mcnt[:, i, :] = np.where(ok, 0.0, NEG)
    out["mcnt"] = mcnt
    jr = np.arange(240) - 112
    bt = 2 * cidx + (p >= 64).astype(np.int64)
    causal = jr[None, :] <= bt[:, None]
    forced = (jr[None, :] == bt[:, None]) | (jr[None, :] == bt[:, None] - 1)
    out["Arel"] = (causal & ~forced).astype(np.float32)
    out["Brel"] = np.where(~causal, -1e4, np.where(forced, 1e4, 0.0)).astype(np.float32)
    dzt = np.zeros((128, 8, 128), np.float32)
    dzt[:, cidx, :] = np.where(p[:, None] <= p[None, :], 0.0, NEG)
    out["dz"] = dzt
    wzt = np.zeros((128, 12, 128), np.float32)
    for r in range(12):
        diff = 128 * (cidx + 4 - r) + p[None, :] - p[:, None]
        wzt[:, r, :] = np.where((diff >= 0) & (diff < 512), 0.0, NEG)
    out["wz"] = wzt
    vz = np.zeros((128, 8, 128), np.float32)
    for r in range(8):
        if r < cidx:
            vz[:, 7 - r, :] = 1.0
        elif r == cidx:
            vz[:, 7 - r, :] = (p[:, None] < p[None, :]).astype(np.float32)
    out["vzr"] = vz
    out["ntri"] = -(p[:, None] >= p[None, :]).astype(np.float32)
    out["nones"] = -np.ones((128, 128), np.float32)
    out["ident"] = np.eye(128, dtype=np.float32)
    out["invf"] = _inv_freq()
    return out


_CONSTS = {}


def run_pb(h_parts, pa_res, positions, W, l, pos_parts=None, with_pa=False):
    key = "pba" if with_pa else "pb"
    if key not in _PROG:
        _PROG[key] = build_pb(with_pa)
    nc = _PROG[key]
    KTg = np.stack([np.asarray(pa_res[c]["KT"]) for c in range(NCORES)])
    Vg = np.stack([np.asarray(pa_res[c]["V"]) for c in range(NCORES)])
    cmp_end = np.arange(511) * 16 + 31
    pc = np.zeros(512, np.int32)
    pc[:511] = positions[cmp_end]
    poscmp = np.ascontiguousarray(pc.reshape(4, 128).T)
    in_maps = []
    for c in range(NCORES):
        if c not in _CONSTS:
            _CONSTS[c] = make_consts(c)
        m = dict(_CONSTS[c])
        m.update({
            "h": h_parts[c], "QT": np.asarray(pa_res[c]["QT"]), "gates": np.asarray(pa_res[c]["gates"]),
            "KTg": KTg, "Vg": Vg,
            "wck1": W["w_cmp_k1"][l], "wck2": W["w_cmp_k2"][l], "wcv1": W["w_cmp_v1"][l], "wcv2": W["w_cmp_v2"][l],
            "cmp_pos": W["cmp_pos"][l], "poscmp": poscmp,
            "ngrp": W["norm_grp"][l][None, :], "w_out": W["w_out"][l], "nffn": W["norm_ffn"][l][None, :],
            "w_gate": W["w_gate"][l], "w_up": W["w_up"][l], "w_down": W["w_down"][l],
            "nfinal": W["norm_final"][None, :],
        })
        if with_pa:
            m.update({"w_in": W["w_in"][l + 1], "nmix": W["norm_mix"][l + 1][None, :], "bg": W["b_gate"][l + 1][None, :],
                      "pos": np.ascontiguousarray(pos_parts[c].reshape(NK, 128).T)})
        in_maps.append(m)
    res = run_bass_kernel_spmd(nc, in_maps, core_ids=list(range(NCORES)))
    return res.results


def kernel(**inputs):
    W = {k: np.asarray(v) for k, v in inputs.items()}
    x = W["x"][0]
    positions = W["positions"][0]
    h_parts = shard_tokens(np.ascontiguousarray(x, dtype=np.float32))
    pos_parts = shard_tokens(positions.astype(np.int32))
    hn = None
    pa = run_pa(h_parts, pos_parts, W["w_in"][0], W["norm_mix"][0], W["b_gate"][0])
    for l in range(4):
        last = (l == 3)
        pb = run_pb(h_parts, pa, positions.astype(np.int32), W, l, pos_parts, with_pa=not last)
        h_parts = [np.asarray(pb[c]["h_out"]) for c in range(NCORES)]
        hn = [np.asarray(pb[c]["hn_out"]) for c in range(NCORES)]
        if not last:
            pa = [{"QT": pb[c]["QT_o"], "KT": pb[c]["KT_o"], "V": pb[c]["V_o"], "gates": pb[c]["gates_o"]}
                  for c in range(NCORES)]
    out = unshard_tokens(hn)
    return out[None].astype(np.float32)
```

```python
from contextlib import ExitStack
import numpy as np
import ml_dtypes
import concourse.bass as bass
import concourse.mybir as mybir
from concourse.bass_utils import run_bass_kernel_spmd

F32 = mybir.dt.float32
BF16 = mybir.dt.bfloat16
I32 = mybir.dt.int32
ALU = mybir.AluOpType
AF = mybir.ActivationFunctionType
AX = mybir.AxisListType

NCORES = 8
D = 2048
S = 8192
TL = 1024
NK = 8
D_IN = 5656
D_FF = 5632
HD = 128
SCALE = HD ** -0.5
NEG = -30000.0
DV = 130
EPS = 1e-6
N_DMA_SEMS = 24
PI = float(np.pi)


class Buf:
    __slots__ = ("name", "w", "r", "psum")

    def __init__(self, name, psum=False):
        self.name = name
        self.w = None
        self.r = {}
        self.psum = psum


class Ctx:
    def __init__(self, nc):
        self.nc = nc
        self.es = ExitStack()
        self.eng = {"pe": nc.tensor, "act": nc.scalar, "dve": nc.vector,
                    "pool": nc.gpsimd, "sp": nc.sync}
        self.sems = {}
        self.cnt = {}
        self.seen = {k: {} for k in self.eng}
        for k in self.eng:
            self.sems[k] = self.es.enter_context(nc.semaphore("s_" + k))
            self.cnt[k] = 0
        for i in range(N_DMA_SEMS):
            k = "d%d" % i
            self.sems[k] = self.es.enter_context(nc.semaphore("s_" + k))
            self.cnt[k] = 0
        self.dma_rr = 0
        self.n_inst = 0

    def sb(self, name, shape, dt, es=None):
        t = (es or self.es).enter_context(self.nc.sbuf_tensor(name, list(shape), dt))
        return t, Buf(name)

    def ps(self, name, shape, dt=F32):
        t = self.es.enter_context(self.nc.psum_tensor(name, list(shape), dt))
        return t, Buf(name, psum=True)

    def _need(self, tok, needs):
        if tok is None:
            return
        sk, v = tok
        if needs.get(sk, 0) < v:
            needs[sk] = v

    def _deps(self, ek, reads, writes):
        needs = {}
        for b in reads:
            self._need(b.w, needs)
            if b.psum:
                for sk, v in b.r.items():
                    self._need((sk, v), needs)
        for b in writes:
            self._need(b.w, needs)
            for sk, v in b.r.items():
                self._need((sk, v), needs)
        e = self.eng[ek]
        seen = self.seen[ek]
        for sk, v in needs.items():
            if sk == ek and ek == "pe":
                continue
            if seen.get(sk, 0) < v:
                e.wait_ge(self.sems[sk], v)
                seen[sk] = v

    def _mark(self, tok, reads, writes):
        sk, v = tok
        for b in reads:
            if b.psum:
                b.w = tok
                b.r = {}
            elif b.r.get(sk, 0) < v:
                b.r[sk] = v
        for b in writes:
            b.w = tok
            b.r = {}

    def op(self, ek, fn, reads=(), writes=()):
        self._deps(ek, reads, writes)
        ins = fn(self.eng[ek])
        self.cnt[ek] += 1
        ins.then_inc(self.sems[ek], 1)
        tok = (ek, self.cnt[ek])
        self._mark(tok, reads, writes)
        self.n_inst += 1
        return tok

    def dma(self, out, in_, reads=(), writes=(), q="sp"):
        dk = "d%d" % self.dma_rr
        self.dma_rr = (self.dma_rr + 1) % N_DMA_SEMS
        prev = self.cnt[dk]
        e = self.eng[q]
        if prev > 0 and self.seen[q].get(dk, 0) < prev:
            e.wait_ge(self.sems[dk], prev)
            self.seen[q][dk] = prev
        self._deps(q, reads, writes)
        ins = e.dma_start(out=out, in_=in_)
        self.cnt[dk] += 16
        ins.then_inc(self.sems[dk], 16)
        tok = (dk, self.cnt[dk])
        self._mark(tok, reads, writes)
        self.n_inst += 1
        return tok

    def coll_allgather(self, in_ap, out_ap, reads=(), writes=()):
        dk = "d%d" % self.dma_rr
        self.dma_rr = (self.dma_rr + 1) % N_DMA_SEMS
        prev = self.cnt[dk]
        e = self.eng["pool"]
        if prev > 0 and self.seen["pool"].get(dk, 0) < prev:
            e.wait_ge(self.sems[dk], prev)
            self.seen["pool"][dk] = prev
        self._deps("pool", reads, writes)
        ins = e.collective_compute("AllGather", ALU.bypass, replica_groups=[list(range(NCORES))],
                                   ins=[in_ap], outs=[out_ap])
        self.cnt[dk] += 16
        ins.then_inc(self.sems[dk], 16)
        tok = (dk, self.cnt[dk])
        self._mark(tok, reads, writes)
        self.n_inst += 1
        return tok

    def wait_tok(self, ek, tok):
        sk, v = tok
        if self.seen[ek].get(sk, 0) < v:
            self.eng[ek].wait_ge(self.sems[sk], v)
            self.seen[ek][sk] = v

    def barrier(self):
        for ek in self.eng:
            for sk, v in self.cnt.items():
                if v > 0 and sk != ek:
                    self.wait_tok(ek, (sk, v))

    def finish(self):
        for sk, v in self.cnt.items():
            if v > 0 and sk != "sp":
                self.wait_tok("sp", (sk, v))
        self.es.close()


def bc(ap, shape):
    return ap.to_broadcast(list(shape))


class K:
    def __init__(self, nc):
        self.nc = nc
        self.c = Ctx(nc)
        c = self.c
        self.bank = []
        self.bankb = []
        self.pair = []
        for i in range(4):
            t, _ = c.ps("bankpair%d" % i, [128, 1024], F32)
            self.pair.append(t)
            for hf in range(2):
                self.bank.append(t[:, hf * 512:(hf + 1) * 512])
                self.bankb.append(Buf("bank%d" % (2 * i + hf), psum=True))
        self.ident, self.ident_b = c.sb("ident_sb", [128, 128], BF16)

    def bank_bf16(self, i):
        return self.bank[i][:].bitcast(BF16)

    def load_ident(self, ident_dram):
        self.c.dma(self.ident[:], ident_dram, writes=[self.ident_b], q="pool")


def emit_rmsnorm_T(kk, h_ap, h_b, gvec, gvec_b, xT, xT_b, kblk, tmp_pool, tbank):
    c = kk.c
    junk, junk_b, ss, ss_b, xn, xn_b = tmp_pool
    c.op("act", lambda e: e.activation(out=junk[:], in_=h_ap, func=AF.Square, accum_out=ss[:]),
         reads=[h_b], writes=[junk_b, ss_b])
    c.op("act", lambda e: e.activation(out=ss[:], in_=ss[:], func=AF.Ln, scale=1.0 / D, bias=EPS),
         reads=[ss_b], writes=[ss_b])
    c.op("act", lambda e: e.activation(out=ss[:], in_=ss[:], func=AF.Exp, scale=-0.5),
         reads=[ss_b], writes=[ss_b])
    c.op("dve", lambda e: e.scalar_tensor_tensor(out=xn[:], in0=h_ap, scalar=ss[:, 0:1], in1=gvec[:],
                                                 op0=ALU.mult, op1=ALU.mult),
         reads=[h_b, ss_b, gvec_b], writes=[xn_b])
    for grp in range(4):
        bi = tbank[grp % 2]
        pt = kk.bank_bf16(bi)
        for j in range(4):
            dc = grp * 4 + j
            c.op("pe", lambda e: e.transpose(out=pt[:, j * 128:(j + 1) * 128],
                                             in_=xn[:, dc * 128:(dc + 1) * 128], identity=kk.ident[:]),
                 reads=[xn_b, kk.ident_b], writes=[kk.bankb[bi]])
        eng = "dve" if grp % 2 == 0 else "act"
        src = pt[:, 0:512].rearrange("p (j t) -> p j t", j=4)
        dst = xT[:, grp * 4:(grp + 1) * 4, kblk * 128:(kblk + 1) * 128]
        if eng == "dve":
            c.op("dve", lambda e: e.tensor_copy(out=dst, in_=src), reads=[kk.bankb[bi]], writes=[xT_b])
        else:
            c.op("act", lambda e: e.copy(out=dst, in_=src), reads=[kk.bankb[bi]], writes=[xT_b])


def emit_sin(c, dst, dst_b, ang, ang_b, shift, ri, ri_b, rf, rf_b, rx, rx_b):
    c.op("dve", lambda e: e.tensor_scalar_add(out=rx[:], in0=ang[:], scalar1=shift), reads=[ang_b], writes=[rx_b])
    c.op("dve", lambda e: e.tensor_scalar_mul(out=rf[:], in0=rx[:], scalar1=1.0 / (2 * PI)), reads=[rx_b], writes=[rf_b])
    c.op("dve", lambda e: e.tensor_copy(out=ri[:], in_=rf[:]), reads=[rf_b], writes=[ri_b])
    c.op("dve", lambda e: e.tensor_copy(out=rf[:], in_=ri[:]), reads=[ri_b], writes=[rf_b])
    c.op("dve", lambda e: e.scalar_tensor_tensor(out=rx[:], in0=rf[:], scalar=-2 * PI, in1=rx[:],
                                                 op0=ALU.mult, op1=ALU.add), reads=[rf_b, rx_b], writes=[rx_b])
    c.op("dve", lambda e: e.tensor_single_scalar(out=rf[:], in_=rx[:], scalar=PI, op=ALU.is_gt), reads=[rx_b], writes=[rf_b])
    c.op("dve", lambda e: e.scalar_tensor_tensor(out=rx[:], in0=rf[:], scalar=-2 * PI, in1=rx[:],
                                                 op0=ALU.mult, op1=ALU.add), reads=[rf_b, rx_b], writes=[rx_b])
    c.op("dve", lambda e: e.tensor_scalar(out=rx[:], in0=rx[:], scalar1=-3.141592, scalar2=3.141592,
                                          op0=ALU.max, op1=ALU.min), reads=[rx_b], writes=[rx_b])
    c.op("act", lambda e: e.activation(out=dst[:], in_=rx[:], func=AF.Sin), reads=[rx_b], writes=[dst_b])

C_QN, C_KC, C_VC, C_KS, C_VS, C_KW, C_VW, C_G, C_SQ, C_SK, C_SV = (
    0, 1024, 1280, 1536, 1792, 2048, 2304, 2560, 2584, 3608, 4632)
PA_SLABS = [
    ("qn", 0, 512), ("qn", 512, 512), ("kcvc", 1024, 512), ("ksvs", 1536, 512),
    ("kwvw", 2048, 512), ("g", 2560, 24), ("sq", 2584, 512), ("sq", 3096, 512),
    ("sk", 3608, 512), ("sk", 4120, 512), ("sv", 4632, 512), ("sv", 5144, 512),
]


def emit_phase_a(kk, h_src, w_in, nmix, bg, pos, invf, QT, KT, V, gates_out, h_resident=None):
    c = kk.c
    nc = kk.nc
    es = ExitStack()
    xT, xT_b = c.sb("a_xT", [128, 16, TL], BF16, es)
    gvec, gvec_b = c.sb("a_gvec", [128, D], F32, es)
    junk, junk_b = c.sb("a_junk", [128, D], F32, es)
    ss, ss_b = c.sb("a_ss", [128, 1], F32, es)
    xn, xn_b = c.sb("a_xn", [128, D], BF16, es)
    hblk = [c.sb("a_h%d" % i, [128, D], F32, es) for i in range(2)]
    wsl = [c.sb("a_w%d" % i, [128, 16, 512], BF16, es) for i in range(2)]
    ev = [c.sb("a_ev%d" % i, [128, 4, 128], BF16, es) for i in range(2)]
    tst = [c.sb("a_ts%d" % i, [128, 4, TL], BF16, es) for i in range(2)]
    vst = [c.sb("a_vs%d" % i, [128, 4, NK, DV], BF16, es) for i in range(2)]
    gsb, gsb_b = c.sb("a_gates", [128, NK, 24], F32, es)
    bgb, bgb_b = c.sb("a_bg", [128, 24], F32, es)
    posi, posi_b = c.sb("a_posi", [128, NK], I32, es)
    posf, posf_b = c.sb("a_posf", [128, NK], F32, es)
    invb, invb_b = c.sb("a_invf", [128, 16], F32, es)
    ang, ang_b = c.sb("a_ang", [128, NK, 16], F32, es)
    cs, cs_b = c.sb("a_cs", [128, NK, 16], F32, es)
    sn, sn_b = c.sb("a_sn", [128, NK, 16], F32, es)
    csq, csq_b = c.sb("a_csq", [128, NK, 16], F32, es)
    snq, snq_b = c.sb("a_snq", [128, NK, 16], F32, es)
    rt = [c.sb("a_rt%d" % i, [128, 4, 16], F32, es) for i in range(4)]
    gtmp, gtmp_b = c.sb("a_gtmp", [128, 24], F32, es)

    c.dma(gvec[:], nmix.broadcast_to([128, D]), writes=[gvec_b])
    c.dma(bgb[:], bg.broadcast_to([128, 24]), writes=[bgb_b])
    c.dma(invb[:], invf.broadcast_to([128, 16]), writes=[invb_b])
    c.dma(posi[:], pos, writes=[posi_b])
    for i in range(2):
        c.op("pool", lambda e: e.memset(vst[i][0][:, :, :, 128:129], 1.0), writes=[vst[i][1]])
        c.op("pool", lambda e: e.memset(vst[i][0][:, :, :, 129:130], 0.0), writes=[vst[i][1]])

    c.op("dve", lambda e: e.tensor_copy(out=posf[:], in_=posi[:]), reads=[posi_b], writes=[posf_b])
    c.op("dve", lambda e: e.tensor_tensor(out=ang[:], in0=bc(posf[:].unsqueeze(2), [128, NK, 16]),
                                          in1=bc(invb[:].unsqueeze(1), [128, NK, 16]), op=ALU.mult),
         reads=[posf_b, invb_b], writes=[ang_b])
    rr_i, rr_ib = c.sb("a_rri", [128, NK, 16], I32, es)
    rr_f, rr_fb = c.sb("a_rrf", [128, NK, 16], F32, es)
    rr_x, rr_xb = c.sb("a_rrx", [128, NK, 16], F32, es)
    for (dst, dst_b, shift) in ((sn, sn_b, 0.0), (cs, cs_b, 0.5 * PI)):
        emit_sin(c, dst, dst_b, ang, ang_b, shift, rr_i, rr_ib, rr_f, rr_fb, rr_x, rr_xb)
    c.op("dve", lambda e: e.tensor_scalar_mul(out=csq[:], in0=cs[:], scalar1=SCALE), reads=[cs_b], writes=[csq_b])
    c.op("dve", lambda e: e.tensor_scalar_mul(out=snq[:], in0=sn[:], scalar1=SCALE), reads=[sn_b], writes=[snq_b])

    xT_bk = [Buf("a_xT_k%d" % i) for i in range(NK)]
    tmp_pool = (junk, junk_b, ss, ss_b, xn, xn_b)
    for k in range(NK):
        if h_resident is None:
            ht, ht_b = hblk[k % 2]
            c.dma(ht[:], h_src[k * 128:(k + 1) * 128, :], writes=[ht_b])
            h_ap = ht[:]
        else:
            ht_b = h_resident[1]
            h_ap = h_resident[0][:, k, :]
        emit_rmsnorm_T(kk, h_ap, ht_b, gvec, gvec_b, xT, xT_bk[k], k, tmp_pool, (6, 7))

    def rope(pt, dst, dst_b, h0, nh, cosb, sinb, tabs_b, k, pb):
        co = bc(cosb[:, k, :].unsqueeze(1), [128, nh, 16])
        si = bc(sinb[:, k, :].unsqueeze(1), [128, nh, 16])
        x1 = pt[:, h0:h0 + nh, 0:16]
        x2 = pt[:, h0:h0 + nh, 16:32]
        t = [r[0][:, 0:nh, :] for r in rt]
        tb = [r[1] for r in rt]
        c.op("dve", lambda e: e.tensor_tensor(out=t[0], in0=x1, in1=co, op=ALU.mult), reads=[pb] + tabs_b, writes=[tb[0]])
        c.op("dve", lambda e: e.tensor_tensor(out=t[1], in0=x2, in1=si, op=ALU.mult), reads=[pb] + tabs_b, writes=[tb[1]])
        c.op("dve", lambda e: e.tensor_tensor(out=t[2], in0=x2, in1=co, op=ALU.mult), reads=[pb] + tabs_b, writes=[tb[2]])
        c.op("dve", lambda e: e.tensor_tensor(out=t[3], in0=x1, in1=si, op=ALU.mult), reads=[pb] + tabs_b, writes=[tb[3]])
        c.op("dve", lambda e: e.tensor_tensor(out=dst[:, h0:h0 + nh, 0:16], in0=t[0], in1=t[1], op=ALU.subtract),
             reads=[tb[0], tb[1]], writes=[dst_b])
        c.op("dve", lambda e: e.tensor_tensor(out=dst[:, h0:h0 + nh, 16:32], in0=t[2], in1=t[3], op=ALU.add),
             reads=[tb[2], tb[3]], writes=[dst_b])

    n_t = 0
    n_v = 0
    for si, (typ, c0, ncol) in enumerate(PA_SLABS):
        wt, wt_b = wsl[si % 2]
        c.dma(wt[:, :, 0:ncol], w_in[:, c0:c0 + ncol].rearrange("(dc p) n -> p dc n", p=128),
              writes=[wt_b], q="pool")
        uses_t = typ in ("qn", "kcvc", "ksvs", "kwvw", "sq", "sk")
        uses_v = typ in ("ksvs", "kwvw", "sv")
        if uses_t:
            ts, ts_b = tst[n_t % 2]
            n_t += 1
        if uses_v:
            vs, vs_b = vst[n_v % 2]
            n_v += 1
        for k in range(NK):
            bi = k % 2
            pb = kk.bankb[bi]
            pfull = kk.bank[bi]
            for dc in range(16):
                c.op("pe", lambda e: e.matmul(pfull[:, 0:ncol], lhsT=xT[:, dc, k * 128:(k + 1) * 128],
                                              rhs=wt[:, dc, 0:ncol], start=(dc == 0), stop=(dc == 15)),
                     reads=[xT_bk[k], wt_b], writes=[pb])
            pt = pfull[:, :].rearrange("p (h e) -> p h e", h=4)
            evt, evt_b = ev[k % 2]
            nT = 0
            if typ == "qn":
                c.op("act", lambda e: e.activation(out=evt[:, :, 32:128], in_=pt[:, :, 32:128], func=AF.Copy, scale=SCALE),
                     reads=[pb], writes=[evt_b])
                rope(pt, evt, evt_b, 0, 4, csq, snq, [csq_b, snq_b], k, pb)
                nT = 4
            elif typ == "sq":
                c.op("act", lambda e: e.activation(out=evt[:], in_=pt, func=AF.Copy, scale=SCALE),
                     reads=[pb], writes=[evt_b])
                nT = 4
            elif typ in ("kcvc", "sk"):
                c.op("act", lambda e: e.copy(out=evt[:], in_=pt), reads=[pb], writes=[evt_b])
                nT = 4
            elif typ in ("ksvs", "kwvw"):
                c.op("act", lambda e: e.copy(out=evt[:, 0:2, 32:128], in_=pt[:, 0:2, 32:128]), reads=[pb], writes=[evt_b])
                rope(pt, evt, evt_b, 0, 2, cs, sn, [cs_b, sn_b], k, pb)
                c.op("act", lambda e: e.copy(out=vs[:, 0:2, k, 0:128], in_=pt[:, 2:4, :]), reads=[pb], writes=[vs_b])
                nT = 2
            elif typ == "sv":
                c.op("act", lambda e: e.copy(out=vs[:, 0:4, k, 0:128], in_=pt), reads=[pb], writes=[vs_b])
            elif typ == "g":
                c.op("dve", lambda e: e.tensor_tensor(out=gtmp[:], in0=pfull[:, 0:24], in1=bgb[:], op=ALU.add),
                     reads=[pb, bgb_b], writes=[gtmp_b])
                c.op("act", lambda e: e.activation(out=gsb[:, k, :], in_=gtmp[:], func=AF.Sigmoid),
                     reads=[gtmp_b], writes=[gsb_b])
            if nT:
                tbi = 6 + (k % 2)
                ptb = kk.bank_bf16(tbi)
                for j in range(nT):
                    c.op("pe", lambda e: e.transpose(out=ptb[:, j * 128:(j + 1) * 128], in_=evt[:, j, :],
                                                     identity=kk.ident[:]),
                         reads=[evt_b, kk.ident_b], writes=[kk.bankb[tbi]])
                c.op("dve", lambda e: e.tensor_copy(
                    out=ts[:, 0:nT, k * 128:(k + 1) * 128],
                    in_=ptb[:, 0:nT * 128].rearrange("p (j t) -> p j t", j=nT)),
                    reads=[kk.bankb[tbi]], writes=[ts_b])
        if typ == "qn":
            h0 = c0 // 128
            c.dma(QT[h0:h0 + 4].rearrange("h p t -> p h t"), ts[:], reads=[ts_b])
        elif typ == "sq":
            h0 = 8 + (c0 - C_SQ) // 128
            c.dma(QT[h0:h0 + 4].rearrange("h p t -> p h t"), ts[:], reads=[ts_b])
        elif typ == "kcvc":
            c.dma(KT[0:4].rearrange("h p t -> p h t"), ts[:], reads=[ts_b])
        elif typ == "ksvs":
            c.dma(KT[4:6].rearrange("h p t -> p h t"), ts[:, 0:2, :], reads=[ts_b])
            c.dma(V[0:2].rearrange("h p k e -> p h k e"), vs[:, 0:2], reads=[vs_b])
        elif typ == "kwvw":
            c.dma(KT[6:8].rearrange("h p t -> p h t"), ts[:, 0:2, :], reads=[ts_b])
            c.dma(V[2:4].rearrange("h p k e -> p h k e"), vs[:, 0:2], reads=[vs_b])
        elif typ == "sk":
            h0 = 8 + (c0 - C_SK) // 128
            c.dma(KT[h0:h0 + 4].rearrange("h p t -> p h t"), ts[:], reads=[ts_b])
        elif typ == "sv":
            h0 = 4 + (c0 - C_SV) // 128
            c.dma(V[h0:h0 + 4].rearrange("h p k e -> p h k e"), vs[:], reads=[vs_b])
        elif typ == "g":
            c.dma(gates_out, gsb[:], reads=[gsb_b])
    c.barrier()
    es.close()


def build_pa():
    nc = bass.Bass("TRN2", target_bir_lowering=False)

    def din(name, shape, dt=F32):
        return nc.dram_tensor(name, list(shape), dt, kind="ExternalInput").ap()

    def dout(name, shape, dt=F32):
        return nc.dram_tensor(name, list(shape), dt, kind="ExternalOutput").ap()

    h = din("h", [TL, D])
    w_in = din("w_in", [D, D_IN])
    nmix = din("nmix", [1, D])
    bg = din("bg", [1, 24])
    pos = din("pos", [128, NK], I32)
    invf = din("invf", [1, 16])
    ident = din("ident", [128, 128])
    QT = dout("QT", [16, 128, TL], BF16)
    KT = dout("KT", [16, 128, TL], BF16)
    V = dout("V", [12, 128, NK, DV], BF16)
    gates = dout("gates", [128, NK, 24])
    kk = K(nc)
    kk.load_ident(ident)
    emit_phase_a(kk, h, w_in, nmix, bg, pos, invf, QT, KT, V, gates)
    kk.c.finish()
    return nc


def _inv_freq():
    return (500000.0 ** (-np.arange(0, 32, 2, dtype=np.float32) / 32)).astype(np.float32)[None, :]


def shard_tokens(a):
    blk = a.reshape(64, 128, *a.shape[1:])
    return [np.ascontiguousarray(blk[c::8].reshape(TL, *a.shape[1:])) for c in range(NCORES)]


def unshard_tokens(parts):
    out = np.empty((64, 128) + parts[0].shape[1:], parts[0].dtype)
    for c in range(NCORES):
        out[c::8] = parts[c].reshape(8, 128, *parts[c].shape[1:])
    return out.reshape(S, *parts[0].shape[1:])


_PROG = {}


def run_pa(h_parts, pos_parts, w_in, nmix, bg):
    if "pa" not in _PROG:
        _PROG["pa"] = build_pa()
    nc = _PROG["pa"]
    ident = np.eye(128, dtype=np.float32)
    invf = _inv_freq()
    in_maps = []
    for c in range(NCORES):
        in_maps.append({
            "h": h_parts[c], "w_in": w_in, "nmix": nmix[None, :], "bg": bg[None, :],
            "pos": np.ascontiguousarray(pos_parts[c].reshape(NK, 128).T), "invf": invf, "ident": ident,
        })
    res = run_bass_kernel_spmd(nc, in_maps, core_ids=list(range(NCORES)))
    return res.results


DEBUG = False
GELU_C0 = 0.7978845608028654
GELU_C1 = 0.044715


def emit_phase_b(kk, P):
    c = kk.c
    nc = kk.nc
    bank, bankb = kk.bank, kk.bankb
    ident, ident_b = kk.ident, kk.ident_b
    es_all = ExitStack()
    es_att = ExitStack()

    def mm(out, lhsT, rhs, start, stop, reads, wb):
        c.op("pe", lambda e: e.matmul(out, lhsT=lhsT, rhs=rhs, start=start, stop=stop, skip_group_check=True),
             reads=reads, writes=[wb])

    onT, onT_b = c.sb("b_onT", [128, 16, TL], BF16, es_all)
    KTr = [c.sb("b_KT%d" % i, [128, 8, TL], BF16, es_att) for i in range(3)]
    Vr = [c.sb("b_V%d" % i, [128, 8, NK, DV], BF16, es_att) for i in range(3)]
    Gm, Gm_b = c.sb("b_G", [128, 64 * 128], BF16, es_att)
    mctn, mctn_b = c.sb("b_mctn", [128, 960], BF16, es_att)
    mcnt, mcnt_b = c.sb("b_mcnt", [128, 3, 128], BF16, es_att)
    Arel, Arel_b = c.sb("b_Arel", [128, 240], F32, es_att)
    Brel, Brel_b = c.sb("b_Brel", [128, 240], F32, es_att)
    dz, dz_b = c.sb("b_dz", [128, 8, 128], BF16, es_att)
    wz, wz_b = c.sb("b_wz", [128, 12, 128], BF16, es_att)
    vzr, vzr_b = c.sb("b_vzr", [128, 8, 128], BF16, es_att)
    ntri, ntri_b = c.sb("b_ntri", [128, 128], BF16, es_att)
    nones, nones_b = c.sb("b_nones", [128, 128], BF16, es_att)
    gates, gates_b = c.sb("b_gates", [128, NK, 24], F32, es_att)
    ggT, ggT_b = c.sb("b_ggT", [128, 16], F32, es_att)
    kcT = [c.sb("b_kcT%d" % g, [128, 512], BF16, es_att) for g in range(2)]
    vcs = [c.sb("b_vc%d" % g, [128, 4, DV], BF16, es_att) for g in range(2)]
    qsb = [c.sb("b_q%d" % i, [128, 4, TL], BF16, es_att) for i in range(1)]
    ef = [c.sb("b_ef%d" % i, [128, 512], F32, es_att) for i in range(2)]
    pb16 = [c.sb("b_p%d" % i, [128, 4, 128], BF16, es_att) for i in range(2)]
    lb16 = [c.sb("b_l%d" % i, [128, 4, 128], BF16, es_att) for i in range(2)]
    padbuf, padbuf_b = c.sb("b_pad", [128, 516], F32, es_att)
    qi, qi_b = c.sb("b_qi", [128, 512], F32, es_att)
    pslc, pslc_b = c.sb("b_pslc", [128, 128], F32, es_att)
    imp, imp_b = c.sb("b_imp", [128, 128], F32, es_att)
    imp2, imp2_b = c.sb("b_imp2", [128, 128], F32, es_att)
    sel2, sel2_b = c.sb("b_sel2", [128, 128], F32, es_att)
    selb, selb_b = c.sb("b_selb", [128, 128], BF16, es_att)
    selT, selT_b = c.sb("b_selT", [128, 128], BF16, es_att)
    m8a, m8a_b = c.sb("b_m8a", [128, 8], F32, es_att)
    m8b, m8b_b = c.sb("b_m8b", [128, 8], F32, es_att)
    sm, sm_b = c.sb("b_sm", [128, 16], F32, es_att)
    oacc, oacc_b = c.sb("b_oacc", [128, 4, 128], F32, es_att)
    onb, onb_b = c.sb("b_onb", [128, 4, 128], BF16, es_att)
    sqj, sqj_b = c.sb("b_sqj", [128, 128], F32, es_att)
    r32, r32_b = c.sb("b_r32", [128, 128], F32, es_att)
    rsum, rsum_b = c.sb("b_rsum", [128, 128], F32, es_att)
    rb, rb_b = c.sb("b_rb", [128, 128], BF16, es_att)
    qs1 = [c.sb("b_qs%d" % i, [128, TL], BF16, es_att) for i in range(2)]

    c.dma(Gm[:], P["Gm"], writes=[Gm_b], q="pool")
    c.dma(mctn[:], P["mctn"], writes=[mctn_b], q="pool")
    c.dma(mcnt[:], P["mcnt"], writes=[mcnt_b], q="pool")
    c.dma(Arel[:], P["Arel"], writes=[Arel_b])
    c.dma(Brel[:], P["Brel"], writes=[Brel_b])
    c.dma(dz[:], P["dz"], writes=[dz_b], q="pool")
    c.dma(wz[:], P["wz"], writes=[wz_b], q="pool")
    c.dma(vzr[:], P["vzr"], writes=[vzr_b], q="pool")
    c.dma(ntri[:], P["ntri"], writes=[ntri_b], q="pool")
    c.dma(nones[:], P["nones"], writes=[nones_b], q="pool")
    c.dma(gates[:], P["gates"], writes=[gates_b])
    with nc.allow_non_contiguous_dma(reason="tiny one-off per-head scale table"):
        c.dma(ggT[:], P["ngrp"].rearrange("o (h d) -> d (o h)", d=128), writes=[ggT_b])
    c.op("dve", lambda e: e.memset(padbuf[:], 0.0), writes=[padbuf_b])

    es_c = ExitStack()
    w1 = [(KTr[1 + i][0][:].rearrange("p c t -> p (c t)").rearrange("p (l h) -> p l h", h=256), KTr[1 + i][1]) for i in range(2)]
    w2 = [c.sb("c_w2%d" % i, [128, 2, 128], BF16, es_c) for i in range(2)]
    cpos, cpos_b = c.sb("c_cpos", [32, 128], BF16, es_c)
    cposT, cposT_b = c.sb("c_cposT", [128, 32], BF16, es_c)
    b1, b1_b = c.sb("c_b1", [128, 4], F32, es_c)
    xs, xs_b = c.sb("c_xs", [128, 512], F32, es_c)
    uu, uu_b = c.sb("c_uu", [128, 512], F32, es_c)
    gT = [c.sb("c_gT%d" % i, [128, 512], BF16, es_c) for i in range(2)]
    ktok, ktok_b = c.sb("c_ktok", [128, 128], BF16, es_c)
    pci, pci_b = c.sb("c_pci", [128, 4], I32, es_c)
    pcf, pcf_b = c.sb("c_pcf", [128, 4], F32, es_c)
    invb, invb_b = c.sb("c_invf", [128, 16], F32, es_c)
    cang, cang_b = c.sb("c_ang", [128, 4, 16], F32, es_c)
    ccs, ccs_b = c.sb("c_cs", [128, 4, 16], F32, es_c)
    csn, csn_b = c.sb("c_sn", [128, 4, 16], F32, es_c)
    rri, rri_b = c.sb("c_rri", [128, 4, 16], I32, es_c)
    rrf, rrf_b = c.sb("c_rrf", [128, 4, 16], F32, es_c)
    rrx, rrx_b = c.sb("c_rrx", [128, 4, 16], F32, es_c)
    crt = [c.sb("c_rt%d" % i, [128, 16], F32, es_c) for i in range(4)]

    c.dma(w1[0][0], P["wck1"].rearrange("(l d) h -> d l h", d=128), writes=[w1[0][1]], q="pool")
    c.dma(w1[1][0], P["wcv1"].rearrange("(l d) h -> d l h", d=128), writes=[w1[1][1]], q="pool")
    c.dma(w2[0][0][:], P["wck2"].rearrange("(c p) d -> p c d", p=128), writes=[w2[0][1]], q="pool")
    c.dma(w2[1][0][:], P["wcv2"].rearrange("(c p) d -> p c d", p=128), writes=[w2[1][1]], q="pool")
    c.dma(cpos[:], P["cmp_pos"], writes=[cpos_b], q="pool")
    c.dma(pci[:], P["poscmp"], writes=[pci_b])
    c.dma(invb[:], P["invf"].broadcast_to([128, 16]), writes=[invb_b])
    c.op("dve", lambda e: e.tensor_copy(out=pcf[:], in_=pci[:]), reads=[pci_b], writes=[pcf_b])
    c.op("dve", lambda e: e.tensor_tensor(out=cang[:], in0=bc(pcf[:].unsqueeze(2), [128, 4, 16]),
                                          in1=bc(invb[:].unsqueeze(1), [128, 4, 16]), op=ALU.mult),
         reads=[pcf_b, invb_b], writes=[cang_b])
    emit_sin(c, csn, csn_b, cang, cang_b, 0.0, rri, rri_b, rrf, rrf_b, rrx, rrx_b)
    emit_sin(c, ccs, ccs_b, cang, cang_b, 0.5 * PI, rri, rri_b, rrf, rrf_b, rrx, rrx_b)
    ptb = kk.bank_bf16(7)
    c.op("pe", lambda e: e.transpose(out=ptb[:, 0:32], in_=cpos[:], identity=ident[0:32, 0:32]),
         reads=[cpos_b, ident_b], writes=[bankb[7]])
    c.op("dve", lambda e: e.tensor_copy(out=cposT[:], in_=ptb[:, 0:32]), reads=[bankb[7]], writes=[cposT_b])
    for X in range(2):
        for hc in range(2):
            for l in range(32):
                mm(bank[6][:, X * 2 + hc:X * 2 + hc + 1], w1[X][0][:, l, hc * 128:(hc + 1) * 128], cposT[:, l:l + 1],
                   (l == 0 and X == 0 and hc == 0), (l == 31), [w1[X][1], cposT_b], bankb[6])
    c.op("dve", lambda e: e.tensor_copy(out=b1[:], in_=bank[6][:, 0:4]), reads=[bankb[6]], writes=[b1_b])
    c.op("dve", lambda e: e.memset(gT[0][0][:], 0.0), writes=[gT[0][1]])
    c.op("dve", lambda e: e.memset(gT[1][0][:], 0.0), writes=[gT[1][1]])
    for g in range(2):
        c.op("pool", lambda e: e.memset(vcs[g][0][:, :, 128:129], 1.0), writes=[vcs[g][1]])
        c.op("pool", lambda e: e.memset(vcs[g][0][:, :, 129:130], 0.0), writes=[vcs[g][1]])

    stg_flat = [KTr[0][0][:].rearrange("p c t -> p (c t)"),
                Vr[1][0][:].rearrange("p c k e -> p (c k e)")[:, 0:8192]]
    stg2 = [Vr[0][0][:].rearrange("p c k e -> p (c k e)")[:, 0:8192].rearrange("p (r n) -> p r n", r=16),
            Vr[2][0][:].rearrange("p c k e -> p (c k e)")[:, 0:8192].rearrange("p (r n) -> p r n", r=16)]
    stg_parts = [[Buf("kcg%d_part%d" % (j, i)) for i in range(8)] for j in range(2)]
    stg2_b = [(Buf("kcg2_lo%d" % j), Buf("kcg2_hi%d" % j)) for j in range(2)]
    for X in range(2):
        for g in range(2):
            hh = 2 * X + g
            sidx = hh % 2
            kcg_flat = stg_flat[sidx]
            kcg_v = kcg_flat.rearrange("p (k c t) -> p k c t", k=8, c=8)
            kcg2 = stg2[sidx]
            kcg2_b, kcg2_hi_b = stg2_b[sidx]
            kcg_parts = stg_parts[sidx]
            for cc in range(8):
                c.dma(kcg_v[:, :, cc, :], P["KTg"][cc, hh].rearrange("d (k t) -> d k t", k=8), writes=[kcg_parts[cc]])
            src_rn = kcg_flat.rearrange("p (n r) -> p r n", r=16)
            c.op("pool", lambda e: e.tensor_copy(out=kcg2[:, 0:8, :], in_=src_rn[:, 0:8, :]), reads=kcg_parts, writes=[kcg2_b])
            c.op("act", lambda e: e.copy(out=kcg2[:, 8:16, :], in_=src_rn[:, 8:16, :]), reads=kcg_parts, writes=[kcg2_hi_b])
            for hc in range(2):
                bi = hc
                for l in range(32):
                    mm(bank[bi][:, 0:511], w1[X][0][:, l, hc * 128:(hc + 1) * 128],
                       kcg2[:, l % 16, (l // 16):(l // 16) + 511], (l == 0), (l == 31), [w1[X][1], kcg2_b, kcg2_hi_b], bankb[bi])
                c.op("act", lambda e: e.activation(out=xs[:, 0:511], in_=bank[bi][:, 0:511], func=AF.Identity,
                                                   bias=b1[:, X * 2 + hc:X * 2 + hc + 1]),
                     reads=[bankb[bi], b1_b], writes=[xs_b])
                c.op("dve", lambda e: e.tensor_tensor(out=uu[:, 0:511], in0=xs[:, 0:511], in1=xs[:, 0:511], op=ALU.mult),
                     reads=[xs_b], writes=[uu_b])
                c.op("dve", lambda e: e.tensor_scalar(out=uu[:, 0:511], in0=uu[:, 0:511], scalar1=GELU_C1, scalar2=1.0,
                                                      op0=ALU.mult, op1=ALU.add), reads=[uu_b], writes=[uu_b])
                c.op("dve", lambda e: e.tensor_tensor(out=uu[:, 0:511], in0=uu[:, 0:511], in1=xs[:, 0:511], op=ALU.mult),
                     reads=[uu_b, xs_b], writes=[uu_b])
                c.op("act", lambda e: e.activation(out=uu[:, 0:511], in_=uu[:, 0:511], func=AF.Sigmoid, scale=2 * GELU_C0),
                     reads=[uu_b], writes=[uu_b])
                c.op("dve", lambda e: e.tensor_tensor(out=gT[hc][0][:, 0:511], in0=uu[:, 0:511], in1=xs[:, 0:511], op=ALU.mult),
                     reads=[uu_b, xs_b], writes=[gT[hc][1]])
            for nch in range(4):
                bi = 2 + nch % 2
                for hc in range(2):
                    mm(bank[bi][:, 0:128], gT[hc][0][:, nch * 128:(nch + 1) * 128], w2[X][0][:, hc, :],
                       (hc == 0), (hc == 1), [gT[hc][1], w2[X][1]], bankb[bi])
                if X == 1:
                    c.op("act", lambda e: e.copy(out=vcs[g][0][:, nch, 0:128], in_=bank[bi][:, 0:128]),
                         reads=[bankb[bi]], writes=[vcs[g][1]])
                else:
                    pt = bank[bi]
                    co = ccs[:, nch, :]
                    si = csn[:, nch, :]
                    t = [r[0][:] for r in crt]
                    tb = [r[1] for r in crt]
                    c.op("act", lambda e: e.copy(out=ktok[:, 32:128], in_=pt[:, 32:128]), reads=[bankb[bi]], writes=[ktok_b])
                    c.op("dve", lambda e: e.tensor_tensor(out=t[0], in0=pt[:, 0:16], in1=co, op=ALU.mult), reads=[bankb[bi], ccs_b], writes=[tb[0]])
                    c.op("dve", lambda e: e.tensor_tensor(out=t[1], in0=pt[:, 16:32], in1=si, op=ALU.mult), reads=[bankb[bi], csn_b], writes=[tb[1]])
                    c.op("dve", lambda e: e.tensor_tensor(out=t[2], in0=pt[:, 16:32], in1=co, op=ALU.mult), reads=[bankb[bi], ccs_b], writes=[tb[2]])
                    c.op("dve", lambda e: e.tensor_tensor(out=t[3], in0=pt[:, 0:16], in1=si, op=ALU.mult), reads=[bankb[bi], csn_b], writes=[tb[3]])
                    c.op("dve", lambda e: e.tensor_tensor(out=ktok[:, 0:16], in0=t[0], in1=t[1], op=ALU.subtract), reads=[tb[0], tb[1]], writes=[ktok_b])
                    c.op("dve", lambda e: e.tensor_tensor(out=ktok[:, 16:32], in0=t[2], in1=t[3], op=ALU.add), reads=[tb[2], tb[3]], writes=[ktok_b])
                    tbi = 6 + nch % 2
                    ptt = kk.bank_bf16(tbi)
                    c.op("pe", lambda e: e.transpose(out=ptt[:, 0:128], in_=ktok[:], identity=ident[:]),
                         reads=[ktok_b, ident_b], writes=[bankb[tbi]])
                    c.op("dve", lambda e: e.tensor_copy(out=kcT[g][0][:, nch * 128:(nch + 1) * 128], in_=ptt[:, 0:128]),
                         reads=[bankb[tbi]], writes=[kcT[g][1]])
    c.barrier()
    es_c.close()

    class Pipe:
        def __init__(self, ngroups):
            self.ng = ngroups
            self.items = []

        def add(self, groups):
            assert len(groups) == self.ng
            self.items.append(groups)

        def run(self):
            n = len(self.items)
            for step in range(n + self.ng - 1):
                for j in range(self.ng):
                    i = step - j
                    if 0 <= i < n and self.items[i][j] is not None:
                        self.items[i][j]()
            self.items = []

    rings = {}

    def ring(name, lst):
        i = rings.get(name, 0)
        rings[name] = i + 1
        return lst[i % len(lst)]

    sm_e, sm_e_b = c.sb("b_sme", [128, 4], F32, es_att)
    sm_n, sm_n_b = c.sb("b_smn", [128, 4], F32, es_att)
    pb3 = pb16 + [c.sb("b_p2", [128, 4, 128], BF16, es_att)]
    lb3 = lb16 + [c.sb("b_l2", [128, 4, 128], BF16, es_att)]
    rb3 = [(rb, rb_b)] + [c.sb("b_rb%d" % i, [128, 128], BF16, es_att) for i in range(1, 5)]
    selT2 = [(selT, selT_b), c.sb("b_selT1", [128, 128], BF16, es_att)]
    qs3 = qs1 + [c.sb("b_qs2", [128, TL], BF16, es_att)]

    def gate_ap(k, branch, h8):
        return gates[:, k, branch * 8 + h8:branch * 8 + h8 + 1]

    def evac_nsa(obanks, k, g, branch, first):
        for h in range(4):
            ob = obanks[h // 2]
            ov = bank[ob][:, 0:2 * DV].rearrange("p (h e) -> p h e", h=2)
            den = sm_e[:, h:h + 1]
            c.op("dve", lambda e: e.tensor_scalar_max(out=den, in0=ov[:, h % 2, 128:129], scalar1=1e-30),
                 reads=[bankb[ob]], writes=[sm_e_b])
            c.op("dve", lambda e: e.reciprocal(out=den, in_=den), reads=[sm_e_b], writes=[sm_e_b])
            c.op("dve", lambda e: e.tensor_tensor(out=den, in0=den, in1=gate_ap(k, branch, 4 * g + h), op=ALU.mult),
                 reads=[sm_e_b, gates_b], writes=[sm_e_b])
            if first:
                c.op("dve", lambda e: e.tensor_scalar_mul(out=oacc[:, h, :], in0=ov[:, h % 2, 0:128], scalar1=den),
                     reads=[bankb[ob], sm_e_b], writes=[oacc_b])
            else:
                c.op("dve", lambda e: e.scalar_tensor_tensor(out=oacc[:, h, :], in0=ov[:, h % 2, 0:128], scalar=den,
                                                             in1=oacc[:, h, :], op0=ALU.mult, op1=ALU.add),
                     reads=[bankb[ob], sm_e_b, oacc_b], writes=[oacc_b])

    def head_norm_T(src_ap_fn, src_b, nh, hh0, k):
        for h in range(nh):
            c.op("act", lambda e: e.activation(out=sqj[:], in_=src_ap_fn(h), func=AF.Square, accum_out=sm_n[:, h:h + 1]),
                 reads=[src_b], writes=[sqj_b, sm_n_b])
        c.op("act", lambda e: e.activation(out=sm_n[:, 0:nh], in_=sm_n[:, 0:nh], func=AF.Ln, scale=1.0 / HD, bias=EPS),
             reads=[sm_n_b], writes=[sm_n_b])
        c.op("act", lambda e: e.activation(out=sm_n[:, 0:nh], in_=sm_n[:, 0:nh], func=AF.Exp, scale=-0.5),
             reads=[sm_n_b], writes=[sm_n_b])
        for h in range(nh):
            c.op("dve", lambda e: e.tensor_scalar_mul(out=onb[:, h, :], in0=src_ap_fn(h), scalar1=sm_n[:, h:h + 1]),
                 reads=[src_b, sm_n_b], writes=[onb_b])
        tbi = 6 + (k % 2)
        ptt = kk.bank_bf16(tbi)
        for h in range(nh):
            c.op("pe", lambda e: e.transpose(out=ptt[:, h * 128:(h + 1) * 128], in_=onb[:, h, :], identity=ident[:]),
                 reads=[onb_b, ident_b], writes=[bankb[tbi]])
        c.op("dve", lambda e: e.tensor_tensor(out=onT[:, hh0:hh0 + nh, k * 128:(k + 1) * 128],
                                              in0=ptt[:, 0:nh * 128].rearrange("p (j t) -> p j t", j=nh),
                                              in1=bc(ggT[:, hh0:hh0 + nh].unsqueeze(2), [128, nh, 128]), op=ALU.mult),
             reads=[bankb[tbi], ggT_b], writes=[onT_b])

    ringkv = {"kt": 0, "v": 0}

    def next_kt():
        i = ringkv["kt"]
        ringkv["kt"] = (i + 1) % 3
        return KTr[i]

    def next_v():
        i = ringkv["v"]
        ringkv["v"] = (i + 1) % 3
        return Vr[i]

    def nsa_block_item(KT_ap, bias_list, V_ap, Qk, q_b, obanks, first, reads_kv, pre=None, post=None):
        sb_i = ring("S", [0, 1])
        pt_, pt_b = ring("P", pb3)
        n = len(bias_list)

        def g0():
            if pre is not None:
                pre()
            mm(bank[sb_i][:], KT_ap, Qk, True, n == 0, reads_kv + [q_b], bankb[sb_i])
            for bi_, (lhsT, rhs, rb_) in enumerate(bias_list):
                mm(bank[sb_i][:], lhsT, rhs, False, bi_ == n - 1, rb_, bankb[sb_i])
            c.op("act", lambda e: e.activation(out=pt_[:].rearrange("p h t -> p (h t)"), in_=bank[sb_i][:], func=AF.Exp),
                 reads=[bankb[sb_i]], writes=[pt_b])

        def g1():
            for h in range(4):
                ob = obanks[h // 2]
                mm(bank[ob][:, (h % 2) * DV:(h % 2 + 1) * DV], pt_[:, h, :], V_ap,
                   (first and h % 2 == 0), False, [pt_b] + reads_kv, bankb[ob])
            if post is not None:
                post()

        return [g0, g1]

    for g in range(2):
        KTs, KTs_b = next_kt()
        KTw, KTw_b = next_kt()
        Vs, Vs_b = next_v()
        Vw, Vw_b = next_v()
        c.dma(KTs[:], P["KTg"][:, 4 + g].rearrange("c d t -> d c t"), writes=[KTs_b])
        c.dma(Vs[:], P["Vg"][:, g].rearrange("c p k e -> p c k e"), writes=[Vs_b])
        c.dma(KTw[:], P["KTg"][:, 6 + g].rearrange("c d t -> d c t"), writes=[KTw_b])
        c.dma(Vw[:], P["Vg"][:, 2 + g].rearrange("c p k e -> p c k e"), writes=[Vw_b])
        qt, qt_b = qsb[0]
        c.dma(qt[:], P["QT"][4 * g:4 * g + 4].rearrange("h d t -> d h t"), writes=[qt_b])
        kct, kct_b = kcT[g]
        vct, vct_b = vcs[g]
        selinfo = {}

        def sel_a1(k):
            offc = 448 - 64 * k
            for h in range(4):
                xb = 6 + (h % 2)
                mm(bank[xb][:], qt[:, h, k * 128:(k + 1) * 128], kct[:], True, False, [qt_b, kct_b], bankb[xb])
                mm(bank[xb][:], ident[:], mctn[:, offc:offc + 512], False, True, [ident_b, mctn_b], bankb[xb])
                et, et_b = ef[h % 2]
                c.op("act", lambda e: e.activation(out=et[:], in_=bank[xb][:], func=AF.Exp, accum_out=sm[:, 4 + h:5 + h]),
                     reads=[bankb[xb]], writes=[et_b, sm_b])
                c.op("dve", lambda e: e.tensor_scalar_max(out=sm[:, 4 + h:5 + h], in0=sm[:, 4 + h:5 + h], scalar1=1e-30),
                     reads=[sm_b], writes=[sm_b])
                c.op("dve", lambda e: e.reciprocal(out=sm[:, 4 + h:5 + h], in_=sm[:, 4 + h:5 + h]), reads=[sm_b], writes=[sm_b])
                if h == 0:
                    c.op("dve", lambda e: e.tensor_scalar_mul(out=padbuf[:, 1:513], in0=et[:], scalar1=sm[:, 4:5]),
                         reads=[et_b, sm_b], writes=[padbuf_b])
                else:
                    c.op("dve", lambda e: e.scalar_tensor_tensor(out=padbuf[:, 1:513], in0=et[:], scalar=sm[:, 4 + h:5 + h],
                                                                 in1=padbuf[:, 1:513], op0=ALU.mult, op1=ALU.add),
                         reads=[et_b, sm_b, padbuf_b], writes=[padbuf_b])
            c.op("dve", lambda e: e.tensor_tensor(out=qi[:], in0=padbuf[:, 1:513], in1=padbuf[:, 0:512], op=ALU.add),
                 reads=[padbuf_b], writes=[qi_b])
            c.op("dve", lambda e: e.tensor_reduce(out=pslc[:], in_=qi[:].rearrange("p (j r) -> p j r", r=4),
                                                  axis=AX.X, op=ALU.add), reads=[qi_b], writes=[pslc_b])
            offa = 112 - 16 * k
            c.op("dve", lambda e: e.tensor_tensor(out=imp[:], in0=pslc[:], in1=Arel[:, offa:offa + 128], op=ALU.mult),
                 reads=[pslc_b, Arel_b], writes=[imp_b])
            c.op("dve", lambda e: e.tensor_tensor(out=imp[:], in0=imp[:], in1=Brel[:, offa:offa + 128], op=ALU.add),
                 reads=[imp_b, Brel_b], writes=[imp_b])
            c.op("dve", lambda e: e.memset(imp[:, 0:1], 1e4), writes=[imp_b])
            c.op("dve", lambda e: e.max(out=m8a[:], in_=imp[:]), reads=[imp_b], writes=[m8a_b])
            c.op("dve", lambda e: e.match_replace(out=imp2[:], in_to_replace=m8a[:], in_values=imp[:], imm_value=-1e9),
                 reads=[imp_b, m8a_b], writes=[imp2_b])
            c.op("dve", lambda e: e.max(out=m8b[:], in_=imp2[:]), reads=[imp2_b], writes=[m8b_b])
            c.op("dve", lambda e: e.tensor_single_scalar(out=sel2[:], in_=imp[:], scalar=-5000.0, op=ALU.is_gt),
                 reads=[imp_b], writes=[sel2_b])
            c.op("dve", lambda e: e.scalar_tensor_tensor(out=sel2[:], in0=imp[:], scalar=m8b[:, 7:8], in1=sel2[:],
                                                         op0=ALU.is_ge, op1=ALU.mult),
                 reads=[imp_b, m8b_b, sel2_b], writes=[sel2_b])
            c.op("dve", lambda e: e.tensor_scalar(out=selb[:], in0=sel2[:], scalar1=-1.0, scalar2=-NEG,
                                                  op0=ALU.add, op1=ALU.mult), reads=[sel2_b], writes=[selb_b])

        def sel_a2(k):
            st, st_b = selT2[k % 2]
            tbi = 6 + (k % 2)
            ptt = kk.bank_bf16(tbi)
            c.op("pe", lambda e: e.transpose(out=ptt[:, 0:128], in_=selb[:], identity=ident[:]),
                 reads=[selb_b, ident_b], writes=[bankb[tbi]])
            c.op("dve", lambda e: e.tensor_copy(out=st[:], in_=ptt[:, 0:128]), reads=[bankb[tbi]], writes=[st_b])

        pipe = Pipe(2)
        sel_a1(0)
        sel_a2(0)
        for k in range(NK):
            Qk = qt[:, :, k * 128:(k + 1) * 128]
            st, st_b = selT2[k % 2]
            selT4 = bc(st[:].unsqueeze(1), [128, 4, 128])
            ob = ring("O", [(2, 3), (4, 5)])
            nchs = k // 2 + 1
            for nch in range(nchs):
                dprime = 16 * nch - 8 * k
                bl = []
                if dprime in (-16, -8, 0):
                    idx = dprime // 8 + 2
                    bl.append((ident[:], bc(mcnt[:, idx, :].unsqueeze(1), [128, 4, 128]), [ident_b, mcnt_b]))
                post = (lambda ob=ob, k=k: evac_nsa(ob, k, g, 0, True)) if nch == nchs - 1 else None
                pipe.add(nsa_block_item(kct[:, nch * 128:(nch + 1) * 128], bl, vct[:, nch, :], Qk, qt_b, ob, nch == 0,
                                        [kct_b, vct_b], post=post))
            ob = ring("O", [(2, 3), (4, 5)])
            nkb = 8 * k + 8
            for kb in range(nkb):
                ck, kq = kb % 8, kb // 8
                bl = [(Gm[:, kb * 128:(kb + 1) * 128], selT4, [Gm_b, st_b])]
                if kb >= 8 * k:
                    bl.append((ident[:], bc(dz[:, kb - 8 * k, :].unsqueeze(1), [128, 4, 128]), [ident_b, dz_b]))
                pre = (lambda k=k: sel_a1(k + 1)) if (kb == min(3, nkb - 1) and k + 1 < NK) else None
                post = (lambda ob=ob, k=k: evac_nsa(ob, k, g, 1, False)) if kb == nkb - 1 else None
                pipe.add(nsa_block_item(KTs[:, ck, kq * 128:(kq + 1) * 128], bl, Vs[:, ck, kq, :], Qk, qt_b, ob, kb == 0,
                                        [KTs_b, Vs_b], pre=pre, post=post))
            ob = ring("O", [(2, 3), (4, 5)])
            rs = [r for r in range(12) if 8 * k - 4 + r >= 0]
            for r in rs:
                kb = 8 * k - 4 + r
                ck, kq = kb % 8, kb // 8
                bl = [(ident[:], bc(wz[:, r, :].unsqueeze(1), [128, 4, 128]), [ident_b, wz_b])]
                pre = (lambda k=k: sel_a2(k + 1)) if (r == rs[2] and k + 1 < NK) else None

                def post_w(ob=ob, k=k):
                    evac_nsa(ob, k, g, 2, False)
                    head_norm_T(lambda h: oacc[:, h, :], oacc_b, 4, 4 * g, k)

                pipe.add(nsa_block_item(KTw[:, ck, kq * 128:(kq + 1) * 128], bl, Vw[:, ck, kq, :], Qk, qt_b, ob, r == rs[0],
                                        [KTw_b, Vw_b], pre=pre, post=post_w if r == rs[-1] else None))
        pipe.run()

    sbkv = {}

    def sb_load(h):
        KTh, KTh_b = next_kt()
        Vh, Vh_b = next_v()
        qh, qh_b = qs3[h % 3]
        c.dma(KTh[:], P["KTg"][:, 8 + h].rearrange("c d t -> d c t"), writes=[KTh_b])
        c.dma(Vh[:], P["Vg"][:, 4 + h].rearrange("c p k e -> p c k e"), writes=[Vh_b])
        c.dma(qh[:], P["QT"][8 + h], writes=[qh_b])
        sbkv[h] = (KTh, KTh_b, Vh, Vh_b, qh, qh_b)

    def sb_item(h, k, ti, ntile, pre):
        KTh, KTh_b, Vh, Vh_b, qh, qh_b = sbkv[h]
        Qk = qh[:, k * 128:(k + 1) * 128]
        obank = 4 + (k % 2)
        kb_hi = 8 * k + 7 - 4 * ti
        zb = ring("Z", [0, 1, 2, 3])
        Z = bank[zb]
        Zv = Z.rearrange("p (u t) -> p u t", u=4)
        et, et_b = ring("E", ef)
        lt, lt_b = ring("L", lb3)
        at, at_b = ring("A", pb3)
        rbi = rings.get("RB", 0)
        rings["RB"] = rbi + 1
        rb_w, rb_w_b = rb3[rbi % 3]
        rb_r, rb_r_b = rb3[(rbi - 1) % 3]

        def g0():
            if pre is not None:
                pre()
            for u in range(4):
                kb = kb_hi - u
                ck, kq = kb % 8, kb // 8
                mm(Z[:, u * 128:(u + 1) * 128], KTh[:, ck, kq * 128:(kq + 1) * 128], Qk, (u == 0), False,
                   [KTh_b, qh_b], bankb[zb])
            c.op("act", lambda e: e.activation(out=et[:], in_=Z[:], func=AF.Exp), reads=[bankb[zb]], writes=[et_b])
            c.op("act", lambda e: e.activation(out=lt[:].rearrange("p u t -> p (u t)"), in_=et[:], func=AF.Ln, bias=1.0),
                 reads=[et_b], writes=[lt_b])
            if ti < 2:
                c.op("pool", lambda e: e.tensor_tensor(out=lt[:], in0=lt[:], in1=vzr[:, 4 * ti:4 * ti + 4, :], op=ALU.mult),
                     reads=[lt_b, vzr_b], writes=[lt_b])
            if ti < ntile - 1:
                c.op("dve", lambda e: e.tensor_reduce(out=rsum[:], in_=lt[:].rearrange("p u t -> p t u"),
                                                      axis=AX.X, op=ALU.add), reads=[lt_b], writes=[rsum_b])
                if ti == 0:
                    c.op("dve", lambda e: e.tensor_copy(out=r32[:], in_=rsum[:]), reads=[rsum_b], writes=[r32_b])
                else:
                    c.op("dve", lambda e: e.tensor_tensor(out=r32[:], in0=r32[:], in1=rsum[:], op=ALU.add),
                         reads=[r32_b, rsum_b], writes=[r32_b])
                c.op("dve", lambda e: e.tensor_copy(out=rb_w[:], in_=r32[:]), reads=[r32_b], writes=[rb_w_b])

        def g1():
            mm(Z[:], ntri[:], lt[:].rearrange("p u t -> p (u t)"), False, False, [ntri_b, lt_b], bankb[zb])
            for u2 in range(3):
                mm(Zv[:, u2 + 1:4, :], nones[:], bc(lt[:, u2, :].unsqueeze(1), [128, 3 - u2, 128]), False, False,
                   [nones_b, lt_b], bankb[zb])
            if ti > 0:
                mm(Zv, nones[:], bc(rb_r[:].unsqueeze(1), [128, 4, 128]), False, True, [nones_b, rb_r_b], bankb[zb])
            c.op("act", lambda e: e.activation(out=at[:].rearrange("p u t -> p (u t)"), in_=Z[:], func=AF.Exp),
                 reads=[bankb[zb]], writes=[at_b])
            if ti < 2:
                c.op("pool", lambda e: e.tensor_tensor(out=at[:], in0=at[:], in1=vzr[:, 4 * ti:4 * ti + 4, :], op=ALU.mult),
                     reads=[at_b, vzr_b], writes=[at_b])

        def g2():
            for u in range(4):
                kb = kb_hi - u
                ck, kq = kb % 8, kb // 8
                mm(bank[obank][:, 0:128], at[:, u, :], Vh[:, ck, kq, 0:128], (ti == 0 and u == 0),
                   (ti == ntile - 1 and u == 3), [at_b, Vh_b], bankb[obank])
            if ti == ntile - 1:
                head_norm_T(lambda hh_: bank[obank][:, 0:128], bankb[obank], 1, 8 + h, k)

        return [g0, g1, g2]

    sb_load(0)
    sb_load(1)
    items = []
    for h in range(8):
        for k in range(NK):
            ntile = 2 * k + 2
            for ti in range(ntile):
                pre = None
                if k == 0 and ti == 0 and h >= 1 and h + 1 < 8:
                    pre = (lambda h=h: sb_load(h + 1))
                items.append((h, k, ti, ntile, pre))
    n_it = len(items)
    built = {}

    def get(i):
        if i not in built:
            h, k, ti, ntile, pre = items[i]
            if h not in sbkv:
                sb_load(h)
            built[i] = sb_item(h, k, ti, ntile, pre)
        return built[i]

    for step in range(n_it + 2):
        for j in range(3):
            i = step - j
            if 0 <= i < n_it:
                get(i)[j]()
                if j == 2:
                    del built[i]

    if "dbg_onT" in P:
        c.dma(P["dbg_onT"], onT[:], reads=[onT_b])
    c.barrier()
    es_att.close()

    es_f = ExitStack()
    hres, hres_b = c.sb("f_h", [128, NK, D], F32, es_f)
    gv, gv_b = c.sb("f_gv", [128, D], F32, es_f)
    junk, junk_b = c.sb("f_junk", [128, D], F32, es_f)
    ss, ss_b = c.sb("f_ss", [128, 1], F32, es_f)
    xn, xn_b = c.sb("f_xn", [128, D], BF16, es_f)
    es_o = ExitStack()
    wo = [c.sb("f_wo%d" % i, [128, 16, 512], BF16, es_o) for i in range(2)]

    hk_b = [Buf("f_h_k%d" % i) for i in range(NK)]
    xTf_b = [Buf("f_xT_k%d" % i) for i in range(NK)]
    for k in range(NK):
        c.dma(hres[:, k, :], P["h"][k * 128:(k + 1) * 128, :], writes=[hk_b[k]])
    for sl in range(4):
        wt, wt_b = wo[sl % 2]
        c.dma(wt[:], P["w_out"][:, sl * 512:(sl + 1) * 512].rearrange("(dc p) n -> p dc n", p=128), writes=[wt_b], q="pool")
        for k in range(NK):
            bi = k % 2
            for dc in range(16):
                mm(bank[bi][:], onT[:, dc, k * 128:(k + 1) * 128], wt[:, dc, :], dc == 0, dc == 15, [onT_b, wt_b], bankb[bi])
            c.op("dve", lambda e: e.tensor_tensor(out=hres[:, k, sl * 512:(sl + 1) * 512], in0=bank[bi][:],
                                                  in1=hres[:, k, sl * 512:(sl + 1) * 512], op=ALU.add),
                 reads=[bankb[bi], hk_b[k]], writes=[hk_b[k]])
    c.barrier()
    es_o.close()
    wg = [c.sb("f_wg%d" % i, [128, 16, 256], BF16, es_f) for i in range(2)]
    wu = [c.sb("f_wu%d" % i, [128, 16, 256], BF16, es_f) for i in range(2)]
    wd = [c.sb("f_wd%d" % i, [128, 2, D], BF16, es_f) for i in range(2)]
    mid = [c.sb("f_mid%d" % i, [128, 2, TL], BF16, es_f) for i in range(2)]
    sg = [c.sb("f_sg%d" % i, [128, 512], F32, es_f) for i in range(2)]
    c.dma(gv[:], P["nffn"].broadcast_to([128, D]), writes=[gv_b])
    tmp_pool = (junk, junk_b, ss, ss_b, xn, xn_b)
    xT, xT_b = onT, onT_b
    for k in range(NK):
        emit_rmsnorm_T(kk, hres[:, k, :], hk_b[k], gv, gv_b, xT, xTf_b[k], k, tmp_pool, (6, 7))
    NSL = D_FF // 256
    mid_hb = [[Buf("f_mid%d_h%d" % (i, j)) for j in range(2)] for i in range(2)]
    for sl in range(NSL):
        wgt, wgt_b = wg[sl % 2]
        wut, wut_b = wu[sl % 2]
        wdt, wdt_b = wd[sl % 2]
        mt, mt_b = mid[sl % 2]
        mt_hb = mid_hb[sl % 2]
        f0 = sl * 256
        c.dma(wgt[:], P["w_gate"][:, f0:f0 + 256].rearrange("(dc p) n -> p dc n", p=128), writes=[wgt_b], q="pool")
        c.dma(wut[:], P["w_up"][:, f0:f0 + 256].rearrange("(dc p) n -> p dc n", p=128), writes=[wut_b], q="pool")
        c.dma(wdt[:], P["w_down"][f0:f0 + 256, :].rearrange("(fc p) n -> p fc n", p=128), writes=[wdt_b], q="pool")
        for fc in range(2):
            for th in range(2):
                ba, bb = 2 * th, 2 * th + 1
                for dc in range(16):
                    mm(bank[ba][:], wgt[:, dc, fc * 128:(fc + 1) * 128], xT[:, dc, th * 512:(th + 1) * 512],
                       dc == 0, dc == 15, [wgt_b] + xTf_b[4 * th:4 * th + 4], bankb[ba])
                for dc in range(16):
                    mm(bank[bb][:], wut[:, dc, fc * 128:(fc + 1) * 128], xT[:, dc, th * 512:(th + 1) * 512],
                       dc == 0, dc == 15, [wut_b] + xTf_b[4 * th:4 * th + 4], bankb[bb])
                sgt, sgt_b = sg[th]
                c.op("act", lambda e: e.activation(out=sgt[:], in_=bank[ba][:], func=AF.Silu), reads=[bankb[ba]], writes=[sgt_b])
                c.op("dve", lambda e: e.tensor_tensor(out=mt[:, fc, th * 512:(th + 1) * 512], in0=bank[bb][:], in1=sgt[:],
                                                      op=ALU.mult), reads=[bankb[bb], sgt_b], writes=[mt_hb[th]])
        for k in range(NK):
            for ct in range(4):
                bi = 4 + (ct % 2) + 2 * (k % 2)
                for fc in range(2):
                    mm(bank[bi][:], mt[:, fc, k * 128:(k + 1) * 128], wdt[:, fc, ct * 512:(ct + 1) * 512],
                       fc == 0, fc == 1, [mt_hb[k // 4], wdt_b], bankb[bi])
                c.op("dve", lambda e: e.tensor_tensor(out=hres[:, k, ct * 512:(ct + 1) * 512], in0=bank[bi][:],
                                                      in1=hres[:, k, ct * 512:(ct + 1) * 512], op=ALU.add),
                     reads=[bankb[bi], hk_b[k]], writes=[hk_b[k]])
    c.dma(gv[:], P["nfinal"].broadcast_to([128, D]), reads=[], writes=[gv_b])
    for k in range(NK):
        c.dma(P["h_out"][k * 128:(k + 1) * 128, :], hres[:, k, :], reads=[hk_b[k]])
        c.op("act", lambda e: e.activation(out=junk[:], in_=hres[:, k, :], func=AF.Square, accum_out=ss[:]),
             reads=[hk_b[k]], writes=[junk_b, ss_b])
        c.op("act", lambda e: e.activation(out=ss[:], in_=ss[:], func=AF.Ln, scale=1.0 / D, bias=EPS), reads=[ss_b], writes=[ss_b])
        c.op("act", lambda e: e.activation(out=ss[:], in_=ss[:], func=AF.Exp, scale=-0.5), reads=[ss_b], writes=[ss_b])
        c.op("dve", lambda e: e.scalar_tensor_tensor(out=junk[:], in0=hres[:, k, :], scalar=ss[:, 0:1], in1=gv[:],
                                                     op0=ALU.mult, op1=ALU.mult),
             reads=[hk_b[k], ss_b, gv_b], writes=[junk_b])
        c.dma(P["hn_out"][k * 128:(k + 1) * 128, :], junk[:], reads=[junk_b])
    c.barrier()
    es_f.close()
    es_all.close()


def build_pb(with_pa=False):
    nc = bass.Bass("TRN2", target_bir_lowering=False)

    def din(name, shape, dt=F32):
        return nc.dram_tensor(name, list(shape), dt, kind="ExternalInput").ap()

    def dout(name, shape, dt=F32):
        return nc.dram_tensor(name, list(shape), dt, kind="ExternalOutput").ap()

    P = {
        "h": din("h", [TL, D]), "QT": din("QT", [16, 128, TL], BF16), "gates": din("gates", [128, NK, 24]),
        "KTg": din("KTg", [8, 16, 128, TL], BF16), "Vg": din("Vg", [8, 12, 128, NK, DV], BF16),
        "wck1": din("wck1", [4096, 256]), "wck2": din("wck2", [256, 128]),
        "wcv1": din("wcv1", [4096, 256]), "wcv2": din("wcv2", [256, 128]),
        "cmp_pos": din("cmp_pos", [32, 128]), "poscmp": din("poscmp", [128, 4], I32), "invf": din("invf", [1, 16]),
        "ngrp": din("ngrp", [1, D]), "w_out": din("w_out", [D, D]), "nffn": din("nffn", [1, D]),
        "w_gate": din("w_gate", [D, D_FF]), "w_up": din("w_up", [D, D_FF]), "w_down": din("w_down", [D_FF, D]),
        "nfinal": din("nfinal", [1, D]), "ident": din("ident", [128, 128]),
        "Gm": din("Gm", [128, 64 * 128]), "mctn": din("mctn", [128, 960]), "mcnt": din("mcnt", [128, 3, 128]),
        "Arel": din("Arel", [128, 240]), "Brel": din("Brel", [128, 240]),
        "dz": din("dz", [128, 8, 128]), "wz": din("wz", [128, 12, 128]), "vzr": din("vzr", [128, 8, 128]),
        "ntri": din("ntri", [128, 128]), "nones": din("nones", [128, 128]),
        "h_out": dout("h_out", [TL, D]), "hn_out": dout("hn_out", [TL, D]),
    }
    if DEBUG:
        P["dbg_onT"] = dout("dbg_onT", [128, 16, TL], BF16)
    if with_pa:
        A = {
            "w_in": din("w_in", [D, D_IN]), "nmix": din("nmix", [1, D]), "bg": din("bg", [1, 24]),
            "pos": din("pos", [128, NK], I32),
            "QT_o": dout("QT_o", [16, 128, TL], BF16), "KT_o": dout("KT_o", [16, 128, TL], BF16),
            "V_o": dout("V_o", [12, 128, NK, DV], BF16), "gates_o": dout("gates_o", [128, NK, 24]),
        }
    kk = K(nc)
    kk.load_ident(P["ident"])
    emit_phase_b(kk, P)
    if with_pa:
        emit_phase_a(kk, P["h_out"], A["w_in"], A["nmix"], A["bg"], A["pos"], P["invf"],
                     A["QT_o"], A["KT_o"], A["V_o"], A["gates_o"])
    kk.c.finish()
    return nc


def make_consts(cidx):
    p = np.arange(128)
    out = {}
    u = np.arange(64 * 128)
    out["Gm"] = (np.arange(128)[:, None] == (u[None, :] // 64)).astype(np.float32)
    idx = np.arange(960)
    m = idx - 448 - 8 * cidx
    out["mctn"] = np.where(16 * m[None, :] + 31 <= p[:, None], 0.0, NEG).astype(np.float32)
    mcnt = np.zeros((128, 3, 128), np.float32)
    for i, dp in enumerate((-16, -8, 0)):
        dd = dp - cidx
        ok = (16 * p[:, None] + 31 + 128 * dd) <= p[None, :]
        mcnt[:, i, :] = np.where(ok, 0.0, NEG)
    out["mcnt"] = mcnt
    jr = np.arange(240) - 112
    bt = 2 * cidx + (p >= 64).astype(np.int64)
    causal = jr[None, :] <= bt[:, None]
    forced = (jr[None, :] == bt[:, None]) | (jr[None, :] == bt[:, None] - 1)
    out["Arel"] = (causal & ~forced).astype(np.float32)
    out["Brel"] = np.where(~causal, -1e4, np.where(forced, 1e4, 0.0)).astype(np.float32)
    dzt = np.zeros((128, 8, 128), np.float32)
    dzt[:, cidx, :] = np.where(p[:, None] <= p[None, :], 0.0, NEG)
    out["dz"] = dzt
    wzt = np.zeros((128, 12, 128), np.float32)
    for r in range(12):
        diff = 128 * (cidx + 4 - r) + p[None, :] - p[:, None]
        wzt[:, r, :] = np.where((diff >= 0) & (diff < 512), 0.0, NEG)
    out["wz"] = wzt
    vz = np.zeros((128, 8, 128), np.float32)
    for r in range(8):
        if r < cidx:
            vz[:, 7 - r, :] = 1.0
        elif r == cidx:
            vz[:, 7 - r, :] = (p[:, None] < p[None, :]).astype(np.float32)
    out["vzr"] = vz
    out["ntri"] = -(p[:, None] >= p[None, :]).astype(np.float32)
    out["nones"] = -np.ones((128, 128), np.float32)
    out["ident"] = np.eye(128, dtype=np.float32)
    out["invf"] = _inv_freq()
    return out


_CONSTS = {}


def run_pb(h_parts, pa_res, positions, W, l, pos_parts=None, with_pa=False):
    key = "pba" if with_pa else "pb"
    if key not in _PROG:
        _PROG[key] = build_pb(with_pa)
    nc = _PROG[key]
    KTg = np.stack([np.asarray(pa_res[c]["KT"]) for c in range(NCORES)])
    Vg = np.stack([np.asarray(pa_res[c]["V"]) for c in range(NCORES)])
    cmp_end = np.arange(511) * 16 + 31
    pc = np.zeros(512, np.int32)
    pc[:511] = positions[cmp_end]
    poscmp = np.ascontiguousarray(pc.reshape(4, 128).T)
    in_maps = []
    for c in range(NCORES):
        if c not in _CONSTS:
            _CONSTS[c] = make_consts(c)
        m = dict(_CONSTS[c])
        m.update({
            "h": h_parts[c], "QT": np.asarray(pa_res[c]["QT"]), "gates": np.asarray(pa_res[c]["gates"]),
            "KTg": KTg, "Vg": Vg,
            "wck1": W["w_cmp_k1"][l], "wck2": W["w_cmp_k2"][l], "wcv1": W["w_cmp_v1"][l], "wcv2": W["w_cmp_v2"][l],
            "cmp_pos": W["cmp_pos"][l], "poscmp": poscmp,
            "ngrp": W["norm_grp"][l][None, :], "w_out": W["w_out"][l], "nffn": W["norm_ffn"][l][None, :],
            "w_gate": W["w_gate"][l], "w_up": W["w_up"][l], "w_down": W["w_down"][l],
            "nfinal": W["norm_final"][None, :],
        })
        if with_pa:
            m.update({"w_in": W["w_in"][l + 1], "nmix": W["norm_mix"][l + 1][None, :], "bg": W["b_gate"][l + 1][None, :],
                      "pos": np.ascontiguousarray(pos_parts[c].reshape(NK, 128).T)})
        in_maps.append(m)
    res = run_bass_kernel_spmd(nc, in_maps, core_ids=list(range(NCORES)))
    return res.results


def kernel(**inputs):
    W = {k: np.asarray(v) for k, v in inputs.items()}
    x = W["x"][0]
    positions = W["positions"][0]
    h_parts = shard_tokens(np.ascontiguousarray(x, dtype=np.float32))
    pos_parts = shard_tokens(positions.astype(np.int32))
    hn = None
    pa = run_pa(h_parts, pos_parts, W["w_in"][0], W["norm_mix"][0], W["b_gate"][0])
    for l in range(4):
        last = (l == 3)
        pb = run_pb(h_parts, pa, positions.astype(np.int32), W, l, pos_parts, with_pa=not last)
        h_parts = [np.asarray(pb[c]["h_out"]) for c in range(NCORES)]
        hn = [np.asarray(pb[c]["hn_out"]) for c in range(NCORES)]
        if not last:
            pa = [{"QT": pb[c]["QT_o"], "KT": pb[c]["KT_o"], "V": pb[c]["V_o"], "gates": pb[c]["gates_o"]}
                  for c in range(NCORES)]
    out = unshard_tokens(hn)
    return out[None].astype(np.float32)
```
